# Optimizing a Trainium2 kernel written in Bass

```python
import jax
import jax.numpy as jnp
from jax import lax
import numpy as np

D_MODEL = 4096
BATCH = 2
SEQ = 8192
DEPTH = 2

HEAD_DIM = 128
FOURIER_WIDTH = D_MODEL // 4
N_FOURIER_GROUPS = FOURIER_WIDTH // HEAD_DIM
ATTN_WIDTH = D_MODEL - FOURIER_WIDTH
N_ATTN_HEADS = ATTN_WIDTH // HEAD_DIM
IN_PROJ_WIDTH = FOURIER_WIDTH + 3 * ATTN_WIDTH
DILATED_PATTERNS = ((128, 1), (512, 4), (2048, 16))
ATTN_BLOCK = 128
ROPE_THETA = 500000.0
ROPE_DIM = HEAD_DIM // 4
N_EXPERTS = 16
EXPERT_FF = (4 * D_MODEL) // N_EXPERTS
CAPACITY_FACTOR = 2
N_MOD = 6
EPS = 1e-6
NEG_INF = -1e30

kernel_name = 'hybrid_fourier_dilated_ec_moe_block'


def rms_norm(x, gain):
    xf = x.astype(jnp.float32)
    inv = lax.rsqrt(jnp.mean(xf * xf, axis=-1, keepdims=True) + EPS)
    return (xf * inv).astype(x.dtype) * gain


def group_rms_norm(y, gain, n_groups):
    b, s, w = y.shape
    yg = y.reshape(b, s, n_groups, w // n_groups)
    return rms_norm(yg, gain.reshape(n_groups, w // n_groups)).reshape(b, s, w)


def partial_rotary(t, positions):
    half = ROPE_DIM // 2
    inv_freq = jnp.float32(ROPE_THETA) ** (-jnp.arange(half, dtype=jnp.float32) * (2.0 / ROPE_DIM))
    ang = positions.astype(jnp.float32)[..., None] * inv_freq
    cos = jnp.cos(ang)[:, :, None, :]
    sin = jnp.sin(ang)[:, :, None, :]
    t1 = t[..., :half].astype(jnp.float32)
    t2 = t[..., half:ROPE_DIM].astype(jnp.float32)
    rot = jnp.concatenate([t1 * cos - t2 * sin, t2 * cos + t1 * sin], axis=-1).astype(t.dtype)
    return jnp.concatenate([rot, t[..., ROPE_DIM:]], axis=-1)


def fourier_mix(u):
    b, s, _ = u.shape
    ug = u.reshape(b, s, N_FOURIER_GROUPS, HEAD_DIM).astype(jnp.float32)
    y = jnp.fft.fft2(ug, axes=(1, 3)).real
    return y.reshape(b, s, FOURIER_WIDTH).astype(u.dtype)


def dilated_window_attention(q, k, v, window, dilation):
    b, h, s, hd = q.shape
    hw = window // (2 * dilation)
    sub_len = s // dilation

    def to_residue(t):
        return t.reshape(b, h, sub_len, dilation, hd).transpose(0, 1, 3, 2, 4)

    qr, kr, vr = to_residue(q), to_residue(k), to_residue(v)
    nblk = -(-sub_len // ATTN_BLOCK)
    lq = nblk * ATTN_BLOCK
    span = ATTN_BLOCK + 2 * hw
    pad_q = [(0, 0)] * 3 + [(0, lq - sub_len), (0, 0)]
    pad_k = [(0, 0)] * 3 + [(hw, lq - sub_len + hw), (0, 0)]
    qb = jnp.pad(qr, pad_q).reshape(b, h, dilation, nblk, ATTN_BLOCK, hd)
    kp = jnp.pad(kr, pad_k)
    vp = jnp.pad(vr, pad_k)
    kidx = jnp.arange(nblk)[:, None] * ATTN_BLOCK + jnp.arange(span)[None, :]
    kw = jnp.take(kp, kidx, axis=3)
    vw = jnp.take(vp, kidx, axis=3)
    scores = jnp.einsum('bhrnqd,bhrnkd->bhrnqk', qb, kw, preferred_element_type=jnp.float32)
    qpos = jnp.arange(nblk)[:, None] * ATTN_BLOCK + jnp.arange(ATTN_BLOCK)[None, :]
    kpos = kidx - hw
    rel = kpos[:, None, :] - qpos[:, :, None]
    valid = (jnp.abs(rel) <= hw) & (kpos[:, None, :] >= 0) & (kpos[:, None, :] < sub_len)
    scores = jnp.where(valid, scores, NEG_INF)
    m = jnp.max(scores, axis=-1, keepdims=True)
    p = jnp.exp(scores - m)
    denom = jnp.sum(p, axis=-1, keepdims=True)
    out = jnp.einsum('bhrnqk,bhrnkd->bhrnqd', p, vw.astype(jnp.float32)) / denom
    lse = (m + jnp.log(denom))[..., 0]
    out = out.reshape(b, h, dilation, lq, hd)[:, :, :, :sub_len]
    out = out.transpose(0, 1, 3, 2, 4).reshape(b, h, s, hd)
    lse = lse.reshape(b, h, dilation, lq)[..., :sub_len].transpose(0, 1, 3, 2).reshape(b, h, s)
    return out, lse


def token_mixer(h, positions, w_in, q_gain, k_gain, out_gain_fourier, out_gain_attn, w_out):
    b, s, _ = h.shape
    u = h @ w_in
    u_f = u[..., :FOURIER_WIDTH]
    q, k, v = jnp.split(u[..., FOURIER_WIDTH:], 3, axis=-1)

    def to_heads(t):
        return t.reshape(b, s, N_ATTN_HEADS, HEAD_DIM)

    q = partial_rotary(rms_norm(to_heads(q), q_gain), positions) * (HEAD_DIM ** -0.5)
    k = partial_rotary(rms_norm(to_heads(k), k_gain), positions)
    v = to_heads(v)
    q, k, v = (t.transpose(0, 2, 1, 3) for t in (q, k, v))

    outs, lses = [], []
    for window, dilation in DILATED_PATTERNS:
        o, l = dilated_window_attention(q, k, v, window, dilation)
        outs.append(o)
        lses.append(l)
    mix = jax.nn.softmax(jnp.stack(lses, axis=0), axis=0)
    y_attn = jnp.einsum('pbhs,pbhsd->bshd', mix, jnp.stack(outs, axis=0))
    y_attn = y_attn.reshape(b, s, ATTN_WIDTH).astype(h.dtype)

    y_four = fourier_mix(u_f)
    y = jnp.concatenate([
        group_rms_norm(y_four, out_gain_fourier, N_FOURIER_GROUPS),
        group_rms_norm(y_attn, out_gain_attn, N_ATTN_HEADS),
    ], axis=-1)
    return y @ w_out


def expert_choice_moe(h, w_router, w_gate, w_up, w_down):
    b, s, d = h.shape
    cap = max(1, min(s, CAPACITY_FACTOR * s // N_EXPERTS))
    logits = jnp.einsum('bsd,de->bse', h, w_router).astype(jnp.float32)
    affinity = jax.nn.softmax(logits, axis=-1)
    gates, idx = lax.top_k(affinity.transpose(0, 2, 1), cap)
    xe = jax.vmap(lambda hb, ib: hb[ib])(h, idx)
    g = jnp.einsum('becd,edf->becf', xe, w_gate)
    up = jnp.einsum('becd,edf->becf', xe, w_up)
    ye = jnp.einsum('becf,efd->becd', jax.nn.silu(g) * up, w_down)
    ye = ye * gates[..., None].astype(ye.dtype)
    return jax.vmap(lambda yb, ib: jax.ops.segment_sum(
        yb.reshape(-1, d), ib.reshape(-1), num_segments=s))(ye, idx)


def setup_inputs(seed: int = 0) -> dict:
    key = jax.random.key(seed)
    ks = jax.random.split(key, 20)
    f32 = jnp.float32
    d = D_MODEL

    def normal(k, shape, scale):
        return jax.random.normal(k, shape, f32) * scale

    def gain(k, shape):
        return 1.0 + 0.02 * jax.random.normal(k, shape, f32)

    return {
        'x': normal(ks[0], (BATCH, SEQ, d), 1.0),
        'c': normal(ks[1], (BATCH, d), 1.0),
        'positions': jnp.broadcast_to(jnp.arange(SEQ, dtype=jnp.int32), (BATCH, SEQ)),
        'norm1_gain': gain(ks[2], (DEPTH, d)),
        'norm2_gain': gain(ks[3], (DEPTH, d)),
        'w_ada': normal(ks[4], (DEPTH, d, N_MOD * d), 0.5 * d ** -0.5),
        'b_ada': normal(ks[5], (DEPTH, N_MOD * d), 0.02),
        'w_in': normal(ks[6], (DEPTH, d, IN_PROJ_WIDTH), d ** -0.5),
        'q_gain': gain(ks[7], (DEPTH, HEAD_DIM)),
        'k_gain': gain(ks[8], (DEPTH, HEAD_DIM)),
        'out_gain_fourier': gain(ks[9], (DEPTH, FOURIER_WIDTH)),
        'out_gain_attn': gain(ks[10], (DEPTH, ATTN_WIDTH)),
        'w_out': normal(ks[11], (DEPTH, d, d), d ** -0.5),
        'w_router': normal(ks[12], (DEPTH, d, N_EXPERTS), d ** -0.5),
        'w_gate': normal(ks[13], (DEPTH, N_EXPERTS, d, EXPERT_FF), d ** -0.5),
        'w_up': normal(ks[14], (DEPTH, N_EXPERTS, d, EXPERT_FF), d ** -0.5),
        'w_down': normal(ks[15], (DEPTH, N_EXPERTS, EXPERT_FF, d), EXPERT_FF ** -0.5),
    }


def reference(x, c, positions, norm1_gain, norm2_gain, w_ada, b_ada, w_in, q_gain, k_gain,
              out_gain_fourier, out_gain_attn, w_out, w_router, w_gate, w_up, w_down):
    cond = jax.nn.silu(c)
    for layer in range(DEPTH):
        mod = cond @ w_ada[layer] + b_ada[layer]
        shift1, scale1, gate1, shift2, scale2, gate2 = jnp.split(mod[:, None, :], N_MOD, axis=-1)
        h = rms_norm(x, norm1_gain[layer]) * (1.0 + scale1) + shift1
        x = x + gate1 * token_mixer(h, positions, w_in[layer], q_gain[layer], k_gain[layer],
                                    out_gain_fourier[layer], out_gain_attn[layer], w_out[layer])
        h = rms_norm(x, norm2_gain[layer]) * (1.0 + scale2) + shift2
        x = x + gate2 * expert_choice_moe(h, w_router[layer], w_gate[layer], w_up[layer], w_down[layer])
    return x
```

```python
import math
import numpy as np
import ml_dtypes
import concourse.bass as bass
import concourse.mybir as mybir
from concourse.bass_utils import run_bass_kernel_spmd

F32 = mybir.dt.float32
BF16 = mybir.dt.bfloat16
I32 = mybir.dt.int32
U32 = mybir.dt.uint32
ALU = mybir.AluOpType
ACT = mybir.ActivationFunctionType
AX = mybir.AxisListType


class Res:
    def __init__(self, k, name, t=None, own_sem=True):
        self.k = k
        self.name = name
        self.t = t
        self.writer = None
        self.readers = []
        self.dsem = None
        self.dcount = 0

    def __getitem__(self, idx):
        return self.t[idx]

    def sem(self):
        if self.dsem is None:
            self.dsem = self.k.new_sem("d_" + self.name)
        return self.dsem


class Eng:
    def __init__(self, k, name, h):
        self.k = k
        self.name = name
        self.h = h
        self.sem = k.new_sem("e_" + name)
        self.count = 0
        self.seen = {}
        self.seen_d = {}


class K:
    def __init__(self, nc):
        self.nc = nc
        self._ctx = []
        self.nsem = 0
        self.engs = {}
        for name, h in (("pe", nc.tensor), ("act", nc.scalar), ("dve", nc.vector),
                        ("pool", nc.gpsimd), ("sp", nc.sync)):
            self.engs[name] = Eng(self, name, h)
        self.all_res = []

    def new_sem(self, name):
        cm = self.nc.semaphore(name + "_%d" % self.nsem)
        self.nsem += 1
        s = cm.__enter__()
        self._ctx.append(cm)
        return s

    def sbuf(self, name, shape, dtype):
        cm = self.nc.sbuf_tensor(name, list(shape), dtype)
        t = cm.__enter__()
        self._ctx.append(cm)
        r = Res(self, name, t)
        self.all_res.append(r)
        return r

    def psum(self, name, shape, dtype):
        cm = self.nc.psum_tensor(name, list(shape), dtype)
        t = cm.__enter__()
        self._ctx.append(cm)
        r = Res(self, name, t)
        self.all_res.append(r)
        return r

    def dram(self, name, ap):
        r = Res(self, name, ap)
        self.all_res.append(r)
        return r

    def close(self):
        for cm in reversed(self._ctx):
            cm.__exit__(None, None, None)
        self._ctx = []

    def _wait(self, e, dep, raw):
        if dep is None:
            return
        if dep[0] == "eng":
            _, fname, seq = dep
            if fname == e.name:
                if not raw or e.name in ("pe", "sp"):
                    return
            if e.seen.get(fname, 0) >= seq:
                return
            e.h.wait_ge(self.engs[fname].sem, seq)
            e.seen[fname] = seq
        else:
            _, res, cnt = dep
            if e.seen_d.get(id(res), 0) >= cnt:
                return
            e.h.wait_ge(res.sem(), 16 * cnt)
            e.seen_d[id(res)] = cnt

    def _deps(self, e, reads, writes):
        for r in reads:
            self._wait(e, r.writer, True)
        for w in writes:
            self._wait(e, w.writer, False)
            for d in w.readers:
                self._wait(e, d, False)

    def op(self, ename, fn, reads=(), writes=(), signal=True):
        e = self.engs[ename]
        self._deps(e, reads, writes)
        ins = fn(e.h)
        seq = e.count + 1
        if signal:
            ins.then_inc(e.sem, 1)
            e.count = seq
        dep = ("eng", ename, seq)
        for r in reads:
            r.readers.append(dep)
        for w in writes:
            w.writer = dep
            w.readers = []
        return ins

    def dma(self, out_ap, in_ap, reads=(), writes=(), semres=None, q="sp", **kw):
        e = self.engs[q]
        self._deps(e, reads, writes)
        sr = semres if semres is not None else (list(writes) + list(reads))[0]
        ins = e.h.dma_start(out=out_ap, in_=in_ap, **kw)
        ins.then_inc(sr.sem(), 16)
        sr.dcount += 1
        dep = ("dma", sr, sr.dcount)
        for r in reads:
            r.readers.append(dep)
        for w in writes:
            w.writer = dep
            w.readers = []
        return ins

    def finish(self, ename="sp"):
        e = self.engs[ename]
        for r in self.all_res:
            if r.dcount:
                self._wait(e, ("dma", r, r.dcount), True)
        for n, f in self.engs.items():
            if n != ename and f.count:
                self._wait(e, ("eng", n, f.count), True)

    def barrier(self):
        for n in self.engs:
            self.finish(n)


def run(nc, in_maps, n=8, trace=False):
    return run_bass_kernel_spmd(nc, in_maps, core_ids=list(range(n)), trace=trace)


def build_p0():
    nc = bass.Bass("TRN2", target_bir_lowering=False)
    NCOL = 3072
    cT = nc.dram_tensor("cT", [128, 64], F32, kind="ExternalInput").ap()
    wa = nc.dram_tensor("wa", [2, 4096, NCOL], F32, kind="ExternalInput").ap()
    ba = nc.dram_tensor("ba", [1, 2 * NCOL], F32, kind="ExternalInput").ap()
    mod = nc.dram_tensor("mod", [2, 2 * NCOL], F32, kind="ExternalOutput").ap()
    k = K(nc)
    ct = k.sbuf("ct", [128, 64], F32)
    sc = k.sbuf("sc", [128, 64], F32)
    ones = k.sbuf("ones", [1, 2], F32)
    bt = k.sbuf("bt", [1, 2 * NCOL], F32)
    ob = k.sbuf("ob", [2, 2 * NCOL], F32)
    wt = [k.sbuf("wt%d" % i, [128, 32, 512], F32) for i in range(2)]
    ps = [k.psum("ps%d" % i, [2, 512], F32) for i in range(2)]
    k.dma(ct[:], cT, writes=[ct])
    k.dma(bt[:], ba, writes=[bt])
    k.op("act", lambda e: e.activation(out=sc[:], in_=ct[:], func=ACT.Silu), reads=[ct], writes=[sc])
    k.op("dve", lambda e: e.memset(ones[:], 1.0), writes=[ones])
    it = 0
    for l in range(2):
        for nb in range(NCOL // 512):
            w = wt[it % 2]; p = ps[it % 2]
            src = wa[l, :, nb * 512:(nb + 1) * 512].rearrange("(kc p) n -> p kc n", p=128)
            k.dma(w[:, 0:16, :], src[:, 0:16, :], writes=[w])
            k.dma(w[:, 16:32, :], src[:, 16:32, :], writes=[w])
            for kc in range(32):
                k.op("pe", lambda e: e.matmul(p[:], lhsT=sc[:, 2 * kc:2 * kc + 2], rhs=w[:, kc, :],
                                              start=(kc == 0), stop=False),
                     reads=[sc, w], writes=[p], signal=False)
            col = l * NCOL + nb * 512
            k.op("pe", lambda e: e.matmul(p[:], lhsT=ones[:], rhs=bt[:, col:col + 512], start=False, stop=True),
                 reads=[ones, bt], writes=[p])
            k.op("dve", lambda e: e.tensor_copy(out=ob[:, col:col + 512], in_=p[:]), reads=[p], writes=[ob])
            it += 1
    k.dma(mod, ob[:], reads=[ob])
    k.finish("sp")
    k.close()
    return nc


EPS = 1e-6

def norm_transpose_affine(k, xt, ident, Gc, Sc, dst, dst_cols, pT, tmp, dst32=None):
    ssq, rs, xs, junk = tmp["ssq"], tmp["rs"], tmp["xs"], tmp["junk"]
    k.op("act", lambda e: e.activation(out=xs[:].bitcast(BF16)[:, 0:4096], in_=xt[:], func=ACT.Square, accum_out=ssq[:]),
         reads=[xt], writes=[xs, ssq])
    k.op("dve", lambda e: e.tensor_scalar(out=rs[:], in0=ssq[:], scalar1=1.0 / 4096, scalar2=EPS,
                                          op0=ALU.mult, op1=ALU.add), reads=[ssq], writes=[rs])
    k.op("act", lambda e: e.activation(out=rs[:], in_=rs[:], func=ACT.Sqrt), reads=[rs], writes=[rs])
    k.op("dve", lambda e: e.reciprocal(out=rs[:], in_=rs[:]), reads=[rs], writes=[rs])
    k.op("dve", lambda e: e.tensor_scalar(out=xs[:], in0=xt[:], scalar1=rs[:, 0:1], scalar2=None, op0=ALU.mult),
         reads=[xt, rs], writes=[xs])
    for g in range(8):
        p = pT[g % 2]
        for j in range(4):
            kc = 4 * g + j
            k.op("pe", lambda e: e.transpose(out=p[:, j, :], in_=xs[:, kc * 128:(kc + 1) * 128], identity=ident[:]),
                 reads=[xs, ident], writes=[p], signal=(j == 3))
        for j in range(4):
            kc = 4 * g + j
            eng = "dve" if (j % 2 == 0) else "pool"
            if dst is None:
                pass
            elif eng == "pool":
                k.op("act", lambda e: e.activation(out=dst[:, kc, dst_cols], in_=p[:, j, :], func=ACT.Identity,
                                                   scale=Gc[:, kc:kc + 1], bias=Sc[:, kc:kc + 1]),
                     reads=[p, Gc, Sc], writes=[dst])
            else:
                k.op("dve", lambda e: e.tensor_scalar(out=dst[:, kc, dst_cols], in0=p[:, j, :],
                                                      scalar1=Gc[:, kc:kc + 1], scalar2=Sc[:, kc:kc + 1],
                                                      op0=ALU.mult, op1=ALU.add),
                     reads=[p, Gc, Sc], writes=[dst])
            if dst32 is not None:
                k.op("act", lambda e: e.activation(out=dst32[:, kc, :], in_=p[:, j, :], func=ACT.Identity,
                                                   scale=Gc[:, kc:kc + 1], bias=Sc[:, kc:kc + 1]),
                     reads=[p, Gc, Sc], writes=[dst32])


def load_w_stage(k, wsrc, stage, Wb, it, s):
    src = wsrc.rearrange("(kc p) n -> p kc n", p=128)
    st = stage[(it * 8 + s) % len(stage)]
    k.dma(st[:], src[:, 4 * s:4 * s + 4, :], writes=[st])
    eng = ("dve", "pool", "act", "pool")[s % 4]
    if eng == "act":
        k.op("act", lambda e: e.activation(out=Wb[:, 4 * s:4 * s + 4, :], in_=st[:], func=ACT.Copy),
             reads=[st], writes=[Wb])
    else:
        k.op(eng, lambda e: e.tensor_copy(out=Wb[:, 4 * s:4 * s + 4, :], in_=st[:]), reads=[st], writes=[Wb])


def load_w_block(k, wsrc, stage, Wb, it):
    for s in range(8):
        load_w_stage(k, wsrc, stage, Wb, it, s)


def build_pa(NT=2048, HALF=1024, NCB=20):
    nc = bass.Bass("TRN2", target_bir_lowering=False)
    NTT = NT // 128
    x = nc.dram_tensor("x", [NT, 4096], F32, kind="ExternalInput").ap()
    cols = nc.dram_tensor("cols", [128, 96], F32, kind="ExternalInput").ap()
    w = nc.dram_tensor("w", [4096, NCB * 512], F32, kind="ExternalInput").ap()
    qkg = nc.dram_tensor("qkg", [128, 256], F32, kind="ExternalInput").ap()
    pos = nc.dram_tensor("pos", [128, NTT], I32, kind="ExternalInput").ap()
    invf = nc.dram_tensor("invf", [128, 16], F32, kind="ExternalInput").ap()
    identd = nc.dram_tensor("ident", [128, 128], F32, kind="ExternalInput").ap()
    u = nc.dram_tensor("u", [NT, NCB * 512], BF16, kind="ExternalOutput").ap()
    k = K(nc)
    colt = k.sbuf("colt", [128, 96], F32)
    Gc = k.sbuf("Gc", [128, 32], F32)
    qk = k.sbuf("qk", [128, 256], F32)
    post = k.sbuf("post", [128, NTT], I32)
    posf = k.sbuf("posf", [128, NTT], F32)
    invt = k.sbuf("invt", [128, 16], F32)
    ident = k.sbuf("ident_s", [128, 128], F32)
    cosA = k.sbuf("cosA", [128, NTT, 16], F32)
    sinA = k.sbuf("sinA", [128, NTT, 16], F32)
    ang = k.sbuf("ang", [128, 16], F32)
    hT = [k.sbuf("hT%d" % i, [128, 32, 128], BF16) for i in range(HALF // 128)]
    Wb = [k.sbuf("Wb%d" % i, [128, 32, 512], BF16) for i in range(2)]
    stage = [k.sbuf("stg%d" % i, [128, 4, 512], F32) for i in range(2)]
    xt = k.sbuf("xt", [128, 4096], F32)
    tmp = {"ssq": k.sbuf("ssq", [128, 1], F32), "rs": k.sbuf("rs", [128, 1], F32),
           "xs": k.sbuf("xs", [128, 4096], F32), "junk": None}
    pT = [k.psum("pT%d" % i, [128, 4, 128], F32) for i in range(2)]
    ps = [k.psum("ps%d" % i, [128, 512], F32) for i in range(3)]
    sq = k.sbuf("sq", [128, 512], BF16)
    s4 = k.sbuf("s4", [128, 4], F32)
    t1 = k.sbuf("t1", [128, 4, 128], F32)
    t2 = k.sbuf("t2", [128, 4, 128], F32)
    ra = k.sbuf("ra", [128, 4, 16], F32)
    rb = k.sbuf("rb", [128, 4, 16], F32)
    rc = k.sbuf("rc", [128, 4, 16], F32)
    rd = k.sbuf("rd", [128, 4, 16], F32)
    ob = [k.sbuf("ob%d" % i, [128, 512], BF16) for i in range(3)]
    udram = k.dram("udram", u)

    k.dma(colt[:], cols, writes=[colt])
    k.dma(qk[:], qkg, writes=[qk])
    k.dma(post[:], pos, writes=[post])
    k.dma(invt[:], invf, writes=[invt])
    k.dma(ident[:], identd, writes=[ident])
    k.op("dve", lambda e: e.tensor_scalar(out=Gc[:], in0=colt[:, 32:64], scalar1=1.0, scalar2=None, op0=ALU.add),
         reads=[colt], writes=[Gc])
    k.op("dve", lambda e: e.tensor_tensor(out=Gc[:], in0=Gc[:], in1=colt[:, 0:32], op=ALU.mult),
         reads=[Gc, colt], writes=[Gc])
    Sc = colt
    class _S:
        def __getitem__(self, idx):
            return colt.t[idx[0], slice(64 + idx[1].start, 64 + idx[1].stop)]
    k.op("dve", lambda e: e.tensor_scalar(out=qk[:, 0:128], in0=qk[:, 0:128], scalar1=128.0 ** -0.5, scalar2=None,
                                          op0=ALU.mult), reads=[qk], writes=[qk])
    k.op("dve", lambda e: e.tensor_copy(out=posf[:], in_=post[:]), reads=[post], writes=[posf])
    angA = k.sbuf("angA", [128, NTT, 16], F32)
    aA = k.sbuf("aA", [128, NTT, 16], F32)
    kI = k.sbuf("kI", [128, NTT, 16], I32)
    kF = k.sbuf("kF", [128, NTT, 16], F32)
    C1 = 6.28125
    C2 = 2.0 * math.pi - C1
    k.op("dve", lambda e: e.tensor_tensor(out=angA[:], in0=posf[:].unsqueeze(2).broadcast_to([128, NTT, 16]),
                                          in1=invt[:].unsqueeze(1).broadcast_to([128, NTT, 16]), op=ALU.mult),
         reads=[posf, invt], writes=[angA])
    for (dstT, off) in ((cosA, 0.5 * math.pi), (sinA, 0.0)):
        k.op("dve", lambda e: e.tensor_scalar(out=aA[:], in0=angA[:], scalar1=off, scalar2=None, op0=ALU.add),
             reads=[angA], writes=[aA])
        k.op("dve", lambda e: e.tensor_scalar(out=kI[:], in0=aA[:], scalar1=1.0 / (2.0 * math.pi), scalar2=None, op0=ALU.mult),
             reads=[aA], writes=[kI])
        k.op("dve", lambda e: e.tensor_copy(out=kF[:], in_=kI[:]), reads=[kI], writes=[kF])
        k.op("dve", lambda e: e.scalar_tensor_tensor(out=aA[:], in0=kF[:], scalar=-C1, in1=aA[:], op0=ALU.mult, op1=ALU.add),
             reads=[kF, aA], writes=[aA])
        k.op("dve", lambda e: e.scalar_tensor_tensor(out=aA[:], in0=kF[:], scalar=-C2, in1=aA[:], op0=ALU.mult, op1=ALU.add),
             reads=[kF, aA], writes=[aA])
        k.op("dve", lambda e: e.tensor_scalar(out=aA[:], in0=aA[:], scalar1=-math.pi, scalar2=math.pi, op0=ALU.max, op1=ALU.min),
             reads=[aA], writes=[aA])
        k.op("act", lambda e: e.activation(out=dstT[:], in_=aA[:], func=ACT.Sin), reads=[aA], writes=[dstT])
    ShiftView = _S()
    class _SRes:
        pass
    it = 0
    pi = 0
    oi = 0
    for half in range(NT // HALF):
        for tt in range(HALF // 128):
            gt = half * (HALF // 128) + tt
            k.dma(xt[:], x[gt * 128:(gt + 1) * 128, :], writes=[xt])
            norm_transpose_affine(k, xt, ident, Gc, _ColView(colt, 64), hT[tt], slice(0, 128), pT, tmp)
        for cb in range(NCB):
            W = Wb[it % 2]
            if it == 0:
                load_w_block(k, w[:, cb * 512:(cb + 1) * 512], stage, W, it)
            it += 1
            nxt = it if it < (NT // HALF) * NCB else None
            nparts = HALF // 128
            kind = "plain"
            if NCB == 20:
                if 2 <= cb < 8: kind = "q"
                elif 8 <= cb < 14: kind = "k"
            else:
                kind = ("plain", "q", "k", "plain")[cb % 4]
            for tt in range(HALF // 128):
                gt = half * (HALF // 128) + tt
                p = ps[pi % 3]; pi += 1
                for kc in range(32):
                    k.op("pe", lambda e: e.matmul(p[:], lhsT=hT[tt][:, kc, :], rhs=W[:, kc, :],
                                                  start=(kc == 0), stop=(kc == 31)),
                         reads=[hT[tt], W], writes=[p], signal=(kc == 31))
                if nxt is not None:
                    ncb = nxt % NCB
                    for s_ in range(8):
                        if s_ * nparts // 8 == tt:
                            load_w_stage(k, w[:, ncb * 512:(ncb + 1) * 512], stage, Wb[nxt % 2], nxt, s_)
                o = ob[oi % 3]; oi += 1
                if kind == "plain":
                    k.op("act", lambda e: e.activation(out=o[:], in_=p[:], func=ACT.Copy), reads=[p], writes=[o])
                else:
                    g0 = 0 if kind == "q" else 128
                    pv = p[:].rearrange("p (h d) -> p h d", h=4)
                    k.op("act", lambda e: e.activation(out=sq[:], in_=p[:], func=ACT.Square), reads=[p], writes=[sq])
                    k.op("dve", lambda e: e.tensor_reduce(out=s4[:], in_=sq[:].rearrange("p (h d) -> p h d", h=4),
                                                          axis=AX.X, op=ALU.add), reads=[sq], writes=[s4])
                    k.op("dve", lambda e: e.tensor_scalar(out=s4[:], in0=s4[:], scalar1=1.0 / 128, scalar2=EPS,
                                                          op0=ALU.mult, op1=ALU.add), reads=[s4], writes=[s4])
                    k.op("act", lambda e: e.activation(out=s4[:], in_=s4[:], func=ACT.Sqrt), reads=[s4], writes=[s4])
                    k.op("dve", lambda e: e.reciprocal(out=s4[:], in_=s4[:]), reads=[s4], writes=[s4])
                    k.op("dve", lambda e: e.tensor_tensor(out=t1[:], in0=pv, in1=s4[:].unsqueeze(2).broadcast_to([128, 4, 128]),
                                                          op=ALU.mult), reads=[p, s4], writes=[t1])
                    k.op("pool", lambda e: e.tensor_tensor(out=t2[:], in0=t1[:],
                                                           in1=qk[:, g0:g0 + 128].unsqueeze(1).broadcast_to([128, 4, 128]),
                                                           op=ALU.mult), reads=[t1, qk], writes=[t2])
                    cb_ = cosA[:, gt, :].unsqueeze(1).broadcast_to([128, 4, 16])
                    sb_ = sinA[:, gt, :].unsqueeze(1).broadcast_to([128, 4, 16])
                    A = t2[:, :, 0:16]; B = t2[:, :, 16:32]
                    k.op("dve", lambda e: e.tensor_tensor(out=ra[:], in0=A, in1=cb_, op=ALU.mult), reads=[t2, cosA], writes=[ra])
                    k.op("pool", lambda e: e.tensor_tensor(out=rb[:], in0=B, in1=sb_, op=ALU.mult), reads=[t2, sinA], writes=[rb])
                    k.op("dve", lambda e: e.tensor_tensor(out=rc[:], in0=B, in1=cb_, op=ALU.mult), reads=[t2, cosA], writes=[rc])
                    k.op("pool", lambda e: e.tensor_tensor(out=rd[:], in0=A, in1=sb_, op=ALU.mult), reads=[t2, sinA], writes=[rd])
                    k.op("dve", lambda e: e.tensor_tensor(out=t2[:, :, 0:16], in0=ra[:], in1=rb[:], op=ALU.subtract),
                         reads=[ra, rb], writes=[t2])
                    k.op("dve", lambda e: e.tensor_tensor(out=t2[:, :, 16:32], in0=rc[:], in1=rd[:], op=ALU.add),
                         reads=[rc, rd], writes=[t2])
                    k.op("act", lambda e: e.activation(out=o[:], in_=t2[:].rearrange("p h d -> p (h d)"), func=ACT.Copy),
                         reads=[t2], writes=[o])
                k.dma(u[gt * 128:(gt + 1) * 128, cb * 512:(cb + 1) * 512], o[:], reads=[o], writes=[udram], q="act")
    k.finish("sp")
    k.close()
    return nc


class _ColView:
    def __init__(self, res, off):
        self.res = res; self.off = off
        self.__dict__["_r"] = res
    def __getitem__(self, idx):
        a, b = idx
        return self.res.t[a, slice(self.off + b.start, self.off + b.stop)]
    def __getattr__(self, n):
        return getattr(self.__dict__["_r"], n)
    def __setattr__(self, n, v):
        if n in ("res", "off"):
            self.__dict__[n] = v
        else:
            setattr(self.__dict__["_r"], n, v)


def host_inputs_pa(xc, gain1, scale1, shift1, w_in, q_gain, k_gain, positions_c):
    def col(v): return np.ascontiguousarray(v.reshape(32, 128).T)
    cols = np.concatenate([col(gain1), col(scale1), col(shift1)], axis=1).astype(np.float32)
    qkg = np.concatenate([np.tile(q_gain[None, :], (128, 1)), np.tile(k_gain[None, :], (128, 1))], axis=1).astype(np.float32)
    NT = xc.shape[0]
    pos = np.ascontiguousarray(positions_c.reshape(NT // 128, 128).T).astype(np.int32)
    invf = (np.float32(500000.0) ** (-np.arange(16, dtype=np.float32) * np.float32(2.0 / 32))).astype(np.float32)
    return {"x": xc, "cols": cols, "w": w_in, "qkg": qkg, "pos": pos,
            "invf": np.tile(invf[None, :], (128, 1)), "ident": np.eye(128, dtype=np.float32)}


PATS = (1, 4, 16)

def build_pb(S=8192, NU=6, NF=2):
    nc = bass.Bass("TRN2", target_bir_lowering=False)
    SP = S + 2112
    qTd = nc.dram_tensor("qT", [NU, 128, S], BF16, kind="ExternalInput").ap()
    kTd = nc.dram_tensor("kT", [NU, 128, S + 2048], BF16, kind="ExternalInput").ap()
    vpd = nc.dram_tensor("vp", [NU, SP, 129], BF16, kind="ExternalInput").ap()
    gad = nc.dram_tensor("ga", [NU, 128, 128], F32, kind="ExternalInput").ap()
    maskd = nc.dram_tensor("mask", [128, 256], BF16, kind="ExternalInput").ap()
    yad = nc.dram_tensor("ya", [NU, S, 128], BF16, kind="ExternalOutput").ap()
    NB = S // 128
    N1 = S // 128
    ufd = nc.dram_tensor("uf", [NF, N1, 128 * 128], BF16, kind="ExternalInput").ap()
    d64d = nc.dram_tensor("d64", [N1, 2 * N1], BF16, kind="ExternalInput").ap()
    gabd = nc.dram_tensor("gab", [2, 128, N1, 256], BF16, kind="ExternalInput").ap()
    csd = nc.dram_tensor("cs", [128, 256], BF16, kind="ExternalInput").ap()
    gfd = nc.dram_tensor("gf", [NF, 128, 128], F32, kind="ExternalInput").ap()
    yfd = nc.dram_tensor("yf", [NF, S, 128], BF16, kind="ExternalOutput").ap()
    accd = [nc.dram_tensor("acc%d" % i, [3, S, 129], F32, kind="Internal").ap() for i in range(2)]
    k = K(nc)
    accR = [k.dram("accR%d" % i, accd[i]) for i in range(2)]
    yaR = k.dram("yaR", yad); yfR = k.dram("yfR", yfd)
    mask = k.sbuf("mask_s", [128, 256], BF16)
    k.dma(mask[:], maskd, writes=[mask])
    qT = k.sbuf("qTs", [128, S], BF16)
    kT = k.sbuf("kTs", [128, S + 2048], BF16)
    NVT = max((S // (128 * d) + 1) * d for d in PATS)
    Vb = [k.sbuf("Vb%d" % i, [128, NVT, 129], BF16) for i in range(2)]
    gaL = [k.sbuf("ga_s%d" % i, [128, 128], F32) for i in range(2)]
    st = [k.psum("st%d" % i, [128, 512], F32) for i in range(2)]
    ops = [k.psum("o%d" % i, [128, 2, 129], F32) for i in range(2)]
    pt = [k.sbuf("pt%d" % i, [128, 512], BF16) for i in range(3)]
    GB_ = 8
    stg = [k.sbuf("stg%d" % i, [128, GB_, 129], F32) for i in range(2)]
    a3 = [k.sbuf("a3_%d" % i, [128, 8, 129], F32) for i in range(3)]
    sq8 = k.sbuf("sq8", [128, 8, 128], BF16)
    s8 = k.sbuf("s8", [128, 8], F32)
    d8 = k.sbuf("d8", [128, 8], F32)
    y1 = k.sbuf("y1", [128, 8, 128], F32)
    yo = [k.sbuf("yo%d" % i, [128, 8, 128], BF16) for i in range(2)]
    vi = 0; bi = 0; gi = 0; yi = 0
    def second_pass(u):
        nonlocal yi
        acc = accd[u % 2]; aR = accR[u % 2]; ga = gaL[u % 2]
        for qd in range(S // 1024):
            for pi_ in range(3):
                src = acc[pi_, qd * 1024:(qd + 1) * 1024, :].rearrange("(p t) c -> p t c", p=128)
                k.dma(a3[pi_][:], src, reads=[aR], writes=[a3[pi_]])
            k.op("dve", lambda e: e.tensor_tensor(out=a3[0][:], in0=a3[0][:], in1=a3[1][:], op=ALU.add),
                 reads=[a3[0], a3[1]], writes=[a3[0]])
            k.op("dve", lambda e: e.tensor_tensor(out=a3[0][:], in0=a3[0][:], in1=a3[2][:], op=ALU.add),
                 reads=[a3[0], a3[2]], writes=[a3[0]])
            num = a3[0][:, :, 0:128]
            den = a3[0][:, :, 128]
            k.op("act", lambda e: e.activation(out=sq8[:], in_=num, func=ACT.Square), reads=[a3[0]], writes=[sq8])
            k.op("dve", lambda e: e.tensor_reduce(out=s8[:], in_=sq8[:], axis=AX.X, op=ALU.add), reads=[sq8], writes=[s8])
            k.op("dve", lambda e: e.tensor_tensor(out=d8[:], in0=den, in1=den, op=ALU.mult), reads=[a3[0]], writes=[d8])
            k.op("dve", lambda e: e.tensor_scalar(out=d8[:], in0=d8[:], scalar1=EPS, scalar2=None, op0=ALU.mult), reads=[d8], writes=[d8])
            k.op("dve", lambda e: e.scalar_tensor_tensor(out=s8[:], in0=s8[:], scalar=1.0 / 128, in1=d8[:], op0=ALU.mult, op1=ALU.add),
                 reads=[s8, d8], writes=[s8])
            k.op("act", lambda e: e.activation(out=s8[:], in_=s8[:], func=ACT.Sqrt), reads=[s8], writes=[s8])
            k.op("dve", lambda e: e.reciprocal(out=s8[:], in_=s8[:]), reads=[s8], writes=[s8])
            k.op("dve", lambda e: e.tensor_tensor(out=y1[:], in0=num, in1=s8[:].unsqueeze(2).broadcast_to([128, 8, 128]), op=ALU.mult),
                 reads=[a3[0], s8], writes=[y1])
            yo_ = yo[yi % 2]; yi += 1
            k.op("pool", lambda e: e.tensor_tensor(out=yo_[:], in0=y1[:], in1=ga[:].unsqueeze(1).broadcast_to([128, 8, 128]), op=ALU.mult),
                 reads=[y1, ga], writes=[yo_])
            k.dma(yad[u, qd * 1024:(qd + 1) * 1024, :].rearrange("(p t) c -> p t c", p=128), yo_[:], reads=[yo_], writes=[yaR])

    for u in range(NU):
        acc = accd[u % 2]; aR = accR[u % 2]
        k.dma(qT[:], qTd[u], writes=[qT])
        k.dma(kT[:], kTd[u], writes=[kT])
        ga = gaL[u % 2]
        k.dma(ga[:], gad[u], writes=[ga])
        def load_v(uu, d, V):
            nblk_ = S // (128 * d)
            for r in range(d):
                base = 1024 - 64 * d + r
                L = (nblk_ + 1) * 128 * d
                src = vpd[uu, base:base + L, :].rearrange("(j p d) c -> p j d c", p=128, d=d)[:, :, 0, :]
                for j0 in range(0, nblk_ + 1, 16):
                    j1 = min(nblk_ + 1, j0 + 16)
                    k.dma(V[:, r * (nblk_ + 1) + j0:r * (nblk_ + 1) + j1, :], src[:, j0:j1, :], writes=[V])

        if u == 0:
            load_v(0, PATS[0], Vb[vi % 2])
        for pi_, d in enumerate(PATS):
            nblk = S // (128 * d)
            V = Vb[vi % 2]; vi += 1
            if pi_ + 1 < len(PATS):
                load_v(u, PATS[pi_ + 1], Vb[vi % 2])
            elif u + 1 < NU:
                load_v(u + 1, PATS[0], Vb[vi % 2])
            groups = [(r, n) for r in range(d) for n in range(0, nblk, 2)]

            def emit_qk(gidx_):
                r, n = groups[gidx_]
                s_ = st[gidx_ % 2]
                for bb in range(2):
                    qs = (128 * (n + bb)) * d + r
                    qcols = qT[:, qs:qs + 127 * d + 1:d]
                    for half in range(2):
                        ks = 1024 + (128 * (n + bb + half) - 64) * d + r
                        c0 = bb * 256 + half * 128
                        k.op("pe", lambda e: e.matmul(s_[:, c0:c0 + 128], lhsT=kT[:, ks:ks + 127 * d + 1:d],
                                                      rhs=qcols, start=True, stop=True),
                             reads=[kT, qT], writes=[s_], signal=(bb == 1 and half == 1))

            emit_qk(0)
            sg = None
            if pi_ == 1 and u > 0:
                second_pass(u - 1)
            for gix_, (r, n) in enumerate(groups):
                n0 = (n // GB_) * GB_
                if n == n0:
                    sg = stg[gi % 2]; gi += 1
                s_ = st[gix_ % 2]; o_ = ops[gix_ % 2]; p_ = pt[gix_ % 3]
                k.op("act", lambda e: e.activation(out=p_[:], in_=s_[:], func=ACT.Exp), reads=[s_], writes=[p_])
                if gix_ + 1 < len(groups):
                    emit_qk(gix_ + 1)
                k.op("dve" if bi % 2 == 0 else "pool",
                     lambda e: e.tensor_tensor(out=p_[:].rearrange("p (b c) -> p b c", b=2), in0=p_[:].rearrange("p (b c) -> p b c", b=2),
                                               in1=mask[:].unsqueeze(1).broadcast_to([128, 2, 256]), op=ALU.mult),
                     reads=[p_, mask], writes=[p_])
                for bb in range(2):
                    for half in range(2):
                        c0 = bb * 256 + half * 128
                        k.op("pe", lambda e: e.matmul(o_[:, bb, :], lhsT=p_[:, c0:c0 + 128],
                                                      rhs=V[:, r * (nblk + 1) + n + bb + half, :], start=(half == 0), stop=(half == 1)),
                             reads=[p_, V], writes=[o_], signal=(bb == 1 and half == 1))
                if bi % 2 == 0:
                    k.op("dve", lambda e: e.tensor_copy(out=sg[:, n - n0:n - n0 + 2, :], in_=o_[:]), reads=[o_], writes=[sg])
                else:
                    k.op("act", lambda e: e.activation(out=sg[:, n - n0:n - n0 + 2, :], in_=o_[:], func=ACT.Copy), reads=[o_], writes=[sg])
                bi += 1
                n1 = min(nblk, n0 + GB_)
                if n + 2 >= n1:
                    seg = acc[pi_, 128 * n0 * d:128 * n1 * d, :].rearrange("(n p d) c -> p n d c", p=128, d=d)[:, :, r, :]
                    k.dma(seg, sg[:, 0:n1 - n0, :], reads=[sg], writes=[aR], q="act")
    second_pass(NU - 1)
    if NF:
        UX = k.sbuf("UX", [128, 16384], BF16)
        Z = k.sbuf("Z", [128, 128, 2 * N1], BF16)
        D64 = k.sbuf("D64", [N1, 2 * N1], BF16)
        CS = k.sbuf("CS", [128, 256], BF16)
        gf = k.sbuf("gf_s", [128, 128], F32)
        GAB = [k.sbuf("GAB%d" % i, [128, 2, 4, 256], BF16) for i in range(2)]
        pz = [k.psum("pz%d" % i, [128, 512], F32) for i in range(2)]
        ys = k.sbuf("ys", [128, N1, 128], BF16)
        sq4 = k.sbuf("sq4", [128, 4, 128], BF16)
        s4 = k.sbuf("s4f", [128, 4], F32)
        y4 = k.sbuf("y4", [128, 4, 128], F32)
        k.dma(D64[:], d64d, writes=[D64])
        k.dma(CS[:], csd, writes=[CS])
        zi = 0; gbi = 0
        for f in range(NF):
            k.dma(UX[0:N1, 0:16384], ufd[f], writes=[UX])
            k.dma(gf[:], gfd[f], writes=[gf])
            W2 = 2 * N1
            per = 512 // W2
            for c0 in range(0, 128, per):
                p_ = pz[zi % 2]; zi += 1
                for j in range(per):
                    c = c0 + j
                    k.op("pe", lambda e: e.matmul(p_[:, j * W2:(j + 1) * W2], lhsT=UX[0:N1, c:16384:128], rhs=D64[:], start=True, stop=True),
                         reads=[UX, D64], writes=[p_], signal=(j == per - 1))
                k.op("act" if zi % 2 else "dve",
                     (lambda e: e.activation(out=Z[:, c0:c0 + per, :].rearrange("p c w -> p (c w)"), in_=p_[:, 0:per * W2], func=ACT.Copy)) if zi % 2 else
                     (lambda e: e.tensor_copy(out=Z[:, c0:c0 + per, :].rearrange("p c w -> p (c w)"), in_=p_[:, 0:per * W2])),
                     reads=[p_], writes=[Z])
            XT = UX[:, 0:N1 * 256].rearrange("p (k w) -> p k w", w=256)
            for kg in range(0, N1, 4):
                G = GAB[gbi % 2]; gbi += 1
                kn = min(4, N1 - kg)
                k.dma(G[:, 0, 0:kn, :], gabd[0, :, kg:kg + kn, :], writes=[G])
                k.dma(G[:, 1, 0:kn, :], gabd[1, :, kg:kg + kn, :], writes=[G])
                for k1 in range(kg, kg + kn):
                    p_ = pz[zi % 2]; zi += 1
                    k.op("pe", lambda e: e.matmul(p_[:, 0:256], lhsT=Z[:, :, k1], rhs=G[:, 0, k1 - kg, :], start=True, stop=False),
                         reads=[Z, G], writes=[p_], signal=False)
                    k.op("pe", lambda e: e.matmul(p_[:, 0:256], lhsT=Z[:, :, N1 + k1], rhs=G[:, 1, k1 - kg, :], start=False, stop=True),
                         reads=[Z, G], writes=[p_])
                    if zi % 2:
                        k.op("act", lambda e: e.activation(out=XT[:, k1, :], in_=p_[:, 0:256], func=ACT.Copy), reads=[p_], writes=[UX])
                    else:
                        k.op("dve", lambda e: e.tensor_copy(out=XT[:, k1, :], in_=p_[:, 0:256]), reads=[p_], writes=[UX])
            for k0 in range(0, N1, 4):
                p_ = pz[zi % 2]; zi += 1
                for j in range(4):
                    k1 = k0 + j
                    k.op("pe", lambda e: e.matmul(p_[:, j * 128:(j + 1) * 128], lhsT=XT[:, k1, 0:128], rhs=CS[:, 0:128], start=True, stop=False),
                         reads=[UX, CS], writes=[p_], signal=False)
                    k.op("pe", lambda e: e.matmul(p_[:, j * 128:(j + 1) * 128], lhsT=XT[:, k1, 128:256], rhs=CS[:, 128:256], start=False, stop=True),
                         reads=[UX, CS], writes=[p_], signal=(j == 3))
                pv = p_[:].rearrange("p (j c) -> p j c", j=4)
                k.op("act", lambda e: e.activation(out=sq4[:], in_=pv, func=ACT.Square), reads=[p_], writes=[sq4])
                k.op("dve", lambda e: e.tensor_reduce(out=s4[:], in_=sq4[:], axis=AX.X, op=ALU.add), reads=[sq4], writes=[s4])
                k.op("dve", lambda e: e.tensor_scalar(out=s4[:], in0=s4[:], scalar1=1.0 / 128, scalar2=EPS, op0=ALU.mult, op1=ALU.add),
                     reads=[s4], writes=[s4])
                k.op("act", lambda e: e.activation(out=s4[:], in_=s4[:], func=ACT.Sqrt), reads=[s4], writes=[s4])
                k.op("dve", lambda e: e.reciprocal(out=s4[:], in_=s4[:]), reads=[s4], writes=[s4])
                k.op("dve", lambda e: e.tensor_tensor(out=y4[:], in0=pv, in1=s4[:].unsqueeze(2).broadcast_to([128, 4, 128]), op=ALU.mult),
                     reads=[p_, s4], writes=[y4])
                k.op("pool", lambda e: e.tensor_tensor(out=ys[:, k0:k0 + 4, :], in0=y4[:], in1=gf[:].unsqueeze(1).broadcast_to([128, 4, 128]), op=ALU.mult),
                     reads=[y4, gf], writes=[ys])
            k.dma(yfd[f].rearrange("(k2 k1) c -> k2 k1 c", k1=N1), ys[:], reads=[ys], writes=[yfR])
    k.finish("sp")
    k.close()
    return nc


def fourier_tables(S):
    N1 = S // 128
    n1 = np.arange(N1)
    th = 2 * np.pi * np.outer(n1, n1) / N1
    d64 = np.concatenate([np.cos(th), -np.sin(th)], axis=1)
    n2 = np.arange(128)[:, None, None]; k1 = np.arange(N1)[None, :, None]; k2 = np.arange(128)[None, None, :]
    th = 2 * np.pi * ((n2 * (k1 + N1 * k2)) % S) / S
    Gr, Gi = np.cos(th), -np.sin(th)
    ga = np.concatenate([Gr, Gi], axis=2); gb = np.concatenate([-Gi, Gr], axis=2)
    c = np.arange(128)
    ph = 2 * np.pi * np.outer(c, c) / 128
    cs = np.concatenate([np.cos(ph), np.sin(ph)], axis=1)
    return d64, np.stack([ga, gb]), cs


def build_pc(NT=2048, HALF=512, NCB=8):
    nc = bass.Bass("TRN2", target_bir_lowering=False)
    NTT = NT // 128
    yTd = nc.dram_tensor("yT", [4096, NT], BF16, kind="ExternalInput").ap()
    x = nc.dram_tensor("x", [NT, 4096], F32, kind="ExternalInput").ap()
    w = nc.dram_tensor("w", [4096, NCB * 512], F32, kind="ExternalInput").ap()
    g1d = nc.dram_tensor("g1", [128, 4096], F32, kind="ExternalInput").ap()
    cols = nc.dram_tensor("cols", [128, 96], F32, kind="ExternalInput").ap()
    wrd = nc.dram_tensor("wr", [128, 32 * 16], F32, kind="ExternalInput").ap()
    identd = nc.dram_tensor("ident", [128, 128], F32, kind="ExternalInput").ap()
    x1 = nc.dram_tensor("x1", [NT, 4096], F32, kind="ExternalOutput").ap()
    affd = nc.dram_tensor("aff", [NT, 16], F32, kind="ExternalOutput").ap()
    k = K(nc)
    x1R = k.dram("x1R", x1); affR = k.dram("affR", affd)
    colt = k.sbuf("colt", [128, 96], F32)
    Gc = k.sbuf("Gc", [128, 32], F32)
    g1 = k.sbuf("g1s", [128, 4096], F32)
    wr = k.sbuf("wrs", [128, 32, 16], F32)
    ident = k.sbuf("ident_s", [128, 128], F32)
    yT = [k.sbuf("yTs%d" % i, [128, 32, 128], BF16) for i in range(HALF // 128)]
    Wb = [k.sbuf("Wb%d" % i, [128, 32, 512], BF16) for i in range(2)]
    stage = [k.sbuf("stg%d" % i, [128, 4, 512], F32) for i in range(2)]
    xt = k.sbuf("xt", [128, 4096], F32)
    tmp = {"ssq": k.sbuf("ssq", [128, 1], F32), "rs": k.sbuf("rs", [128, 1], F32),
           "xs": k.sbuf("xs", [128, 4096], F32), "junk": None}
    h32 = k.sbuf("h32", [128, 32, 128], F32)
    xc = [k.sbuf("xc%d" % i, [128, 512], F32) for i in range(3)]
    tt_ = [k.sbuf("tt%d" % i, [128, 512], F32) for i in range(3)]
    pT = [k.psum("pT%d" % i, [128, 4, 128], F32) for i in range(2)]
    ps = [k.psum("ps%d" % i, [128, 512], F32) for i in range(3)]
    pr = k.psum("pr", [128, 16], F32)
    mx = k.sbuf("mx", [128, 1], F32); sm = k.sbuf("sm", [128, 1], F32)
    ex = k.sbuf("ex", [128, 16], F32); af = k.sbuf("af", [128, 16], F32)
    k.dma(colt[:], cols, writes=[colt]); k.dma(g1[:], g1d, writes=[g1])
    k.dma(wr[:].rearrange("p a b -> p (a b)"), wrd, writes=[wr]); k.dma(ident[:], identd, writes=[ident])
    k.op("dve", lambda e: e.tensor_scalar(out=Gc[:], in0=colt[:, 32:64], scalar1=1.0, scalar2=None, op0=ALU.add), reads=[colt], writes=[Gc])
    k.op("dve", lambda e: e.tensor_tensor(out=Gc[:], in0=Gc[:], in1=colt[:, 0:32], op=ALU.mult), reads=[Gc, colt], writes=[Gc])
    it = 0; pi = 0; oi = 0
    for half in range(NT // HALF):
        for tt in range(HALF // 128):
            k.dma(yT[tt][:], yTd[:, half * HALF + tt * 128:half * HALF + (tt + 1) * 128].rearrange("(kc p) t -> p kc t", p=128), writes=[yT[tt]])
        for cb in range(NCB):
            W = Wb[it % 2]
            if it == 0:
                load_w_block(k, w[:, cb * 512:(cb + 1) * 512], stage, W, it)
            it += 1
            nxt = it if it < (NT // HALF) * NCB else None
            nparts = HALF // 128
            for tt in range(HALF // 128):
                gt = half * (HALF // 128) + tt
                p = ps[pi % 3]; pi += 1
                xcb = xc[oi % 3]; tb = tt_[oi % 3]; oi += 1
                k.dma(xcb[:], x[gt * 128:(gt + 1) * 128, cb * 512:(cb + 1) * 512], writes=[xcb])
                for kc in range(32):
                    k.op("pe", lambda e: e.matmul(p[:], lhsT=yT[tt][:, kc, :], rhs=W[:, kc, :],
                                                  start=(kc == 0), stop=(kc == 31)), reads=[yT[tt], W], writes=[p], signal=(kc == 31))
                if nxt is not None:
                    ncb = nxt % NCB
                    for s_ in range(8):
                        if s_ * nparts // 8 == tt:
                            load_w_stage(k, w[:, ncb * 512:(ncb + 1) * 512], stage, Wb[nxt % 2], nxt, s_)
                k.op("dve", lambda e: e.tensor_tensor(out=tb[:], in0=p[:], in1=g1[:, cb * 512:(cb + 1) * 512], op=ALU.mult),
                     reads=[p, g1], writes=[tb])
                k.op("pool", lambda e: e.tensor_tensor(out=tb[:], in0=tb[:], in1=xcb[:], op=ALU.add), reads=[tb, xcb], writes=[tb])
                k.dma(x1[gt * 128:(gt + 1) * 128, cb * 512:(cb + 1) * 512], tb[:], reads=[tb], writes=[x1R], q="act")
    Sv = _ColView(colt, 64)
    for gt in range(NTT):
        k.dma(xt[:], x1[gt * 128:(gt + 1) * 128, :], reads=[x1R], writes=[xt])
        norm_transpose_affine(k, xt, ident, Gc, Sv, None, None, pT, tmp, dst32=h32)
        for kc in range(32):
            k.op("pe", lambda e: e.matmul(pr[:], lhsT=h32[:, kc, :], rhs=wr[:, kc, :], start=(kc == 0), stop=(kc == 31)),
                 reads=[h32, wr], writes=[pr], signal=(kc == 31))
        k.op("dve", lambda e: e.tensor_reduce(out=mx[:], in_=pr[:], axis=AX.X, op=ALU.max), reads=[pr], writes=[mx])
        k.op("dve", lambda e: e.tensor_scalar(out=mx[:], in0=mx[:], scalar1=-1.0, scalar2=None, op0=ALU.mult), reads=[mx], writes=[mx])
        k.op("act", lambda e: e.activation(out=ex[:], in_=pr[:], func=ACT.Exp, bias=mx[:, 0:1], accum_out=sm[:]),
             reads=[pr, mx], writes=[ex, sm])
        k.op("dve", lambda e: e.reciprocal(out=sm[:], in_=sm[:]), reads=[sm], writes=[sm])
        k.op("dve", lambda e: e.tensor_scalar(out=af[:], in0=ex[:], scalar1=sm[:, 0:1], scalar2=None, op0=ALU.mult),
             reads=[ex, sm], writes=[af])
        k.dma(affd[gt * 128:(gt + 1) * 128, :], af[:], reads=[af], writes=[affR])
    k.finish("sp"); k.close()
    return nc


CAP = 1024
ZROW = 16 * 1024

def build_pd1(NITER=34):
    nc = bass.Bass("TRN2", target_bir_lowering=False)
    affd = nc.dram_tensor("affu", [128, 4 * 64], F32, kind="ExternalInput").ap()
    eoffd = nc.dram_tensor("eoff", [128, 4], F32, kind="ExternalInput").ap()
    onesd = nc.dram_tensor("ones", [128, 128], F32, kind="ExternalInput").ap()
    lowd = nc.dram_tensor("lstrict", [128, 128], F32, kind="ExternalInput").ap()
    identd = nc.dram_tensor("ident", [128, 128], F32, kind="ExternalInput").ap()
    gidxd = nc.dram_tensor("gidx", [128, 4 * 64], I32, kind="ExternalOutput").ap()
    maskd = nc.dram_tensor("msk", [128, 4 * 64], F32, kind="ExternalOutput").ap()
    k = K(nc)
    aff = k.sbuf("aff", [128, 4, 64], F32); eoff = k.sbuf("eoff_s", [128, 4], F32)
    ones = k.sbuf("ones_s", [128, 128], F32); low = k.sbuf("low_s", [128, 128], F32); ident = k.sbuf("ident_s", [128, 128], F32)
    lo = k.sbuf("lo", [128, 4], F32); hi = k.sbuf("hi", [128, 4], F32); mid = k.sbuf("mid", [128, 4], F32)
    cmp_ = k.sbuf("cmp", [128, 4, 64], F32); cnt = k.sbuf("cnt", [128, 4], F32); ge = k.sbuf("ge", [128, 4], F32)
    d1 = k.sbuf("d1", [128, 4], F32)
    tot = k.psum("tot", [128, 4], F32)
    k.dma(aff[:].rearrange("p a b -> p (a b)"), affd, writes=[aff]); k.dma(eoff[:], eoffd, writes=[eoff])
    k.dma(ones[:], onesd, writes=[ones]); k.dma(low[:], lowd, writes=[low]); k.dma(ident[:], identd, writes=[ident])
    k.op("dve", lambda e: e.memset(lo[:], 0.0), writes=[lo])
    k.op("dve", lambda e: e.memset(hi[:], 1.0), writes=[hi])
    for it in range(NITER):
        k.op("dve", lambda e: e.tensor_tensor(out=mid[:], in0=lo[:], in1=hi[:], op=ALU.add), reads=[lo, hi], writes=[mid])
        k.op("dve", lambda e: e.tensor_scalar(out=mid[:], in0=mid[:], scalar1=0.5, scalar2=None, op0=ALU.mult), reads=[mid], writes=[mid])
        k.op("dve", lambda e: e.tensor_tensor(out=cmp_[:], in0=aff[:], in1=mid[:].unsqueeze(2).broadcast_to([128, 4, 64]), op=ALU.is_ge),
             reads=[aff, mid], writes=[cmp_])
        k.op("dve", lambda e: e.tensor_reduce(out=cnt[:], in_=cmp_[:], axis=AX.X, op=ALU.add), reads=[cmp_], writes=[cnt])
        k.op("pe", lambda e: e.matmul(tot[:], lhsT=ones[:], rhs=cnt[:], start=True, stop=True), reads=[ones, cnt], writes=[tot])
        k.op("dve", lambda e: e.tensor_scalar(out=ge[:], in0=tot[:], scalar1=float(CAP), scalar2=None, op0=ALU.is_ge), reads=[tot], writes=[ge])
        k.op("dve", lambda e: e.tensor_tensor(out=d1[:], in0=mid[:], in1=lo[:], op=ALU.subtract), reads=[mid, lo], writes=[d1])
        k.op("dve", lambda e: e.tensor_tensor(out=d1[:], in0=d1[:], in1=ge[:], op=ALU.mult), reads=[d1, ge], writes=[d1])
        k.op("dve", lambda e: e.tensor_tensor(out=lo[:], in0=lo[:], in1=d1[:], op=ALU.add), reads=[lo, d1], writes=[lo])
        k.op("dve", lambda e: e.tensor_tensor(out=d1[:], in0=hi[:], in1=mid[:], op=ALU.subtract), reads=[hi, mid], writes=[d1])
        k.op("dve", lambda e: e.tensor_tensor(out=d1[:], in0=d1[:], in1=ge[:], op=ALU.mult), reads=[d1, ge], writes=[d1])
        k.op("dve", lambda e: e.tensor_tensor(out=hi[:], in0=mid[:], in1=d1[:], op=ALU.add), reads=[mid, d1], writes=[hi])
    msk = k.sbuf("msk_s", [128, 4, 64], F32)
    k.op("dve", lambda e: e.tensor_tensor(out=msk[:], in0=aff[:], in1=lo[:].unsqueeze(2).broadcast_to([128, 4, 64]), op=ALU.is_ge),
         reads=[aff, lo], writes=[msk])
    k.op("dve", lambda e: e.tensor_reduce(out=cnt[:], in_=msk[:], axis=AX.X, op=ALU.add), reads=[msk], writes=[cnt])
    offp = k.psum("offp", [128, 4], F32)
    k.op("pe", lambda e: e.matmul(offp[:], lhsT=low[:], rhs=cnt[:], start=True, stop=True), reads=[low, cnt], writes=[offp])
    offs = k.sbuf("offs", [128, 4], F32)
    k.op("dve", lambda e: e.tensor_copy(out=offs[:], in_=offp[:]), reads=[offp], writes=[offs])
    mT = k.sbuf("mT", [64, 128], F32)
    tp = k.psum("tp", [64, 128], F32)
    wp = k.psum("wp", [128, 64], F32)
    pos = k.sbuf("pos", [128, 4, 64], F32)
    sel = k.sbuf("sel", [128, 4, 64], F32)
    gi = k.sbuf("gi", [128, 4, 64], I32)
    for u in range(4):
        k.op("pe", lambda e: e.transpose(out=tp[:], in_=msk[:, u, :], identity=ident[:]), reads=[msk, ident], writes=[tp])
        k.op("dve", lambda e: e.tensor_copy(out=mT[:], in_=tp[:]), reads=[tp], writes=[mT])
        k.op("pe", lambda e: e.matmul(wp[:], lhsT=mT[:], rhs=low[0:64, 0:64], start=True, stop=True), reads=[mT, low], writes=[wp])
        k.op("dve", lambda e: e.tensor_scalar(out=pos[:, u, :], in0=wp[:], scalar1=offs[:, u:u + 1], scalar2=None, op0=ALU.add),
             reads=[wp, offs], writes=[pos])
    k.op("dve", lambda e: e.tensor_scalar(out=sel[:], in0=pos[:], scalar1=float(CAP), scalar2=None, op0=ALU.is_lt), reads=[pos], writes=[sel])
    k.op("dve", lambda e: e.tensor_tensor(out=sel[:], in0=sel[:], in1=msk[:], op=ALU.mult), reads=[sel, msk], writes=[sel])
    k.op("dve", lambda e: e.tensor_tensor(out=pos[:], in0=pos[:], in1=eoff[:].unsqueeze(2).broadcast_to([128, 4, 64]), op=ALU.add),
         reads=[pos, eoff], writes=[pos])
    k.op("dve", lambda e: e.tensor_tensor(out=pos[:], in0=pos[:], in1=sel[:], op=ALU.mult), reads=[pos, sel], writes=[pos])
    k.op("dve", lambda e: e.tensor_scalar(out=pos[:], in0=pos[:], scalar1=float(ZROW), scalar2=None, op0=ALU.add), reads=[pos], writes=[pos])
    k.op("dve", lambda e: e.tensor_copy(out=gi[:], in_=pos[:]), reads=[pos], writes=[gi])
    gR = k.dram("gR", gidxd); mR = k.dram("mR", maskd)
    k.dma(gidxd, gi[:].rearrange("p a b -> p (a b)"), reads=[gi], writes=[gR])
    k.dma(maskd, sel[:].rearrange("p a b -> p (a b)"), reads=[sel], writes=[mR])
    k.finish("sp"); k.close()
    return nc

def d1_consts():
    ii = np.arange(128)
    return {"ones": np.ones((128, 128), np.float32), "lstrict": (ii[:, None] < ii[None, :]).astype(np.float32),
            "ident": np.eye(128, dtype=np.float32)}


def build_pd2(NUNIT=4, NSL=1024):
    nc = bass.Bass("TRN2", target_bir_lowering=False)
    NST = NSL // 128
    xed = nc.dram_tensor("xe", [NUNIT, NSL, 4096], F32, kind="ExternalInput").ap()
    afd = nc.dram_tensor("affs", [NUNIT, 128, NST], F32, kind="ExternalInput").ap()
    cold = nc.dram_tensor("cols", [NUNIT, 128, 96], F32, kind="ExternalInput").ap()
    NE = (NUNIT + 1) // 2
    wgd = nc.dram_tensor("wg", [NE, 4096, 1024], F32, kind="ExternalInput").ap()
    wud = nc.dram_tensor("wu", [NE, 4096, 1024], F32, kind="ExternalInput").ap()
    wdd = nc.dram_tensor("wd", [NE, 1024, 4096], F32, kind="ExternalInput").ap()
    identd = nc.dram_tensor("ident", [128, 128], F32, kind="ExternalInput").ap()
    yed = nc.dram_tensor("ye", [NUNIT, NSL, 4096], F32, kind="ExternalOutput").ap()
    k = K(nc)
    yR = k.dram("yR", yed)
    ident = k.sbuf("ident_s", [128, 128], F32)
    k.dma(ident[:], identd, writes=[ident])
    colt = k.sbuf("colt", [128, 96], F32); Gc = k.sbuf("Gc", [128, 32], F32); afs = k.sbuf("afs", [128, NST], F32)
    xeT = k.sbuf("xeT", [128, 32, NSL], BF16)
    xt = k.sbuf("xt", [128, 4096], F32)
    tmp = {"ssq": k.sbuf("ssq", [128, 1], F32), "rs": k.sbuf("rs", [128, 1], F32), "xs": k.sbuf("xs", [128, 4096], F32), "junk": None}
    pT = [k.psum("pT%d" % i, [128, 4, 128], F32) for i in range(2)]
    stg = [k.sbuf("stg%d" % i, [128, 4096], F32) for i in range(2)]
    wb = [k.sbuf("wb%d" % i, [128, 4096], BF16) for i in range(4)]
    h1T = k.sbuf("h1T", [128, 8, NSL], BF16)
    sg = [k.sbuf("sg%d" % i, [128, 512], F32) for i in range(2)]
    pg = [k.psum("pg%d" % i, [128, 512], F32) for i in range(2)]
    pu = [k.psum("pu%d" % i, [128, 512], F32) for i in range(2)]
    py = [k.psum("py%d" % i, [128, 512], F32) for i in range(2)]
    ot = [k.sbuf("ot%d" % i, [128, 512], F32) for i in range(3)]
    si = 0; wi = 0; gi = 0; yi = 0; oi = 0
    NSH = max(1, NSL // 512); SHW = min(512, NSL)
    for u in range(NUNIT):
        e_ = u // 2
        k.dma(colt[:], cold[u], writes=[colt]); k.dma(afs[:], afd[u], writes=[afs])
        k.op("dve", lambda e: e.tensor_scalar(out=Gc[:], in0=colt[:, 32:64], scalar1=1.0, scalar2=None, op0=ALU.add), reads=[colt], writes=[Gc])
        k.op("dve", lambda e: e.tensor_tensor(out=Gc[:], in0=Gc[:], in1=colt[:, 0:32], op=ALU.mult), reads=[Gc, colt], writes=[Gc])
        Sv = _ColView(colt, 64)
        for st in range(NST):
            k.dma(xt[:], xed[u, st * 128:(st + 1) * 128, :], writes=[xt])
            norm_transpose_affine(k, xt, ident, Gc, Sv, xeT, slice(st * 128, (st + 1) * 128), pT, tmp)
        def load_gu(fc):
            nonlocal si, wi
            wbs = []
            for wsrc in (wgd, wud):
                s_ = stg[si % 2]; si += 1
                b_ = wb[wi % 4]; wi += 1
                sv = s_[:].rearrange("p (kc n) -> p kc n", n=128)
                src = wsrc[e_, :, fc * 128:(fc + 1) * 128].rearrange("(kc p) n -> p kc n", p=128)
                k.dma(sv[:, 0:16, :], src[:, 0:16, :], writes=[s_])
                k.dma(sv[:, 16:32, :], src[:, 16:32, :], writes=[s_])
                k.op("pool", lambda e: e.tensor_copy(out=b_[:, 0:1536], in_=s_[:, 0:1536]), reads=[s_], writes=[b_])
                k.op("dve", lambda e: e.tensor_copy(out=b_[:, 1536:4096], in_=s_[:, 1536:4096]), reads=[s_], writes=[b_])
                wbs.append(b_)
            return wbs

        def load_d(db):
            nonlocal si, wi
            s_ = stg[si % 2]; si += 1
            b_ = wb[wi % 4]; wi += 1
            sv = s_[:].rearrange("p (fc n) -> p fc n", n=512)
            src = wdd[e_, :, db * 512:(db + 1) * 512].rearrange("(fc p) n -> p fc n", p=128)
            k.dma(sv[:, 0:4, :], src[:, 0:4, :], writes=[s_])
            k.dma(sv[:, 4:8, :], src[:, 4:8, :], writes=[s_])
            k.op("pool", lambda e: e.tensor_copy(out=b_[:, 0:1536], in_=s_[:, 0:1536]), reads=[s_], writes=[b_])
            k.op("dve", lambda e: e.tensor_copy(out=b_[:, 1536:4096], in_=s_[:, 1536:4096]), reads=[s_], writes=[b_])
            return b_

        pend = load_gu(0)
        for fc in range(8):
            wbs = pend
            if fc + 1 < 8:
                pend = load_gu(fc + 1)
            else:
                pend_d = load_d(0)
            for sh in range(NSH):
                g_ = pg[gi % 2]; u_ = pu[gi % 2]; s2 = sg[gi % 2]; gi += 1
                for (pp, b_) in ((g_, wbs[0]), (u_, wbs[1])):
                    bv = b_[:].rearrange("p (kc n) -> p kc n", n=128)
                    for kc in range(32):
                        k.op("pe", lambda e: e.matmul(pp[:, 0:SHW], lhsT=bv[:, kc, :], rhs=xeT[:, kc, sh * SHW:(sh + 1) * SHW],
                                                      start=(kc == 0), stop=(kc == 31)), reads=[b_, xeT], writes=[pp], signal=(kc == 31))
                k.op("act", lambda e: e.activation(out=s2[:, 0:SHW], in_=g_[:, 0:SHW], func=ACT.Silu), reads=[g_], writes=[s2])
                k.op("dve", lambda e: e.tensor_tensor(out=h1T[:, fc, sh * SHW:(sh + 1) * SHW], in0=s2[:, 0:SHW], in1=u_[:, 0:SHW], op=ALU.mult),
                     reads=[s2, u_], writes=[h1T])
        for db in range(8):
            b_ = pend_d
            if db + 1 < 8:
                pend_d = load_d(db + 1)
            bv = b_[:].rearrange("p (fc n) -> p fc n", n=512)
            for st in range(NST):
                y_ = py[yi % 2]; yi += 1
                for fc in range(8):
                    k.op("pe", lambda e: e.matmul(y_[:], lhsT=h1T[:, fc, st * 128:(st + 1) * 128], rhs=bv[:, fc, :],
                                                  start=(fc == 0), stop=(fc == 7)), reads=[h1T, b_], writes=[y_], signal=(fc == 7))
                o_ = ot[oi % 3]; oi += 1
                k.op("act", lambda e: e.activation(out=o_[:], in_=y_[:], func=ACT.Copy, scale=afs[:, st:st + 1]), reads=[y_, afs], writes=[o_])
                k.dma(yed[u, st * 128:(st + 1) * 128, db * 512:(db + 1) * 512], o_[:], reads=[o_], writes=[yR], q="act")
    k.finish("sp"); k.close()
    return nc


ZROW = 16 * 1024

def build_pe(NTOK=4096, CW=2048):
    nc = bass.Bass("TRN2", target_bir_lowering=False)
    NTT = NTOK // 128
    x1d = nc.dram_tensor("x1c", [NTOK, CW], F32, kind="ExternalInput").ap()
    yed = nc.dram_tensor("yec", [ZROW + 1, CW], F32, kind="ExternalInput").ap()
    gid = nc.dram_tensor("gidx", [128, NTT * 16], I32, kind="ExternalInput").ap()
    g2d = nc.dram_tensor("g2", [128, CW], F32, kind="ExternalInput").ap()
    x2d = nc.dram_tensor("x2c", [NTOK, CW], F32, kind="ExternalOutput").ap()
    k = K(nc)
    xR = k.dram("xR", x2d)
    gix = k.sbuf("gix", [128, NTT * 16], I32)
    g2 = k.sbuf("g2s", [128, CW], F32)
    k.dma(gix[:], gid, writes=[gix]); k.dma(g2[:], g2d, writes=[g2])
    NG = 6
    breg = nc.gpsimd.to_reg(ZROW - 1)
    zt = k.sbuf("zt", [128, CW], F32)
    k.op("dve", lambda e: e.memset(zt[:], 0.0), writes=[zt])
    G = [k.sbuf("G%d" % i, [128, CW], F32) for i in range(NG)]
    xt = [k.sbuf("xt%d" % i, [128, CW], F32) for i in range(2)]
    acc = [k.sbuf("acc%d" % i, [128, CW], F32) for i in range(2)]
    gi = 0
    for tt in range(NTT):
        x_ = xt[tt % 2]; a_ = acc[tt % 2]
        k.dma(x_[:], x1d[tt * 128:(tt + 1) * 128, :], writes=[x_])
        for e_ in range(16):
            g_ = G[gi % NG]; gi += 1
            col = tt * 16 + e_
            en = k.engs["pool"]
            k._deps(en, [gix], [g_])
            k.op("act", lambda e: e.activation(out=g_[:], in_=zt[:], func=ACT.Copy), reads=[zt], writes=[g_])
            k._deps(en, [gix], [g_])
            ins = nc.gpsimd.indirect_dma_start(out=g_[:], out_offset=None, in_=yed,
                                               in_offset=bass.IndirectOffsetOnAxis(ap=gix[:, col:col + 1].bitcast(U32), axis=0),
                                               bounds_check=breg, oob_is_err=False)
            ins.then_inc(g_.sem(), 16)
            g_.dcount += 1
            dep = ("dma", g_, g_.dcount)
            gix.readers.append(dep); g_.writer = dep; g_.readers = []
            if e_ == 0:
                k.op("dve", lambda e: e.tensor_copy(out=a_[:], in_=g_[:]), reads=[g_], writes=[a_])
            else:
                k.op("dve", lambda e: e.tensor_tensor(out=a_[:], in0=a_[:], in1=g_[:], op=ALU.add), reads=[a_, g_], writes=[a_])
        k.op("dve", lambda e: e.tensor_tensor(out=a_[:], in0=a_[:], in1=g2[:], op=ALU.mult), reads=[a_, g2], writes=[a_])
        k.op("pool", lambda e: e.tensor_tensor(out=a_[:], in0=a_[:], in1=x_[:], op=ALU.add), reads=[a_, x_], writes=[a_])
        k.dma(x2d[tt * 128:(tt + 1) * 128, :], a_[:], reads=[a_], writes=[xR])
    k.finish("sp"); k.close()
    return nc


BF = ml_dtypes.bfloat16
_PROGS = {}


def _prog(name, fn):
    if name not in _PROGS:
        _PROGS[name] = fn()
    return _PROGS[name]


def _col(v):
    return np.ascontiguousarray(np.asarray(v, np.float32).reshape(32, 128).T)


def _rep(v):
    v = np.asarray(v, np.float32)
    return np.ascontiguousarray(np.broadcast_to(v[None, :], (128, v.shape[0])))


def kernel(x, c, positions, norm1_gain, norm2_gain, w_ada, b_ada, w_in, q_gain, k_gain,
           out_gain_fourier, out_gain_attn, w_out, w_router, w_gate, w_up, w_down):
    f32 = np.float32
    x = np.asarray(x, f32); c = np.asarray(c, f32); positions = np.asarray(positions, np.int32)
    norm1_gain = np.asarray(norm1_gain, f32); norm2_gain = np.asarray(norm2_gain, f32)
    w_ada = np.asarray(w_ada, f32); b_ada = np.asarray(b_ada, f32); w_in = np.asarray(w_in, f32)
    q_gain = np.asarray(q_gain, f32); k_gain = np.asarray(k_gain, f32)
    out_gain_fourier = np.asarray(out_gain_fourier, f32); out_gain_attn = np.asarray(out_gain_attn, f32)
    w_out = np.asarray(w_out, f32); w_router = np.asarray(w_router, f32)
    w_gate = np.asarray(w_gate, f32); w_up = np.asarray(w_up, f32); w_down = np.asarray(w_down, f32)
    B, S, D = x.shape
    NC = 8
    ident = np.eye(128, dtype=f32)

    cT = np.ascontiguousarray(c.T.reshape(32, 128, 2).transpose(1, 0, 2).reshape(128, 64))
    ims = []
    for cc in range(NC):
        sl = slice(cc * 3072, (cc + 1) * 3072)
        ims.append({"cT": cT, "wa": np.ascontiguousarray(w_ada[:, :, sl]),
                    "ba": np.ascontiguousarray(b_ada[:, sl]).reshape(1, -1)})
    res = run(_prog("p0", build_p0), ims)
    mod = np.zeros((2, 2, 6 * D), f32)
    for cc in range(NC):
        o = res.results[cc]["mod"].reshape(2, 2, 3072)
        mod[:, :, cc * 3072:(cc + 1) * 3072] = o.transpose(1, 0, 2)
    del ims, res

    ii = np.arange(128)
    maskc = np.concatenate([(ii[:, None] >= ii[None, :]), (ii[:, None] <= ii[None, :])], axis=1).astype(BF)
    d64, gab, cs = fourier_tables(S)
    d64 = d64.astype(BF); gab = gab.astype(BF); cs = cs.astype(BF)
    d1c = d1_consts()
    TPC = (B * S) // NC
    CPB = NC // B

    for l in range(2):
        shift1, scale1, gate1, shift2, scale2, gate2 = [mod[l][:, i * D:(i + 1) * D] for i in range(6)]
        ims = []
        for cc in range(NC):
            b = cc // CPB; t0 = (cc % CPB) * TPC
            ims.append(host_inputs_pa(np.ascontiguousarray(x[b, t0:t0 + TPC]), norm1_gain[l], scale1[b], shift1[b], w_in[l],
                                      q_gain[l], k_gain[l], positions[b, t0:t0 + TPC]))
        res = run(_prog("pa", build_pa), ims)
        U = np.stack([res.results[cc]["u"] for cc in range(NC)], 0).reshape(B, S, 10240)
        del ims, res
        ims = []
        for cc in range(NC):
            qT = np.zeros((6, 128, S), BF); kT = np.zeros((6, 128, S + 2048), BF); vp = np.zeros((6, S + 2112, 129), BF)
            ga = np.zeros((6, 128, 128), f32)
            for b in range(B):
                for hh in range(3):
                    h = 3 * cc + hh; u = b * 3 + hh
                    qT[u] = U[b, :, 1024 + h * 128:1024 + (h + 1) * 128].T
                    kT[u, :, 1024:1024 + S] = U[b, :, 4096 + h * 128:4096 + (h + 1) * 128].T
                    vp[u, 1024:1024 + S, :128] = U[b, :, 7168 + h * 128:7168 + (h + 1) * 128]
                    vp[u, 1024:1024 + S, 128] = 1.0
                    ga[u] = _rep(out_gain_attn[l][h * 128:(h + 1) * 128])
            uf = np.stack([np.ascontiguousarray(U[b, :, cc * 128:(cc + 1) * 128]).reshape(S // 128, 128 * 128) for b in range(B)], 0)
            gf = np.stack([_rep(out_gain_fourier[l][cc * 128:(cc + 1) * 128])] * B, 0)
            ims.append({"qT": qT, "kT": kT, "vp": vp, "ga": ga, "mask": maskc, "uf": uf, "d64": d64, "gab": gab, "cs": cs, "gf": gf})
        res = run(_prog("pb", build_pb), ims)
        y = np.zeros((B, S, D), BF)
        for cc in range(NC):
            ya = res.results[cc]["ya"]; yf = res.results[cc]["yf"]
            for b in range(B):
                y[b, :, cc * 128:(cc + 1) * 128] = yf[b]
                for hh in range(3):
                    h = 3 * cc + hh
                    y[b, :, 1024 + h * 128:1024 + (h + 1) * 128] = ya[b * 3 + hh]
        del ims, res, U
        wr = np.ascontiguousarray(w_router[l].reshape(32, 128, 16).transpose(1, 0, 2).reshape(128, 512))
        ims = []
        for cc in range(NC):
            b = cc // CPB; t0 = (cc % CPB) * TPC
            ims.append({"yT": np.ascontiguousarray(y[b, t0:t0 + TPC].T), "x": np.ascontiguousarray(x[b, t0:t0 + TPC]), "w": w_out[l],
                        "g1": _rep(gate1[b]), "cols": np.concatenate([_col(norm2_gain[l]), _col(scale2[b]), _col(shift2[b])], 1),
                        "wr": wr, "ident": ident})
        res = run(_prog("pc", build_pc), ims)
        x1 = np.stack([res.results[cc]["x1"] for cc in range(NC)], 0).reshape(B, S, D)
        aff = np.stack([res.results[cc]["aff"] for cc in range(NC)], 0).reshape(B, S, 16)
        del ims, res, y
        ims = []
        for cc in range(NC):
            affu = np.zeros((128, 4, 64), f32); eo = np.zeros(4, f32)
            for u in range(4):
                e = 2 * cc + u // 2; b = u % 2
                affu[:, u, :] = aff[b, :, e].reshape(128, 64)
                eo[u] = e * 1024 - ZROW
            ims.append({"affu": affu.reshape(128, 256), "eoff": _rep(eo), **d1c})
        res = run(_prog("pd1", build_pd1), ims)
        gfull = np.full((B, S, 16), ZROW, np.int32)
        idxs = {}
        for cc in range(NC):
            g = res.results[cc]["gidx"].reshape(128, 4, 64); m = res.results[cc]["msk"].reshape(128, 4, 64)
            for u in range(4):
                e = 2 * cc + u // 2; b = u % 2
                gfull[b, :, e] = g[:, u, :].reshape(S)
                sel = np.flatnonzero(m[:, u, :].reshape(S) > 0.5)[:1024]
                if sel.shape[0] < 1024:
                    sel = np.concatenate([sel, np.zeros(1024 - sel.shape[0], sel.dtype)])
                idxs[(b, e)] = sel
        del ims, res
        ims = []
        for cc in range(NC):
            xe = np.zeros((4, 1024, D), f32); affs = np.zeros((4, 128, 8), f32); cols = np.zeros((4, 128, 96), f32)
            for u in range(4):
                e = 2 * cc + u // 2; b = u % 2
                sel = idxs[(b, e)]
                xe[u] = x1[b, sel]
                affs[u] = aff[b, sel, e].reshape(8, 128).T
                cols[u] = np.concatenate([_col(norm2_gain[l]), _col(scale2[b]), _col(shift2[b])], 1)
            ims.append({"xe": xe, "affs": affs, "cols": cols, "wg": np.ascontiguousarray(w_gate[l, 2 * cc:2 * cc + 2]),
                        "wu": np.ascontiguousarray(w_up[l, 2 * cc:2 * cc + 2]), "wd": np.ascontiguousarray(w_down[l, 2 * cc:2 * cc + 2]),
                        "ident": ident})
        res = run(_prog("pd2", build_pd2), ims)
        yeall = np.zeros((B, ZROW + 1, D), f32)
        for cc in range(NC):
            ye = res.results[cc]["ye"]
            for u in range(4):
                e = 2 * cc + u // 2; b = u % 2
                yeall[b, e * 1024:(e + 1) * 1024] = ye[u]
        del ims, res
        ims = []
        for cc in range(NC):
            b = cc // CPB; th = (cc % CPB) // 2; ch = cc % 2
            ts_ = slice(th * 4096, (th + 1) * 4096); cs_ = slice(ch * 2048, (ch + 1) * 2048)
            gi = np.ascontiguousarray(gfull[b][ts_].reshape(4096 // 128, 128, 16).transpose(1, 0, 2).reshape(128, -1))
            ims.append({"x1c": np.ascontiguousarray(x1[b][ts_, cs_]), "yec": np.ascontiguousarray(yeall[b][:, cs_]), "gidx": gi,
                        "g2": _rep(gate2[b][cs_])})
        res = run(_prog("pe", build_pe), ims)
        xn = np.zeros((B, S, D), f32)
        for cc in range(NC):
            b = cc // CPB; th = (cc % CPB) // 2; ch = cc % 2
            xn[b][th * 4096:(th + 1) * 4096, ch * 2048:(ch + 1) * 2048] = res.results[cc]["x2c"]
        del ims, res, x1, yeall
        x = xn
    return x
```

```python
import math
import numpy as np
import ml_dtypes
import concourse.bass as bass
import concourse.mybir as mybir
from concourse.bass_utils import run_bass_kernel_spmd

F32 = mybir.dt.float32
BF16 = mybir.dt.bfloat16
I32 = mybir.dt.int32
U32 = mybir.dt.uint32
ALU = mybir.AluOpType
ACT = mybir.ActivationFunctionType
AX = mybir.AxisListType


class Res:
    def __init__(self, k, name, t=None, own_sem=True):
        self.k = k
        self.name = name
        self.t = t
        self.writer = None
        self.readers = []
        self.dsem = None
        self.dcount = 0

    def __getitem__(self, idx):
        return self.t[idx]

    def sem(self):
        if self.dsem is None:
            self.dsem = self.k.new_sem("d_" + self.name)
        return self.dsem


class Eng:
    def __init__(self, k, name, h):
        self.k = k
        self.name = name
        self.h = h
        self.sem = k.new_sem("e_" + name)
        self.count = 0
        self.seen = {}
        self.seen_d = {}


class K:
    def __init__(self, nc):
        self.nc = nc
        self._ctx = []
        self.nsem = 0
        self.engs = {}
        for name, h in (("pe", nc.tensor), ("act", nc.scalar), ("dve", nc.vector),
                        ("pool", nc.gpsimd), ("sp", nc.sync)):
            self.engs[name] = Eng(self, name, h)
        self.all_res = []

    def new_sem(self, name):
        cm = self.nc.semaphore(name + "_%d" % self.nsem)
        self.nsem += 1
        s = cm.__enter__()
        self._ctx.append(cm)
        return s

    def sbuf(self, name, shape, dtype):
        cm = self.nc.sbuf_tensor(name, list(shape), dtype)
        t = cm.__enter__()
        self._ctx.append(cm)
        r = Res(self, name, t)
        self.all_res.append(r)
        return r

    def psum(self, name, shape, dtype):
        cm = self.nc.psum_tensor(name, list(shape), dtype)
        t = cm.__enter__()
        self._ctx.append(cm)
        r = Res(self, name, t)
        self.all_res.append(r)
        return r

    def dram(self, name, ap):
        r = Res(self, name, ap)
        self.all_res.append(r)
        return r

    def close(self):
        for cm in reversed(self._ctx):
            cm.__exit__(None, None, None)
        self._ctx = []

    def _wait(self, e, dep, raw):
        if dep is None:
            return
        if dep[0] == "eng":
            _, fname, seq = dep
            if fname == e.name:
                if not raw or e.name in ("pe", "sp"):
                    return
            if e.seen.get(fname, 0) >= seq:
                return
            e.h.wait_ge(self.engs[fname].sem, seq)
            e.seen[fname] = seq
        else:
            _, res, cnt = dep
            if e.seen_d.get(id(res), 0) >= cnt:
                return
            e.h.wait_ge(res.sem(), 16 * cnt)
            e.seen_d[id(res)] = cnt

    def _deps(self, e, reads, writes):
        for r in reads:
            self._wait(e, r.writer, True)
        for w in writes:
            self._wait(e, w.writer, False)
            for d in w.readers:
                self._wait(e, d, False)

    def op(self, ename, fn, reads=(), writes=(), signal=True):
        e = self.engs[ename]
        self._deps(e, reads, writes)
        ins = fn(e.h)
        seq = e.count + 1
        if signal:
            ins.then_inc(e.sem, 1)
            e.count = seq
        dep = ("eng", ename, seq)
        for r in reads:
            r.readers.append(dep)
        for w in writes:
            w.writer = dep
            w.readers = []
        return ins

    def dma(self, out_ap, in_ap, reads=(), writes=(), semres=None, q="sp", **kw):
        e = self.engs[q]
        self._deps(e, reads, writes)
        sr = semres if semres is not None else (list(writes) + list(reads))[0]
        ins = e.h.dma_start(out=out_ap, in_=in_ap, **kw)
        ins.then_inc(sr.sem(), 16)
        sr.dcount += 1
        dep = ("dma", sr, sr.dcount)
        for r in reads:
            r.readers.append(dep)
        for w in writes:
            w.writer = dep
            w.readers = []
        return ins

    def finish(self, ename="sp"):
        e = self.engs[ename]
        for r in self.all_res:
            if r.dcount:
                self._wait(e, ("dma", r, r.dcount), True)
        for n, f in self.engs.items():
            if n != ename and f.count:
                self._wait(e, ("eng", n, f.count), True)

    def barrier(self):
        for n in self.engs:
            self.finish(n)


def run(nc, in_maps, n=8, trace=False):
    return run_bass_kernel_spmd(nc, in_maps, core_ids=list(range(n)), trace=trace)


def build_p0():
    nc = bass.Bass("TRN2", target_bir_lowering=False)
    NCOL = 3072
    cT = nc.dram_tensor("cT", [128, 64], F32, kind="ExternalInput").ap()
    wa = nc.dram_tensor("wa", [2, 4096, NCOL], F32, kind="ExternalInput").ap()
    ba = nc.dram_tensor("ba", [1, 2 * NCOL], F32, kind="ExternalInput").ap()
    mod = nc.dram_tensor("mod", [2, 2 * NCOL], F32, kind="ExternalOutput").ap()
    k = K(nc)
    ct = k.sbuf("ct", [128, 64], F32)
    sc = k.sbuf("sc", [128, 64], F32)
    ones = k.sbuf("ones", [1, 2], F32)
    bt = k.sbuf("bt", [1, 2 * NCOL], F32)
    ob = k.sbuf("ob", [2, 2 * NCOL], F32)
    wt = [k.sbuf("wt%d" % i, [128, 32, 512], F32) for i in range(2)]
    ps = [k.psum("ps%d" % i, [2, 512], F32) for i in range(2)]
    k.dma(ct[:], cT, writes=[ct])
    k.dma(bt[:], ba, writes=[bt])
    k.op("act", lambda e: e.activation(out=sc[:], in_=ct[:], func=ACT.Silu), reads=[ct], writes=[sc])
    k.op("dve", lambda e: e.memset(ones[:], 1.0), writes=[ones])
    it = 0
    for l in range(2):
        for nb in range(NCOL // 512):
            w = wt[it % 2]; p = ps[it % 2]
            src = wa[l, :, nb * 512:(nb + 1) * 512].rearrange("(kc p) n -> p kc n", p=128)
            k.dma(w[:, 0:16, :], src[:, 0:16, :], writes=[w])
            k.dma(w[:, 16:32, :], src[:, 16:32, :], writes=[w])
            for kc in range(32):
                k.op("pe", lambda e: e.matmul(p[:], lhsT=sc[:, 2 * kc:2 * kc + 2], rhs=w[:, kc, :],
                                              start=(kc == 0), stop=False),
                     reads=[sc, w], writes=[p], signal=False)
            col = l * NCOL + nb * 512
            k.op("pe", lambda e: e.matmul(p[:], lhsT=ones[:], rhs=bt[:, col:col + 512], start=False, stop=True),
                 reads=[ones, bt], writes=[p])
            k.op("dve", lambda e: e.tensor_copy(out=ob[:, col:col + 512], in_=p[:]), reads=[p], writes=[ob])
            it += 1
    k.dma(mod, ob[:], reads=[ob])
    k.finish("sp")
    k.close()
    return nc


EPS = 1e-6

def nta_stage1(k, xt, tmp):
    ssq, rs, xs = tmp["ssq"], tmp["rs"], tmp["xs"]
    k.op("act", lambda e: e.activation(out=xs[:].bitcast(BF16)[:, 0:4096], in_=xt[:], func=ACT.Square, accum_out=ssq[:]),
         reads=[xt], writes=[xs, ssq])
    k.op("dve", lambda e: e.tensor_scalar(out=rs[:], in0=ssq[:], scalar1=1.0 / 4096, scalar2=EPS,
                                          op0=ALU.mult, op1=ALU.add), reads=[ssq], writes=[rs])
    k.op("act", lambda e: e.activation(out=rs[:], in_=rs[:], func=ACT.Sqrt), reads=[rs], writes=[rs])
    k.op("dve", lambda e: e.reciprocal(out=rs[:], in_=rs[:]), reads=[rs], writes=[rs])
    k.op("dve", lambda e: e.tensor_scalar(out=xs[:], in0=xt[:], scalar1=rs[:, 0:1], scalar2=None, op0=ALU.mult),
         reads=[xt, rs], writes=[xs])


def nta_stage2(k, ident, Gc, Sc, dst, dst_cols, pT, tmp, dst32=None):
    xs = tmp["xs"]
    for g in range(8):
        p = pT[g % 2]
        for j in range(4):
            kc = 4 * g + j
            k.op("pe", lambda e: e.transpose(out=p[:, j, :], in_=xs[:, kc * 128:(kc + 1) * 128], identity=ident[:]),
                 reads=[xs, ident], writes=[p], signal=(j == 3))
        for j in range(4):
            kc = 4 * g + j
            eng = "dve" if (j % 2 == 0) else "pool"
            if dst is None:
                pass
            elif eng == "pool":
                k.op("act", lambda e: e.activation(out=dst[:, kc, dst_cols], in_=p[:, j, :], func=ACT.Identity,
                                                   scale=Gc[:, kc:kc + 1], bias=Sc[:, kc:kc + 1]),
                     reads=[p, Gc, Sc], writes=[dst])
            else:
                k.op("dve", lambda e: e.tensor_scalar(out=dst[:, kc, dst_cols], in0=p[:, j, :],
                                                      scalar1=Gc[:, kc:kc + 1], scalar2=Sc[:, kc:kc + 1],
                                                      op0=ALU.mult, op1=ALU.add),
                     reads=[p, Gc, Sc], writes=[dst])
            if dst32 is not None:
                k.op("act", lambda e: e.activation(out=dst32[:, kc, :], in_=p[:, j, :], func=ACT.Identity,
                                                   scale=Gc[:, kc:kc + 1], bias=Sc[:, kc:kc + 1]),
                     reads=[p, Gc, Sc], writes=[dst32])


def norm_transpose_affine(k, xt, ident, Gc, Sc, dst, dst_cols, pT, tmp, dst32=None):
    nta_stage1(k, xt, tmp)
    nta_stage2(k, ident, Gc, Sc, dst, dst_cols, pT, tmp, dst32=dst32)


def load_w_stage(k, wsrc, stage, Wb, it, s):
    src = wsrc.rearrange("(kc p) n -> p kc n", p=128)
    st = stage[(it * 8 + s) % len(stage)]
    k.dma(st[:], src[:, 4 * s:4 * s + 4, :], writes=[st])
    eng = ("dve", "pool", "act", "pool")[s % 4]
    if eng == "act":
        k.op("act", lambda e: e.activation(out=Wb[:, 4 * s:4 * s + 4, :], in_=st[:], func=ACT.Copy),
             reads=[st], writes=[Wb])
    else:
        k.op(eng, lambda e: e.tensor_copy(out=Wb[:, 4 * s:4 * s + 4, :], in_=st[:]), reads=[st], writes=[Wb])


def load_w_block(k, wsrc, stage, Wb, it):
    for s in range(8):
        load_w_stage(k, wsrc, stage, Wb, it, s)


def build_pa(NT=2048, HALF=1024, NCB=20):
    nc = bass.Bass("TRN2", target_bir_lowering=False)
    NTT = NT // 128
    x = nc.dram_tensor("x", [NT, 4096], F32, kind="ExternalInput").ap()
    cols = nc.dram_tensor("cols", [128, 96], F32, kind="ExternalInput").ap()
    w = nc.dram_tensor("w", [4096, NCB * 512], F32, kind="ExternalInput").ap()
    qkg = nc.dram_tensor("qkg", [128, 256], F32, kind="ExternalInput").ap()
    pos = nc.dram_tensor("pos", [128, NTT], I32, kind="ExternalInput").ap()
    invf = nc.dram_tensor("invf", [128, 16], F32, kind="ExternalInput").ap()
    identd = nc.dram_tensor("ident", [128, 128], F32, kind="ExternalInput").ap()
    u = nc.dram_tensor("u", [NT, NCB * 512], BF16, kind="ExternalOutput").ap()
    k = K(nc)
    colt = k.sbuf("colt", [128, 96], F32)
    Gc = k.sbuf("Gc", [128, 32], F32)
    qk = k.sbuf("qk", [128, 256], F32)
    post = k.sbuf("post", [128, NTT], I32)
    posf = k.sbuf("posf", [128, NTT], F32)
    invt = k.sbuf("invt", [128, 16], F32)
    ident = k.sbuf("ident_s", [128, 128], F32)
    cosA = k.sbuf("cosA", [128, NTT, 16], F32)
    sinA = k.sbuf("sinA", [128, NTT, 16], F32)
    ang = k.sbuf("ang", [128, 16], F32)
    hT = [k.sbuf("hT%d" % i, [128, 32, 128], BF16) for i in range(HALF // 128)]
    Wb = [k.sbuf("Wb%d" % i, [128, 32, 512], BF16) for i in range(2)]
    stage = [k.sbuf("stg%d" % i, [128, 4, 512], F32) for i in range(2)]
    xt = k.sbuf("xt", [128, 4096], F32)
    tmp = {"ssq": k.sbuf("ssq", [128, 1], F32), "rs": k.sbuf("rs", [128, 1], F32),
           "xs": k.sbuf("xs", [128, 4096], F32), "junk": None}
    pT = [k.psum("pT%d" % i, [128, 4, 128], F32) for i in range(2)]
    ps = [k.psum("ps%d" % i, [128, 512], F32) for i in range(3)]
    sq = k.sbuf("sq", [128, 512], BF16)
    s4 = k.sbuf("s4", [128, 4], F32)
    t1 = k.sbuf("t1", [128, 4, 128], F32)
    t2 = k.sbuf("t2", [128, 4, 128], F32)
    ra = k.sbuf("ra", [128, 4, 16], F32)
    rb = k.sbuf("rb", [128, 4, 16], F32)
    rc = k.sbuf("rc", [128, 4, 16], F32)
    rd = k.sbuf("rd", [128, 4, 16], F32)
    ob = [k.sbuf("ob%d" % i, [128, 512], BF16) for i in range(3)]
    udram = k.dram("udram", u)

    k.dma(colt[:], cols, writes=[colt])
    k.dma(qk[:], qkg, writes=[qk])
    k.dma(post[:], pos, writes=[post])
    k.dma(invt[:], invf, writes=[invt])
    k.dma(ident[:], identd, writes=[ident])
    k.op("dve", lambda e: e.tensor_scalar(out=Gc[:], in0=colt[:, 32:64], scalar1=1.0, scalar2=None, op0=ALU.add),
         reads=[colt], writes=[Gc])
    k.op("dve", lambda e: e.tensor_tensor(out=Gc[:], in0=Gc[:], in1=colt[:, 0:32], op=ALU.mult),
         reads=[Gc, colt], writes=[Gc])
    Sc = colt
    class _S:
        def __getitem__(self, idx):
            return colt.t[idx[0], slice(64 + idx[1].start, 64 + idx[1].stop)]
    k.op("dve", lambda e: e.tensor_scalar(out=qk[:, 0:128], in0=qk[:, 0:128], scalar1=128.0 ** -0.5, scalar2=None,
                                          op0=ALU.mult), reads=[qk], writes=[qk])
    k.op("dve", lambda e: e.tensor_copy(out=posf[:], in_=post[:]), reads=[post], writes=[posf])
    angA = k.sbuf("angA", [128, NTT, 16], F32)
    aA = k.sbuf("aA", [128, NTT, 16], F32)
    kI = k.sbuf("kI", [128, NTT, 16], I32)
    kF = k.sbuf("kF", [128, NTT, 16], F32)
    C1 = 6.28125
    C2 = 2.0 * math.pi - C1
    k.op("dve", lambda e: e.tensor_tensor(out=angA[:], in0=posf[:].unsqueeze(2).broadcast_to([128, NTT, 16]),
                                          in1=invt[:].unsqueeze(1).broadcast_to([128, NTT, 16]), op=ALU.mult),
         reads=[posf, invt], writes=[angA])
    for (dstT, off) in ((cosA, 0.5 * math.pi), (sinA, 0.0)):
        k.op("dve", lambda e: e.tensor_scalar(out=aA[:], in0=angA[:], scalar1=off, scalar2=None, op0=ALU.add),
             reads=[angA], writes=[aA])
        k.op("dve", lambda e: e.tensor_scalar(out=kI[:], in0=aA[:], scalar1=1.0 / (2.0 * math.pi), scalar2=None, op0=ALU.mult),
             reads=[aA], writes=[kI])
        k.op("dve", lambda e: e.tensor_copy(out=kF[:], in_=kI[:]), reads=[kI], writes=[kF])
        k.op("dve", lambda e: e.scalar_tensor_tensor(out=aA[:], in0=kF[:], scalar=-C1, in1=aA[:], op0=ALU.mult, op1=ALU.add),
             reads=[kF, aA], writes=[aA])
        k.op("dve", lambda e: e.scalar_tensor_tensor(out=aA[:], in0=kF[:], scalar=-C2, in1=aA[:], op0=ALU.mult, op1=ALU.add),
             reads=[kF, aA], writes=[aA])
        k.op("dve", lambda e: e.tensor_scalar(out=aA[:], in0=aA[:], scalar1=-math.pi, scalar2=math.pi, op0=ALU.max, op1=ALU.min),
             reads=[aA], writes=[aA])
        k.op("act", lambda e: e.activation(out=dstT[:], in_=aA[:], func=ACT.Sin), reads=[aA], writes=[dstT])
    ShiftView = _S()
    class _SRes:
        pass
    it = 0
    pi = 0
    oi = 0
    for half in range(NT // HALF):
        for tt in range(HALF // 128):
            gt = half * (HALF // 128) + tt
            k.dma(xt[:], x[gt * 128:(gt + 1) * 128, :], writes=[xt])
            norm_transpose_affine(k, xt, ident, Gc, _ColView(colt, 64), hT[tt], slice(0, 128), pT, tmp)
        for cb in range(NCB):
            W = Wb[it % 2]
            if it == 0:
                load_w_block(k, w[:, cb * 512:(cb + 1) * 512], stage, W, it)
            it += 1
            nxt = it if it < (NT // HALF) * NCB else None
            nparts = HALF // 128
            kind = "plain"
            if NCB == 20:
                if 2 <= cb < 8: kind = "q"
                elif 8 <= cb < 14: kind = "k"
            else:
                kind = ("plain", "q", "k", "plain")[cb % 4]
            for tt in range(HALF // 128):
                gt = half * (HALF // 128) + tt
                p = ps[pi % 3]; pi += 1
                for kc in range(32):
                    k.op("pe", lambda e: e.matmul(p[:], lhsT=hT[tt][:, kc, :], rhs=W[:, kc, :],
                                                  start=(kc == 0), stop=(kc == 31)),
                         reads=[hT[tt], W], writes=[p], signal=(kc == 31))
                if nxt is not None:
                    ncb = nxt % NCB
                    for s_ in range(8):
                        if s_ * nparts // 8 == tt:
                            load_w_stage(k, w[:, ncb * 512:(ncb + 1) * 512], stage, Wb[nxt % 2], nxt, s_)
                o = ob[oi % 3]; oi += 1
                if kind == "plain":
                    k.op("act", lambda e: e.activation(out=o[:], in_=p[:], func=ACT.Copy), reads=[p], writes=[o])
                else:
                    g0 = 0 if kind == "q" else 128
                    pv = p[:].rearrange("p (h d) -> p h d", h=4)
                    k.op("act", lambda e: e.activation(out=sq[:], in_=p[:], func=ACT.Square), reads=[p], writes=[sq])
                    k.op("dve", lambda e: e.tensor_reduce(out=s4[:], in_=sq[:].rearrange("p (h d) -> p h d", h=4),
                                                          axis=AX.X, op=ALU.add), reads=[sq], writes=[s4])
                    k.op("dve", lambda e: e.tensor_scalar(out=s4[:], in0=s4[:], scalar1=1.0 / 128, scalar2=EPS,
                                                          op0=ALU.mult, op1=ALU.add), reads=[s4], writes=[s4])
                    k.op("act", lambda e: e.activation(out=s4[:], in_=s4[:], func=ACT.Sqrt), reads=[s4], writes=[s4])
                    k.op("dve", lambda e: e.reciprocal(out=s4[:], in_=s4[:]), reads=[s4], writes=[s4])
                    k.op("dve", lambda e: e.tensor_tensor(out=t1[:], in0=pv, in1=s4[:].unsqueeze(2).broadcast_to([128, 4, 128]),
                                                          op=ALU.mult), reads=[p, s4], writes=[t1])
                    k.op("pool", lambda e: e.tensor_tensor(out=t2[:], in0=t1[:],
                                                           in1=qk[:, g0:g0 + 128].unsqueeze(1).broadcast_to([128, 4, 128]),
                                                           op=ALU.mult), reads=[t1, qk], writes=[t2])
                    cb_ = cosA[:, gt, :].unsqueeze(1).broadcast_to([128, 4, 16])
                    sb_ = sinA[:, gt, :].unsqueeze(1).broadcast_to([128, 4, 16])
                    A = t2[:, :, 0:16]; B = t2[:, :, 16:32]
                    k.op("dve", lambda e: e.tensor_tensor(out=ra[:], in0=A, in1=cb_, op=ALU.mult), reads=[t2, cosA], writes=[ra])
                    k.op("pool", lambda e: e.tensor_tensor(out=rb[:], in0=B, in1=sb_, op=ALU.mult), reads=[t2, sinA], writes=[rb])
                    k.op("dve", lambda e: e.tensor_tensor(out=rc[:], in0=B, in1=cb_, op=ALU.mult), reads=[t2, cosA], writes=[rc])
                    k.op("pool", lambda e: e.tensor_tensor(out=rd[:], in0=A, in1=sb_, op=ALU.mult), reads=[t2, sinA], writes=[rd])
                    k.op("dve", lambda e: e.tensor_tensor(out=t2[:, :, 0:16], in0=ra[:], in1=rb[:], op=ALU.subtract),
                         reads=[ra, rb], writes=[t2])
                    k.op("dve", lambda e: e.tensor_tensor(out=t2[:, :, 16:32], in0=rc[:], in1=rd[:], op=ALU.add),
                         reads=[rc, rd], writes=[t2])
                    k.op("act", lambda e: e.activation(out=o[:], in_=t2[:].rearrange("p h d -> p (h d)"), func=ACT.Copy),
                         reads=[t2], writes=[o])
                k.dma(u[gt * 128:(gt + 1) * 128, cb * 512:(cb + 1) * 512], o[:], reads=[o], writes=[udram], q="act")
    k.finish("sp")
    k.close()
    return nc


class _ColView:
    def __init__(self, res, off):
        self.res = res; self.off = off
        self.__dict__["_r"] = res
    def __getitem__(self, idx):
        a, b = idx
        return self.res.t[a, slice(self.off + b.start, self.off + b.stop)]
    def __getattr__(self, n):
        return getattr(self.__dict__["_r"], n)
    def __setattr__(self, n, v):
        if n in ("res", "off"):
            self.__dict__[n] = v
        else:
            setattr(self.__dict__["_r"], n, v)


def host_inputs_pa(xc, gain1, scale1, shift1, w_in, q_gain, k_gain, positions_c):
    def col(v): return np.ascontiguousarray(v.reshape(32, 128).T)
    cols = np.concatenate([col(gain1), col(scale1), col(shift1)], axis=1).astype(np.float32)
    qkg = np.concatenate([np.tile(q_gain[None, :], (128, 1)), np.tile(k_gain[None, :], (128, 1))], axis=1).astype(np.float32)
    NT = xc.shape[0]
    pos = np.ascontiguousarray(positions_c.reshape(NT // 128, 128).T).astype(np.int32)
    invf = (np.float32(500000.0) ** (-np.arange(16, dtype=np.float32) * np.float32(2.0 / 32))).astype(np.float32)
    return {"x": xc, "cols": cols, "w": w_in, "qkg": qkg, "pos": pos,
            "invf": np.tile(invf[None, :], (128, 1)), "ident": np.eye(128, dtype=np.float32)}


PATS = (1, 4, 16)

def build_pb(S=8192, NU=6, NF=2):
    nc = bass.Bass("TRN2", target_bir_lowering=False)
    SP = S + 2112
    qTd = nc.dram_tensor("qT", [NU, 128, S], BF16, kind="ExternalInput").ap()
    kTd = nc.dram_tensor("kT", [NU, 128, S + 2048], BF16, kind="ExternalInput").ap()
    vpd = nc.dram_tensor("vp", [NU, SP, 129], BF16, kind="ExternalInput").ap()
    gad = nc.dram_tensor("ga", [NU, 128, 128], F32, kind="ExternalInput").ap()
    maskd = nc.dram_tensor("mask", [128, 256], BF16, kind="ExternalInput").ap()
    yad = nc.dram_tensor("ya", [NU, S, 128], BF16, kind="ExternalOutput").ap()
    NB = S // 128
    N1 = S // 128
    ufd = nc.dram_tensor("uf", [NF, N1, 128 * 128], BF16, kind="ExternalInput").ap()
    d64d = nc.dram_tensor("d64", [N1, 2 * N1], BF16, kind="ExternalInput").ap()
    gabd = nc.dram_tensor("gab", [2, 128, N1, 256], BF16, kind="ExternalInput").ap()
    csd = nc.dram_tensor("cs", [128, 256], BF16, kind="ExternalInput").ap()
    gfd = nc.dram_tensor("gf", [NF, 128, 128], F32, kind="ExternalInput").ap()
    yfd = nc.dram_tensor("yf", [NF, S, 128], BF16, kind="ExternalOutput").ap()
    accd = [nc.dram_tensor("acc%d" % i, [3, S, 129], F32, kind="Internal").ap() for i in range(2)]
    k = K(nc)
    accR = [k.dram("accR%d" % i, accd[i]) for i in range(2)]
    yaR = k.dram("yaR", yad); yfR = k.dram("yfR", yfd)
    mask = k.sbuf("mask_s", [128, 256], BF16)
    k.dma(mask[:], maskd, writes=[mask])
    qT = k.sbuf("qTs", [128, S], BF16)
    kT = k.sbuf("kTs", [128, S + 2048], BF16)
    NVT = max((S // (128 * d) + 1) * d for d in PATS)
    Vb = [k.sbuf("Vb%d" % i, [128, NVT, 129], BF16) for i in range(2)]
    gaL = [k.sbuf("ga_s%d" % i, [128, 128], F32) for i in range(2)]
    st = [k.psum("st%d" % i, [128, 512], F32) for i in range(2)]
    ops = [k.psum("o%d" % i, [128, 2, 129], F32) for i in range(2)]
    pt = [k.sbuf("pt%d" % i, [128, 512], BF16) for i in range(3)]
    GB_ = 8
    stg = [k.sbuf("stg%d" % i, [128, GB_, 129], F32) for i in range(2)]
    a3 = [k.sbuf("a3_%d" % i, [128, 8, 129], F32) for i in range(3)]
    sq8 = k.sbuf("sq8", [128, 8, 128], BF16)
    s8 = k.sbuf("s8", [128, 8], F32)
    d8 = k.sbuf("d8", [128, 8], F32)
    y1 = k.sbuf("y1", [128, 8, 128], F32)
    yo = [k.sbuf("yo%d" % i, [128, 8, 128], BF16) for i in range(2)]
    vi = 0; bi = 0; gi = 0; yi = 0
    def second_pass(u):
        nonlocal yi
        acc = accd[u % 2]; aR = accR[u % 2]; ga = gaL[u % 2]
        for qd in range(S // 1024):
            for pi_ in range(3):
                src = acc[pi_, qd * 1024:(qd + 1) * 1024, :].rearrange("(p t) c -> p t c", p=128)
                k.dma(a3[pi_][:], src, reads=[aR], writes=[a3[pi_]])
            k.op("dve", lambda e: e.tensor_tensor(out=a3[0][:], in0=a3[0][:], in1=a3[1][:], op=ALU.add),
                 reads=[a3[0], a3[1]], writes=[a3[0]])
            k.op("dve", lambda e: e.tensor_tensor(out=a3[0][:], in0=a3[0][:], in1=a3[2][:], op=ALU.add),
                 reads=[a3[0], a3[2]], writes=[a3[0]])
            num = a3[0][:, :, 0:128]
            den = a3[0][:, :, 128]
            k.op("act", lambda e: e.activation(out=sq8[:], in_=num, func=ACT.Square), reads=[a3[0]], writes=[sq8])
            k.op("dve", lambda e: e.tensor_reduce(out=s8[:], in_=sq8[:], axis=AX.X, op=ALU.add), reads=[sq8], writes=[s8])
            k.op("dve", lambda e: e.tensor_tensor(out=d8[:], in0=den, in1=den, op=ALU.mult), reads=[a3[0]], writes=[d8])
            k.op("dve", lambda e: e.tensor_scalar(out=d8[:], in0=d8[:], scalar1=EPS, scalar2=None, op0=ALU.mult), reads=[d8], writes=[d8])
            k.op("dve", lambda e: e.scalar_tensor_tensor(out=s8[:], in0=s8[:], scalar=1.0 / 128, in1=d8[:], op0=ALU.mult, op1=ALU.add),
                 reads=[s8, d8], writes=[s8])
            k.op("act", lambda e: e.activation(out=s8[:], in_=s8[:], func=ACT.Sqrt), reads=[s8], writes=[s8])
            k.op("dve", lambda e: e.reciprocal(out=s8[:], in_=s8[:]), reads=[s8], writes=[s8])
            k.op("dve", lambda e: e.tensor_tensor(out=y1[:], in0=num, in1=s8[:].unsqueeze(2).broadcast_to([128, 8, 128]), op=ALU.mult),
                 reads=[a3[0], s8], writes=[y1])
            yo_ = yo[yi % 2]; yi += 1
            k.op("pool", lambda e: e.tensor_tensor(out=yo_[:], in0=y1[:], in1=ga[:].unsqueeze(1).broadcast_to([128, 8, 128]), op=ALU.mult),
                 reads=[y1, ga], writes=[yo_])
            k.dma(yad[u, qd * 1024:(qd + 1) * 1024, :].rearrange("(p t) c -> p t c", p=128), yo_[:], reads=[yo_], writes=[yaR])

    for u in range(NU):
        acc = accd[u % 2]; aR = accR[u % 2]
        k.dma(qT[:], qTd[u], writes=[qT])
        k.dma(kT[:], kTd[u], writes=[kT])
        ga = gaL[u % 2]
        k.dma(ga[:], gad[u], writes=[ga])
        def load_v(uu, d, V):
            nblk_ = S // (128 * d)
            for r in range(d):
                base = 1024 - 64 * d + r
                L = (nblk_ + 1) * 128 * d
                src = vpd[uu, base:base + L, :].rearrange("(j p d) c -> p j d c", p=128, d=d)[:, :, 0, :]
                for j0 in range(0, nblk_ + 1, 16):
                    j1 = min(nblk_ + 1, j0 + 16)
                    k.dma(V[:, r * (nblk_ + 1) + j0:r * (nblk_ + 1) + j1, :], src[:, j0:j1, :], writes=[V])

        if u == 0:
            load_v(0, PATS[0], Vb[vi % 2])
        for pi_, d in enumerate(PATS):
            nblk = S // (128 * d)
            V = Vb[vi % 2]; vi += 1
            if pi_ + 1 < len(PATS):
                load_v(u, PATS[pi_ + 1], Vb[vi % 2])
            elif u + 1 < NU:
                load_v(u + 1, PATS[0], Vb[vi % 2])
            groups = [(r, n) for r in range(d) for n in range(0, nblk, 2)]

            def emit_qk(gidx_):
                r, n = groups[gidx_]
                s_ = st[gidx_ % 2]
                for bb in range(2):
                    qs = (128 * (n + bb)) * d + r
                    qcols = qT[:, qs:qs + 127 * d + 1:d]
                    for half in range(2):
                        ks = 1024 + (128 * (n + bb + half) - 64) * d + r
                        c0 = bb * 256 + half * 128
                        k.op("pe", lambda e: e.matmul(s_[:, c0:c0 + 128], lhsT=kT[:, ks:ks + 127 * d + 1:d],
                                                      rhs=qcols, start=True, stop=True),
                             reads=[kT, qT], writes=[s_], signal=(bb == 1 and half == 1))

            emit_qk(0)
            sg = None
            if pi_ == 1 and u > 0:
                second_pass(u - 1)
            for gix_, (r, n) in enumerate(groups):
                n0 = (n // GB_) * GB_
                if n == n0:
                    sg = stg[gi % 2]; gi += 1
                s_ = st[gix_ % 2]; o_ = ops[gix_ % 2]; p_ = pt[gix_ % 3]
                k.op("act", lambda e: e.activation(out=p_[:], in_=s_[:], func=ACT.Exp), reads=[s_], writes=[p_])
                if gix_ + 1 < len(groups):
                    emit_qk(gix_ + 1)
                k.op("dve" if bi % 2 == 0 else "pool",
                     lambda e: e.tensor_tensor(out=p_[:].rearrange("p (b c) -> p b c", b=2), in0=p_[:].rearrange("p (b c) -> p b c", b=2),
                                               in1=mask[:].unsqueeze(1).broadcast_to([128, 2, 256]), op=ALU.mult),
                     reads=[p_, mask], writes=[p_])
                for bb in range(2):
                    for half in range(2):
                        c0 = bb * 256 + half * 128
                        k.op("pe", lambda e: e.matmul(o_[:, bb, :], lhsT=p_[:, c0:c0 + 128],
                                                      rhs=V[:, r * (nblk + 1) + n + bb + half, :], start=(half == 0), stop=(half == 1)),
                             reads=[p_, V], writes=[o_], signal=(bb == 1 and half == 1))
                if bi % 2 == 0:
                    k.op("dve", lambda e: e.tensor_copy(out=sg[:, n - n0:n - n0 + 2, :], in_=o_[:]), reads=[o_], writes=[sg])
                else:
                    k.op("act", lambda e: e.activation(out=sg[:, n - n0:n - n0 + 2, :], in_=o_[:], func=ACT.Copy), reads=[o_], writes=[sg])
                bi += 1
                n1 = min(nblk, n0 + GB_)
                if n + 2 >= n1:
                    seg = acc[pi_, 128 * n0 * d:128 * n1 * d, :].rearrange("(n p d) c -> p n d c", p=128, d=d)[:, :, r, :]
                    k.dma(seg, sg[:, 0:n1 - n0, :], reads=[sg], writes=[aR], q="act")
    second_pass(NU - 1)
    if NF:
        UX = k.sbuf("UX", [128, 16384], BF16)
        Z = k.sbuf("Z", [128, 128, 2 * N1], BF16)
        D64 = k.sbuf("D64", [N1, 2 * N1], BF16)
        CS = k.sbuf("CS", [128, 256], BF16)
        gf = k.sbuf("gf_s", [128, 128], F32)
        GAB = [k.sbuf("GAB%d" % i, [128, 2, 4, 256], BF16) for i in range(2)]
        pz = [k.psum("pz%d" % i, [128, 512], F32) for i in range(2)]
        ys = k.sbuf("ys", [128, N1, 128], BF16)
        sq4 = k.sbuf("sq4", [128, 4, 128], BF16)
        s4 = k.sbuf("s4f", [128, 4], F32)
        y4 = k.sbuf("y4", [128, 4, 128], F32)
        k.dma(D64[:], d64d, writes=[D64])
        k.dma(CS[:], csd, writes=[CS])
        zi = 0; gbi = 0
        for f in range(NF):
            k.dma(UX[0:N1, 0:16384], ufd[f], writes=[UX])
            k.dma(gf[:], gfd[f], writes=[gf])
            W2 = 2 * N1
            per = 512 // W2
            for c0 in range(0, 128, per):
                p_ = pz[zi % 2]; zi += 1
                for j in range(per):
                    c = c0 + j
                    k.op("pe", lambda e: e.matmul(p_[:, j * W2:(j + 1) * W2], lhsT=UX[0:N1, c:16384:128], rhs=D64[:], start=True, stop=True),
                         reads=[UX, D64], writes=[p_], signal=(j == per - 1))
                k.op("act" if zi % 2 else "dve",
                     (lambda e: e.activation(out=Z[:, c0:c0 + per, :].rearrange("p c w -> p (c w)"), in_=p_[:, 0:per * W2], func=ACT.Copy)) if zi % 2 else
                     (lambda e: e.tensor_copy(out=Z[:, c0:c0 + per, :].rearrange("p c w -> p (c w)"), in_=p_[:, 0:per * W2])),
                     reads=[p_], writes=[Z])
            XT = UX[:, 0:N1 * 256].rearrange("p (k w) -> p k w", w=256)
            for kg in range(0, N1, 4):
                G = GAB[gbi % 2]; gbi += 1
                kn = min(4, N1 - kg)
                k.dma(G[:, 0, 0:kn, :], gabd[0, :, kg:kg + kn, :], writes=[G])
                k.dma(G[:, 1, 0:kn, :], gabd[1, :, kg:kg + kn, :], writes=[G])
                for k1 in range(kg, kg + kn):
                    p_ = pz[zi % 2]; zi += 1
                    k.op("pe", lambda e: e.matmul(p_[:, 0:256], lhsT=Z[:, :, k1], rhs=G[:, 0, k1 - kg, :], start=True, stop=False),
                         reads=[Z, G], writes=[p_], signal=False)
                    k.op("pe", lambda e: e.matmul(p_[:, 0:256], lhsT=Z[:, :, N1 + k1], rhs=G[:, 1, k1 - kg, :], start=False, stop=True),
                         reads=[Z, G], writes=[p_])
                    if zi % 2:
                        k.op("act", lambda e: e.activation(out=XT[:, k1, :], in_=p_[:, 0:256], func=ACT.Copy), reads=[p_], writes=[UX])
                    else:
                        k.op("dve", lambda e: e.tensor_copy(out=XT[:, k1, :], in_=p_[:, 0:256]), reads=[p_], writes=[UX])
            for k0 in range(0, N1, 4):
                p_ = pz[zi % 2]; zi += 1
                for j in range(4):
                    k1 = k0 + j
                    k.op("pe", lambda e: e.matmul(p_[:, j * 128:(j + 1) * 128], lhsT=XT[:, k1, 0:128], rhs=CS[:, 0:128], start=True, stop=False),
                         reads=[UX, CS], writes=[p_], signal=False)
                    k.op("pe", lambda e: e.matmul(p_[:, j * 128:(j + 1) * 128], lhsT=XT[:, k1, 128:256], rhs=CS[:, 128:256], start=False, stop=True),
                         reads=[UX, CS], writes=[p_], signal=(j == 3))
                pv = p_[:].rearrange("p (j c) -> p j c", j=4)
                k.op("act", lambda e: e.activation(out=sq4[:], in_=pv, func=ACT.Square), reads=[p_], writes=[sq4])
                k.op("dve", lambda e: e.tensor_reduce(out=s4[:], in_=sq4[:], axis=AX.X, op=ALU.add), reads=[sq4], writes=[s4])
                k.op("dve", lambda e: e.tensor_scalar(out=s4[:], in0=s4[:], scalar1=1.0 / 128, scalar2=EPS, op0=ALU.mult, op1=ALU.add),
                     reads=[s4], writes=[s4])
                k.op("act", lambda e: e.activation(out=s4[:], in_=s4[:], func=ACT.Sqrt), reads=[s4], writes=[s4])
                k.op("dve", lambda e: e.reciprocal(out=s4[:], in_=s4[:]), reads=[s4], writes=[s4])
                k.op("dve", lambda e: e.tensor_tensor(out=y4[:], in0=pv, in1=s4[:].unsqueeze(2).broadcast_to([128, 4, 128]), op=ALU.mult),
                     reads=[p_, s4], writes=[y4])
                k.op("pool", lambda e: e.tensor_tensor(out=ys[:, k0:k0 + 4, :], in0=y4[:], in1=gf[:].unsqueeze(1).broadcast_to([128, 4, 128]), op=ALU.mult),
                     reads=[y4, gf], writes=[ys])
            k.dma(yfd[f].rearrange("(k2 k1) c -> k2 k1 c", k1=N1), ys[:], reads=[ys], writes=[yfR])
    k.finish("sp")
    k.close()
    return nc


def fourier_tables(S):
    N1 = S // 128
    n1 = np.arange(N1)
    th = 2 * np.pi * np.outer(n1, n1) / N1
    d64 = np.concatenate([np.cos(th), -np.sin(th)], axis=1)
    n2 = np.arange(128)[:, None, None]; k1 = np.arange(N1)[None, :, None]; k2 = np.arange(128)[None, None, :]
    th = 2 * np.pi * ((n2 * (k1 + N1 * k2)) % S) / S
    Gr, Gi = np.cos(th), -np.sin(th)
    ga = np.concatenate([Gr, Gi], axis=2); gb = np.concatenate([-Gi, Gr], axis=2)
    c = np.arange(128)
    ph = 2 * np.pi * np.outer(c, c) / 128
    cs = np.concatenate([np.cos(ph), np.sin(ph)], axis=1)
    return d64, np.stack([ga, gb]), cs


def build_pc(NT=2048, HALF=512, NCB=8):
    nc = bass.Bass("TRN2", target_bir_lowering=False)
    NTT = NT // 128
    yTd = nc.dram_tensor("yT", [4096, NT], BF16, kind="ExternalInput").ap()
    x = nc.dram_tensor("x", [NT, 4096], F32, kind="ExternalInput").ap()
    w = nc.dram_tensor("w", [4096, NCB * 512], F32, kind="ExternalInput").ap()
    g1d = nc.dram_tensor("g1", [128, 4096], F32, kind="ExternalInput").ap()
    cols = nc.dram_tensor("cols", [128, 96], F32, kind="ExternalInput").ap()
    wrd = nc.dram_tensor("wr", [128, 32 * 16], F32, kind="ExternalInput").ap()
    identd = nc.dram_tensor("ident", [128, 128], F32, kind="ExternalInput").ap()
    x1 = nc.dram_tensor("x1", [NT, 4096], F32, kind="ExternalOutput").ap()
    affd = nc.dram_tensor("aff", [NT, 16], F32, kind="ExternalOutput").ap()
    k = K(nc)
    x1R = k.dram("x1R", x1); affR = k.dram("affR", affd)
    colt = k.sbuf("colt", [128, 96], F32)
    Gc = k.sbuf("Gc", [128, 32], F32)
    g1 = k.sbuf("g1s", [128, 4096], F32)
    wr = k.sbuf("wrs", [128, 32, 16], F32)
    ident = k.sbuf("ident_s", [128, 128], F32)
    yT = [k.sbuf("yTs%d" % i, [128, 32, 128], BF16) for i in range(HALF // 128)]
    Wb = [k.sbuf("Wb%d" % i, [128, 32, 512], BF16) for i in range(2)]
    stage = [k.sbuf("stg%d" % i, [128, 4, 512], F32) for i in range(2)]
    xt = k.sbuf("xt", [128, 4096], F32)
    tmp = {"ssq": k.sbuf("ssq", [128, 1], F32), "rs": k.sbuf("rs", [128, 1], F32),
           "xs": k.sbuf("xs", [128, 4096], F32), "junk": None}
    h32 = k.sbuf("h32", [128, 32, 128], F32)
    xc = [k.sbuf("xc%d" % i, [128, 512], F32) for i in range(3)]
    tt_ = [k.sbuf("tt%d" % i, [128, 512], F32) for i in range(3)]
    pT = [k.psum("pT%d" % i, [128, 4, 128], F32) for i in range(2)]
    ps = [k.psum("ps%d" % i, [128, 512], F32) for i in range(3)]
    pr = k.psum("pr", [128, 16], F32)
    mx = k.sbuf("mx", [128, 1], F32); sm = k.sbuf("sm", [128, 1], F32)
    ex = k.sbuf("ex", [128, 16], F32); af = k.sbuf("af", [128, 16], F32)
    k.dma(colt[:], cols, writes=[colt]); k.dma(g1[:], g1d, writes=[g1])
    k.dma(wr[:].rearrange("p a b -> p (a b)"), wrd, writes=[wr]); k.dma(ident[:], identd, writes=[ident])
    k.op("dve", lambda e: e.tensor_scalar(out=Gc[:], in0=colt[:, 32:64], scalar1=1.0, scalar2=None, op0=ALU.add), reads=[colt], writes=[Gc])
    k.op("dve", lambda e: e.tensor_tensor(out=Gc[:], in0=Gc[:], in1=colt[:, 0:32], op=ALU.mult), reads=[Gc, colt], writes=[Gc])
    it = 0; pi = 0; oi = 0
    for half in range(NT // HALF):
        for tt in range(HALF // 128):
            k.dma(yT[tt][:], yTd[:, half * HALF + tt * 128:half * HALF + (tt + 1) * 128].rearrange("(kc p) t -> p kc t", p=128), writes=[yT[tt]])
        for cb in range(NCB):
            W = Wb[it % 2]
            if it == 0:
                load_w_block(k, w[:, cb * 512:(cb + 1) * 512], stage, W, it)
            it += 1
            nxt = it if it < (NT // HALF) * NCB else None
            nparts = HALF // 128
            for tt in range(HALF // 128):
                gt = half * (HALF // 128) + tt
                p = ps[pi % 3]; pi += 1
                xcb = xc[oi % 3]; tb = tt_[oi % 3]; oi += 1
                k.dma(xcb[:], x[gt * 128:(gt + 1) * 128, cb * 512:(cb + 1) * 512], writes=[xcb])
                for kc in range(32):
                    k.op("pe", lambda e: e.matmul(p[:], lhsT=yT[tt][:, kc, :], rhs=W[:, kc, :],
                                                  start=(kc == 0), stop=(kc == 31)), reads=[yT[tt], W], writes=[p], signal=(kc == 31))
                if nxt is not None:
                    ncb = nxt % NCB
                    for s_ in range(8):
                        if s_ * nparts // 8 == tt:
                            load_w_stage(k, w[:, ncb * 512:(ncb + 1) * 512], stage, Wb[nxt % 2], nxt, s_)
                k.op("dve", lambda e: e.tensor_tensor(out=tb[:], in0=p[:], in1=g1[:, cb * 512:(cb + 1) * 512], op=ALU.mult),
                     reads=[p, g1], writes=[tb])
                k.op("pool", lambda e: e.tensor_tensor(out=tb[:], in0=tb[:], in1=xcb[:], op=ALU.add), reads=[tb, xcb], writes=[tb])
                k.dma(x1[gt * 128:(gt + 1) * 128, cb * 512:(cb + 1) * 512], tb[:], reads=[tb], writes=[x1R], q="act")
    Sv = _ColView(colt, 64)
    for gt in range(NTT):
        k.dma(xt[:], x1[gt * 128:(gt + 1) * 128, :], reads=[x1R], writes=[xt])
        norm_transpose_affine(k, xt, ident, Gc, Sv, None, None, pT, tmp, dst32=h32)
        for kc in range(32):
            k.op("pe", lambda e: e.matmul(pr[:], lhsT=h32[:, kc, :], rhs=wr[:, kc, :], start=(kc == 0), stop=(kc == 31)),
                 reads=[h32, wr], writes=[pr], signal=(kc == 31))
        k.op("dve", lambda e: e.tensor_reduce(out=mx[:], in_=pr[:], axis=AX.X, op=ALU.max), reads=[pr], writes=[mx])
        k.op("dve", lambda e: e.tensor_scalar(out=mx[:], in0=mx[:], scalar1=-1.0, scalar2=None, op0=ALU.mult), reads=[mx], writes=[mx])
        k.op("act", lambda e: e.activation(out=ex[:], in_=pr[:], func=ACT.Exp, bias=mx[:, 0:1], accum_out=sm[:]),
             reads=[pr, mx], writes=[ex, sm])
        k.op("dve", lambda e: e.reciprocal(out=sm[:], in_=sm[:]), reads=[sm], writes=[sm])
        k.op("dve", lambda e: e.tensor_scalar(out=af[:], in0=ex[:], scalar1=sm[:, 0:1], scalar2=None, op0=ALU.mult),
             reads=[ex, sm], writes=[af])
        k.dma(affd[gt * 128:(gt + 1) * 128, :], af[:], reads=[af], writes=[affR])
    k.finish("sp"); k.close()
    return nc


CAP = 1024
ZROW = 16 * 1024

def build_pd1(NITER=34):
    nc = bass.Bass("TRN2", target_bir_lowering=False)
    affd = nc.dram_tensor("affu", [128, 4 * 64], F32, kind="ExternalInput").ap()
    eoffd = nc.dram_tensor("eoff", [128, 4], F32, kind="ExternalInput").ap()
    onesd = nc.dram_tensor("ones", [128, 128], F32, kind="ExternalInput").ap()
    lowd = nc.dram_tensor("lstrict", [128, 128], F32, kind="ExternalInput").ap()
    identd = nc.dram_tensor("ident", [128, 128], F32, kind="ExternalInput").ap()
    gidxd = nc.dram_tensor("gidx", [128, 4 * 64], I32, kind="ExternalOutput").ap()
    maskd = nc.dram_tensor("msk", [128, 4 * 64], F32, kind="ExternalOutput").ap()
    k = K(nc)
    aff = k.sbuf("aff", [128, 4, 64], F32); eoff = k.sbuf("eoff_s", [128, 4], F32)
    ones = k.sbuf("ones_s", [128, 128], F32); low = k.sbuf("low_s", [128, 128], F32); ident = k.sbuf("ident_s", [128, 128], F32)
    lo = k.sbuf("lo", [128, 4], F32); hi = k.sbuf("hi", [128, 4], F32); mid = k.sbuf("mid", [128, 4], F32)
    cmp_ = k.sbuf("cmp", [128, 4, 64], F32); cnt = k.sbuf("cnt", [128, 4], F32); ge = k.sbuf("ge", [128, 4], F32)
    d1 = k.sbuf("d1", [128, 4], F32)
    tot = k.psum("tot", [128, 4], F32)
    k.dma(aff[:].rearrange("p a b -> p (a b)"), affd, writes=[aff]); k.dma(eoff[:], eoffd, writes=[eoff])
    k.dma(ones[:], onesd, writes=[ones]); k.dma(low[:], lowd, writes=[low]); k.dma(ident[:], identd, writes=[ident])
    k.op("dve", lambda e: e.memset(lo[:], 0.0), writes=[lo])
    k.op("dve", lambda e: e.memset(hi[:], 1.0), writes=[hi])
    for it in range(NITER):
        k.op("dve", lambda e: e.tensor_tensor(out=mid[:], in0=lo[:], in1=hi[:], op=ALU.add), reads=[lo, hi], writes=[mid])
        k.op("dve", lambda e: e.tensor_scalar(out=mid[:], in0=mid[:], scalar1=0.5, scalar2=None, op0=ALU.mult), reads=[mid], writes=[mid])
        k.op("dve", lambda e: e.tensor_tensor(out=cmp_[:], in0=aff[:], in1=mid[:].unsqueeze(2).broadcast_to([128, 4, 64]), op=ALU.is_ge),
             reads=[aff, mid], writes=[cmp_])
        k.op("dve", lambda e: e.tensor_reduce(out=cnt[:], in_=cmp_[:], axis=AX.X, op=ALU.add), reads=[cmp_], writes=[cnt])
        k.op("pe", lambda e: e.matmul(tot[:], lhsT=ones[:], rhs=cnt[:], start=True, stop=True), reads=[ones, cnt], writes=[tot])
        k.op("dve", lambda e: e.tensor_scalar(out=ge[:], in0=tot[:], scalar1=float(CAP), scalar2=None, op0=ALU.is_ge), reads=[tot], writes=[ge])
        k.op("dve", lambda e: e.tensor_tensor(out=d1[:], in0=mid[:], in1=lo[:], op=ALU.subtract), reads=[mid, lo], writes=[d1])
        k.op("dve", lambda e: e.tensor_tensor(out=d1[:], in0=d1[:], in1=ge[:], op=ALU.mult), reads=[d1, ge], writes=[d1])
        k.op("dve", lambda e: e.tensor_tensor(out=lo[:], in0=lo[:], in1=d1[:], op=ALU.add), reads=[lo, d1], writes=[lo])
        k.op("dve", lambda e: e.tensor_tensor(out=d1[:], in0=hi[:], in1=mid[:], op=ALU.subtract), reads=[hi, mid], writes=[d1])
        k.op("dve", lambda e: e.tensor_tensor(out=d1[:], in0=d1[:], in1=ge[:], op=ALU.mult), reads=[d1, ge], writes=[d1])
        k.op("dve", lambda e: e.tensor_tensor(out=hi[:], in0=mid[:], in1=d1[:], op=ALU.add), reads=[mid, d1], writes=[hi])
    msk = k.sbuf("msk_s", [128, 4, 64], F32)
    k.op("dve", lambda e: e.tensor_tensor(out=msk[:], in0=aff[:], in1=lo[:].unsqueeze(2).broadcast_to([128, 4, 64]), op=ALU.is_ge),
         reads=[aff, lo], writes=[msk])
    k.op("dve", lambda e: e.tensor_reduce(out=cnt[:], in_=msk[:], axis=AX.X, op=ALU.add), reads=[msk], writes=[cnt])
    offp = k.psum("offp", [128, 4], F32)
    k.op("pe", lambda e: e.matmul(offp[:], lhsT=low[:], rhs=cnt[:], start=True, stop=True), reads=[low, cnt], writes=[offp])
    offs = k.sbuf("offs", [128, 4], F32)
    k.op("dve", lambda e: e.tensor_copy(out=offs[:], in_=offp[:]), reads=[offp], writes=[offs])
    mT = k.sbuf("mT", [64, 128], F32)
    tp = k.psum("tp", [64, 128], F32)
    wp = k.psum("wp", [128, 64], F32)
    pos = k.sbuf("pos", [128, 4, 64], F32)
    sel = k.sbuf("sel", [128, 4, 64], F32)
    gi = k.sbuf("gi", [128, 4, 64], I32)
    for u in range(4):
        k.op("pe", lambda e: e.transpose(out=tp[:], in_=msk[:, u, :], identity=ident[:]), reads=[msk, ident], writes=[tp])
        k.op("dve", lambda e: e.tensor_copy(out=mT[:], in_=tp[:]), reads=[tp], writes=[mT])
        k.op("pe", lambda e: e.matmul(wp[:], lhsT=mT[:], rhs=low[0:64, 0:64], start=True, stop=True), reads=[mT, low], writes=[wp])
        k.op("dve", lambda e: e.tensor_scalar(out=pos[:, u, :], in0=wp[:], scalar1=offs[:, u:u + 1], scalar2=None, op0=ALU.add),
             reads=[wp, offs], writes=[pos])
    k.op("dve", lambda e: e.tensor_scalar(out=sel[:], in0=pos[:], scalar1=float(CAP), scalar2=None, op0=ALU.is_lt), reads=[pos], writes=[sel])
    k.op("dve", lambda e: e.tensor_tensor(out=sel[:], in0=sel[:], in1=msk[:], op=ALU.mult), reads=[sel, msk], writes=[sel])
    k.op("dve", lambda e: e.tensor_tensor(out=pos[:], in0=pos[:], in1=eoff[:].unsqueeze(2).broadcast_to([128, 4, 64]), op=ALU.add),
         reads=[pos, eoff], writes=[pos])
    k.op("dve", lambda e: e.tensor_tensor(out=pos[:], in0=pos[:], in1=sel[:], op=ALU.mult), reads=[pos, sel], writes=[pos])
    k.op("dve", lambda e: e.tensor_scalar(out=pos[:], in0=pos[:], scalar1=float(ZROW), scalar2=None, op0=ALU.add), reads=[pos], writes=[pos])
    k.op("dve", lambda e: e.tensor_copy(out=gi[:], in_=pos[:]), reads=[pos], writes=[gi])
    gR = k.dram("gR", gidxd); mR = k.dram("mR", maskd)
    k.dma(gidxd, gi[:].rearrange("p a b -> p (a b)"), reads=[gi], writes=[gR])
    k.dma(maskd, sel[:].rearrange("p a b -> p (a b)"), reads=[sel], writes=[mR])
    k.finish("sp"); k.close()
    return nc

def d1_consts():
    ii = np.arange(128)
    return {"ones": np.ones((128, 128), np.float32), "lstrict": (ii[:, None] < ii[None, :]).astype(np.float32),
            "ident": np.eye(128, dtype=np.float32)}


def build_pd2(NUNIT=4, NSL=1024):
    nc = bass.Bass("TRN2", target_bir_lowering=False)
    NST = NSL // 128
    xed = nc.dram_tensor("xe", [NUNIT, NSL, 4096], F32, kind="ExternalInput").ap()
    afd = nc.dram_tensor("affs", [NUNIT, 128, NST], F32, kind="ExternalInput").ap()
    cold = nc.dram_tensor("cols", [NUNIT, 128, 96], F32, kind="ExternalInput").ap()
    NE = (NUNIT + 1) // 2
    wgd = nc.dram_tensor("wg", [NE, 4096, 1024], F32, kind="ExternalInput").ap()
    wud = nc.dram_tensor("wu", [NE, 4096, 1024], F32, kind="ExternalInput").ap()
    wdd = nc.dram_tensor("wd", [NE, 1024, 4096], F32, kind="ExternalInput").ap()
    identd = nc.dram_tensor("ident", [128, 128], F32, kind="ExternalInput").ap()
    yed = nc.dram_tensor("ye", [NUNIT, NSL, 4096], F32, kind="ExternalOutput").ap()
    k = K(nc)
    yR = k.dram("yR", yed)
    ident = k.sbuf("ident_s", [128, 128], F32)
    k.dma(ident[:], identd, writes=[ident])
    coltL = [k.sbuf("colt%d" % i, [128, 96], F32) for i in range(2)]; GcL = [k.sbuf("Gc%d" % i, [128, 32], F32) for i in range(2)]
    afsL = [k.sbuf("afs%d" % i, [128, NST], F32) for i in range(2)]
    xt = k.sbuf("xt", [128, 4096], F32)
    tmp = {"ssq": k.sbuf("ssq", [128, 1], F32), "rs": k.sbuf("rs", [128, 1], F32), "xs": k.sbuf("xs", [128, 4096], F32), "junk": None}
    pT = [k.psum("pT%d" % i, [128, 4, 128], F32) for i in range(2)]
    stg = [k.sbuf("stg%d" % i, [128, 4096], F32) for i in range(2)]
    wb = [k.sbuf("wb%d" % i, [128, 4096], BF16) for i in range(4)]
    h1T = k.sbuf("h1T", [128, 8, NSL], BF16)
    sg = [k.sbuf("sg%d" % i, [128, 512], F32) for i in range(2)]
    pg = [k.psum("pg%d" % i, [128, 512], F32) for i in range(2)]
    pu = [k.psum("pu%d" % i, [128, 512], F32) for i in range(2)]
    py = [k.psum("py%d" % i, [128, 512], F32) for i in range(2)]
    ot = [k.sbuf("ot%d" % i, [128, 512], F32) for i in range(3)]
    si = 0; wi = 0; gi = 0; yi = 0; oi = 0
    NSH = max(1, NSL // 512); SHW = min(512, NSL)
    TPH = SHW // 128
    xeTL = [k.sbuf("xeT%d" % i, [128, 32, SHW], BF16) for i in range(NSH)]

    def prep_unit(u):
        colt = coltL[u % 2]; Gc = GcL[u % 2]
        k.dma(colt[:], cold[u], writes=[colt]); k.dma(afsL[u % 2][:], afd[u], writes=[afsL[u % 2]])
        k.op("dve", lambda e: e.tensor_scalar(out=Gc[:], in0=colt[:, 32:64], scalar1=1.0, scalar2=None, op0=ALU.add), reads=[colt], writes=[Gc])
        k.op("dve", lambda e: e.tensor_tensor(out=Gc[:], in0=Gc[:], in1=colt[:, 0:32], op=ALU.mult), reads=[Gc, colt], writes=[Gc])

    def norm_tile(u, st):
        k.dma(xt[:], xed[u, st * 128:(st + 1) * 128, :], writes=[xt])
        c0 = (st % TPH) * 128
        norm_transpose_affine(k, xt, ident, GcL[u % 2], _ColView(coltL[u % 2], 64), xeTL[st // TPH], slice(c0, c0 + 128), pT, tmp)

    prep_unit(0)
    for st in range(NST):
        norm_tile(0, st)
    for u in range(NUNIT):
        e_ = u // 2
        afs = afsL[u % 2]
        def load_gu(fc):
            nonlocal si, wi
            wbs = []
            for wsrc in (wgd, wud):
                s_ = stg[si % 2]; si += 1
                b_ = wb[wi % 4]; wi += 1
                sv = s_[:].rearrange("p (kc n) -> p kc n", n=128)
                src = wsrc[e_, :, fc * 128:(fc + 1) * 128].rearrange("(kc p) n -> p kc n", p=128)
                k.dma(sv[:, 0:16, :], src[:, 0:16, :], writes=[s_])
                k.dma(sv[:, 16:32, :], src[:, 16:32, :], writes=[s_])
                k.op("pool", lambda e: e.tensor_copy(out=b_[:, 0:1536], in_=s_[:, 0:1536]), reads=[s_], writes=[b_])
                k.op("dve", lambda e: e.tensor_copy(out=b_[:, 1536:4096], in_=s_[:, 1536:4096]), reads=[s_], writes=[b_])
                wbs.append(b_)
            return wbs

        def load_d(db):
            nonlocal si, wi
            s_ = stg[si % 2]; si += 1
            b_ = wb[wi % 4]; wi += 1
            sv = s_[:].rearrange("p (fc n) -> p fc n", n=512)
            src = wdd[e_, :, db * 512:(db + 1) * 512].rearrange("(fc p) n -> p fc n", p=128)
            k.dma(sv[:, 0:4, :], src[:, 0:4, :], writes=[s_])
            k.dma(sv[:, 4:8, :], src[:, 4:8, :], writes=[s_])
            k.op("pool", lambda e: e.tensor_copy(out=b_[:, 0:1536], in_=s_[:, 0:1536]), reads=[s_], writes=[b_])
            k.op("dve", lambda e: e.tensor_copy(out=b_[:, 1536:4096], in_=s_[:, 1536:4096]), reads=[s_], writes=[b_])
            return b_

        pend = load_gu(0)
        for fc in range(8):
            wbs = pend
            if fc + 1 < 8:
                pend = load_gu(fc + 1)
            else:
                pend_d = load_d(0)
            for sh in range(NSH):
                g_ = pg[gi % 2]; u_ = pu[gi % 2]; s2 = sg[gi % 2]; gi += 1
                for (pp, b_) in ((g_, wbs[0]), (u_, wbs[1])):
                    bv = b_[:].rearrange("p (kc n) -> p kc n", n=128)
                    for kc in range(32):
                        k.op("pe", lambda e: e.matmul(pp[:, 0:SHW], lhsT=bv[:, kc, :], rhs=xeTL[sh][:, kc, :],
                                                      start=(kc == 0), stop=(kc == 31)), reads=[b_, xeTL[sh]], writes=[pp], signal=(kc == 31))
                k.op("act", lambda e: e.activation(out=s2[:, 0:SHW], in_=g_[:, 0:SHW], func=ACT.Silu), reads=[g_], writes=[s2])
                k.op("dve", lambda e: e.tensor_tensor(out=h1T[:, fc, sh * SHW:(sh + 1) * SHW], in0=s2[:, 0:SHW], in1=u_[:, 0:SHW], op=ALU.mult),
                     reads=[s2, u_], writes=[h1T])
        for db in range(8):
            b_ = pend_d
            if db + 1 < 8:
                pend_d = load_d(db + 1)
            bv = b_[:].rearrange("p (fc n) -> p fc n", n=512)
            for st in range(NST):
                y_ = py[yi % 2]; yi += 1
                for fc in range(8):
                    k.op("pe", lambda e: e.matmul(y_[:], lhsT=h1T[:, fc, st * 128:(st + 1) * 128], rhs=bv[:, fc, :],
                                                  start=(fc == 0), stop=(fc == 7)), reads=[h1T, b_], writes=[y_], signal=(fc == 7))
                o_ = ot[oi % 3]; oi += 1
                k.op("act", lambda e: e.activation(out=o_[:], in_=y_[:], func=ACT.Copy, scale=afs[:, st:st + 1]), reads=[y_, afs], writes=[o_])
                k.dma(yed[u, st * 128:(st + 1) * 128, db * 512:(db + 1) * 512], o_[:], reads=[o_], writes=[yR], q="act")
            if u + 1 < NUNIT:
                if db == 0:
                    prep_unit(u + 1)
                for st2 in range(db * NST // 8, (db + 1) * NST // 8):
                    norm_tile(u + 1, st2)
    k.finish("sp"); k.close()
    return nc


ZROW = 16 * 1024

def build_pe(NTOK=4096, CW=2048):
    nc = bass.Bass("TRN2", target_bir_lowering=False)
    NTT = NTOK // 128
    x1d = nc.dram_tensor("x1c", [NTOK, CW], F32, kind="ExternalInput").ap()
    yed = nc.dram_tensor("yec", [ZROW + 1, CW], F32, kind="ExternalInput").ap()
    gid = nc.dram_tensor("gidx", [128, NTT * 16], I32, kind="ExternalInput").ap()
    g2d = nc.dram_tensor("g2", [128, CW], F32, kind="ExternalInput").ap()
    x2d = nc.dram_tensor("x2c", [NTOK, CW], F32, kind="ExternalOutput").ap()
    k = K(nc)
    xR = k.dram("xR", x2d)
    gix = k.sbuf("gix", [128, NTT * 16], I32)
    g2 = k.sbuf("g2s", [128, CW], F32)
    k.dma(gix[:], gid, writes=[gix]); k.dma(g2[:], g2d, writes=[g2])
    NG = 6
    breg = nc.gpsimd.to_reg(ZROW - 1)
    zt = k.sbuf("zt", [128, CW], F32)
    k.op("dve", lambda e: e.memset(zt[:], 0.0), writes=[zt])
    G = [k.sbuf("G%d" % i, [128, CW], F32) for i in range(NG)]
    xt = [k.sbuf("xt%d" % i, [128, CW], F32) for i in range(2)]
    acc = [k.sbuf("acc%d" % i, [128, CW], F32) for i in range(2)]
    gi = 0
    for tt in range(NTT):
        x_ = xt[tt % 2]; a_ = acc[tt % 2]
        k.dma(x_[:], x1d[tt * 128:(tt + 1) * 128, :], writes=[x_])
        for e_ in range(16):
            g_ = G[gi % NG]; gi += 1
            col = tt * 16 + e_
            en = k.engs["pool"]
            k._deps(en, [gix], [g_])
            k.op("act", lambda e: e.activation(out=g_[:], in_=zt[:], func=ACT.Copy), reads=[zt], writes=[g_])
            k._deps(en, [gix], [g_])
            ins = nc.gpsimd.indirect_dma_start(out=g_[:], out_offset=None, in_=yed,
                                               in_offset=bass.IndirectOffsetOnAxis(ap=gix[:, col:col + 1].bitcast(U32), axis=0),
                                               bounds_check=breg, oob_is_err=False)
            ins.then_inc(g_.sem(), 16)
            g_.dcount += 1
            dep = ("dma", g_, g_.dcount)
            gix.readers.append(dep); g_.writer = dep; g_.readers = []
            if e_ == 0:
                k.op("dve", lambda e: e.tensor_copy(out=a_[:], in_=g_[:]), reads=[g_], writes=[a_])
            else:
                k.op("dve", lambda e: e.tensor_tensor(out=a_[:], in0=a_[:], in1=g_[:], op=ALU.add), reads=[a_, g_], writes=[a_])
        k.op("dve", lambda e: e.tensor_tensor(out=a_[:], in0=a_[:], in1=g2[:], op=ALU.mult), reads=[a_, g2], writes=[a_])
        k.op("pool", lambda e: e.tensor_tensor(out=a_[:], in0=a_[:], in1=x_[:], op=ALU.add), reads=[a_, x_], writes=[a_])
        k.dma(x2d[tt * 128:(tt + 1) * 128, :], a_[:], reads=[a_], writes=[xR])
    k.finish("sp"); k.close()
    return nc


BF = ml_dtypes.bfloat16
_PROGS = {}


def _prog(name, fn):
    if name not in _PROGS:
        _PROGS[name] = fn()
    return _PROGS[name]


def _col(v):
    return np.ascontiguousarray(np.asarray(v, np.float32).reshape(32, 128).T)


def _rep(v):
    v = np.asarray(v, np.float32)
    return np.ascontiguousarray(np.broadcast_to(v[None, :], (128, v.shape[0])))


def kernel(x, c, positions, norm1_gain, norm2_gain, w_ada, b_ada, w_in, q_gain, k_gain,
           out_gain_fourier, out_gain_attn, w_out, w_router, w_gate, w_up, w_down):
    f32 = np.float32
    x = np.asarray(x, f32); c = np.asarray(c, f32); positions = np.asarray(positions, np.int32)
    norm1_gain = np.asarray(norm1_gain, f32); norm2_gain = np.asarray(norm2_gain, f32)
    w_ada = np.asarray(w_ada, f32); b_ada = np.asarray(b_ada, f32); w_in = np.asarray(w_in, f32)
    q_gain = np.asarray(q_gain, f32); k_gain = np.asarray(k_gain, f32)
    out_gain_fourier = np.asarray(out_gain_fourier, f32); out_gain_attn = np.asarray(out_gain_attn, f32)
    w_out = np.asarray(w_out, f32); w_router = np.asarray(w_router, f32)
    w_gate = np.asarray(w_gate, f32); w_up = np.asarray(w_up, f32); w_down = np.asarray(w_down, f32)
    B, S, D = x.shape
    NC = 8
    ident = np.eye(128, dtype=f32)

    cT = np.ascontiguousarray(c.T.reshape(32, 128, 2).transpose(1, 0, 2).reshape(128, 64))
    ims = []
    for cc in range(NC):
        sl = slice(cc * 3072, (cc + 1) * 3072)
        ims.append({"cT": cT, "wa": np.ascontiguousarray(w_ada[:, :, sl]),
                    "ba": np.ascontiguousarray(b_ada[:, sl]).reshape(1, -1)})
    res = run(_prog("p0", build_p0), ims)
    mod = np.zeros((2, 2, 6 * D), f32)
    for cc in range(NC):
        o = res.results[cc]["mod"].reshape(2, 2, 3072)
        mod[:, :, cc * 3072:(cc + 1) * 3072] = o.transpose(1, 0, 2)
    del ims, res

    ii = np.arange(128)
    maskc = np.concatenate([(ii[:, None] >= ii[None, :]), (ii[:, None] <= ii[None, :])], axis=1).astype(BF)
    d64, gab, cs = fourier_tables(S)
    d64 = d64.astype(BF); gab = gab.astype(BF); cs = cs.astype(BF)
    d1c = d1_consts()
    TPC = (B * S) // NC
    CPB = NC // B

    for l in range(2):
        shift1, scale1, gate1, shift2, scale2, gate2 = [mod[l][:, i * D:(i + 1) * D] for i in range(6)]
        ims = []
        for cc in range(NC):
            b = cc // CPB; t0 = (cc % CPB) * TPC
            ims.append(host_inputs_pa(np.ascontiguousarray(x[b, t0:t0 + TPC]), norm1_gain[l], scale1[b], shift1[b], w_in[l],
                                      q_gain[l], k_gain[l], positions[b, t0:t0 + TPC]))
        res = run(_prog("pa", build_pa), ims)
        U = np.stack([res.results[cc]["u"] for cc in range(NC)], 0).reshape(B, S, 10240)
        del ims, res
        ims = []
        for cc in range(NC):
            qT = np.zeros((6, 128, S), BF); kT = np.zeros((6, 128, S + 2048), BF); vp = np.zeros((6, S + 2112, 129), BF)
            ga = np.zeros((6, 128, 128), f32)
            for b in range(B):
                for hh in range(3):
                    h = 3 * cc + hh; u = b * 3 + hh
                    qT[u] = U[b, :, 1024 + h * 128:1024 + (h + 1) * 128].T
                    kT[u, :, 1024:1024 + S] = U[b, :, 4096 + h * 128:4096 + (h + 1) * 128].T
                    vp[u, 1024:1024 + S, :128] = U[b, :, 7168 + h * 128:7168 + (h + 1) * 128]
                    vp[u, 1024:1024 + S, 128] = 1.0
                    ga[u] = _rep(out_gain_attn[l][h * 128:(h + 1) * 128])
            uf = np.stack([np.ascontiguousarray(U[b, :, cc * 128:(cc + 1) * 128]).reshape(S // 128, 128 * 128) for b in range(B)], 0)
            gf = np.stack([_rep(out_gain_fourier[l][cc * 128:(cc + 1) * 128])] * B, 0)
            ims.append({"qT": qT, "kT": kT, "vp": vp, "ga": ga, "mask": maskc, "uf": uf, "d64": d64, "gab": gab, "cs": cs, "gf": gf})
        res = run(_prog("pb", build_pb), ims)
        y = np.zeros((B, S, D), BF)
        for cc in range(NC):
            ya = res.results[cc]["ya"]; yf = res.results[cc]["yf"]
            for b in range(B):
                y[b, :, cc * 128:(cc + 1) * 128] = yf[b]
                for hh in range(3):
                    h = 3 * cc + hh
                    y[b, :, 1024 + h * 128:1024 + (h + 1) * 128] = ya[b * 3 + hh]
        del ims, res, U
        wr = np.ascontiguousarray(w_router[l].reshape(32, 128, 16).transpose(1, 0, 2).reshape(128, 512))
        ims = []
        for cc in range(NC):
            b = cc // CPB; t0 = (cc % CPB) * TPC
            ims.append({"yT": np.ascontiguousarray(y[b, t0:t0 + TPC].T), "x": np.ascontiguousarray(x[b, t0:t0 + TPC]), "w": w_out[l],
                        "g1": _rep(gate1[b]), "cols": np.concatenate([_col(norm2_gain[l]), _col(scale2[b]), _col(shift2[b])], 1),
                        "wr": wr, "ident": ident})
        res = run(_prog("pc", build_pc), ims)
        x1 = np.stack([res.results[cc]["x1"] for cc in range(NC)], 0).reshape(B, S, D)
        aff = np.stack([res.results[cc]["aff"] for cc in range(NC)], 0).reshape(B, S, 16)
        del ims, res, y
        ims = []
        for cc in range(NC):
            affu = np.zeros((128, 4, 64), f32); eo = np.zeros(4, f32)
            for u in range(4):
                e = 2 * cc + u // 2; b = u % 2
                affu[:, u, :] = aff[b, :, e].reshape(128, 64)
                eo[u] = e * 1024 - ZROW
            ims.append({"affu": affu.reshape(128, 256), "eoff": _rep(eo), **d1c})
        res = run(_prog("pd1", build_pd1), ims)
        gfull = np.full((B, S, 16), ZROW, np.int32)
        idxs = {}
        for cc in range(NC):
            g = res.results[cc]["gidx"].reshape(128, 4, 64); m = res.results[cc]["msk"].reshape(128, 4, 64)
            for u in range(4):
                e = 2 * cc + u // 2; b = u % 2
                gfull[b, :, e] = g[:, u, :].reshape(S)
                sel = np.flatnonzero(m[:, u, :].reshape(S) > 0.5)[:1024]
                if sel.shape[0] < 1024:
                    sel = np.concatenate([sel, np.zeros(1024 - sel.shape[0], sel.dtype)])
                idxs[(b, e)] = sel
        del ims, res
        ims = []
        for cc in range(NC):
            xe = np.zeros((4, 1024, D), f32); affs = np.zeros((4, 128, 8), f32); cols = np.zeros((4, 128, 96), f32)
            for u in range(4):
                e = 2 * cc + u // 2; b = u % 2
                sel = idxs[(b, e)]
                xe[u] = x1[b, sel]
                affs[u] = aff[b, sel, e].reshape(8, 128).T
                cols[u] = np.concatenate([_col(norm2_gain[l]), _col(scale2[b]), _col(shift2[b])], 1)
            ims.append({"xe": xe, "affs": affs, "cols": cols, "wg": np.ascontiguousarray(w_gate[l, 2 * cc:2 * cc + 2]),
                        "wu": np.ascontiguousarray(w_up[l, 2 * cc:2 * cc + 2]), "wd": np.ascontiguousarray(w_down[l, 2 * cc:2 * cc + 2]),
                        "ident": ident})
        res = run(_prog("pd2", build_pd2), ims)
        yeall = np.zeros((B, ZROW + 1, D), f32)
        for cc in range(NC):
            ye = res.results[cc]["ye"]
            for u in range(4):
                e = 2 * cc + u // 2; b = u % 2
                yeall[b, e * 1024:(e + 1) * 1024] = ye[u]
        del ims, res
        ims = []
        for cc in range(NC):
            b = cc // CPB; th = (cc % CPB) // 2; ch = cc % 2
            ts_ = slice(th * 4096, (th + 1) * 4096); cs_ = slice(ch * 2048, (ch + 1) * 2048)
            gi = np.ascontiguousarray(gfull[b][ts_].reshape(4096 // 128, 128, 16).transpose(1, 0, 2).reshape(128, -1))
            ims.append({"x1c": np.ascontiguousarray(x1[b][ts_, cs_]), "yec": np.ascontiguousarray(yeall[b][:, cs_]), "gidx": gi,
                        "g2": _rep(gate2[b][cs_])})
        res = run(_prog("pe", build_pe), ims)
        xn = np.zeros((B, S, D), f32)
        for cc in range(NC):
            b = cc // CPB; th = (cc % CPB) // 2; ch = cc % 2
            xn[b][th * 4096:(th + 1) * 4096, ch * 2048:(ch + 1) * 2048] = res.results[cc]["x2c"]
        del ims, res, x1, yeall
        x = xn
    return x
```

```python
import math
import numpy as np
import ml_dtypes
import concourse.bass as bass
import concourse.mybir as mybir
from concourse.bass_utils import run_bass_kernel_spmd

F32 = mybir.dt.float32
BF16 = mybir.dt.bfloat16
I32 = mybir.dt.int32
U32 = mybir.dt.uint32
ALU = mybir.AluOpType
ACT = mybir.ActivationFunctionType
AX = mybir.AxisListType


class Res:
    def __init__(self, k, name, t=None, own_sem=True):
        self.k = k
        self.name = name
        self.t = t
        self.writer = None
        self.readers = []
        self.dsem = None
        self.dcount = 0

    def __getitem__(self, idx):
        return self.t[idx]

    def sem(self):
        if self.dsem is None:
            self.dsem = self.k.new_sem("d_" + self.name)
        return self.dsem


class Eng:
    def __init__(self, k, name, h):
        self.k = k
        self.name = name
        self.h = h
        self.sem = k.new_sem("e_" + name)
        self.count = 0
        self.seen = {}
        self.seen_d = {}


class K:
    def __init__(self, nc):
        self.nc = nc
        self._ctx = []
        self.nsem = 0
        self.engs = {}
        for name, h in (("pe", nc.tensor), ("act", nc.scalar), ("dve", nc.vector),
                        ("pool", nc.gpsimd), ("sp", nc.sync)):
            self.engs[name] = Eng(self, name, h)
        self.all_res = []

    def new_sem(self, name):
        cm = self.nc.semaphore(name + "_%d" % self.nsem)
        self.nsem += 1
        s = cm.__enter__()
        self._ctx.append(cm)
        return s

    def sbuf(self, name, shape, dtype):
        cm = self.nc.sbuf_tensor(name, list(shape), dtype)
        t = cm.__enter__()
        self._ctx.append(cm)
        r = Res(self, name, t)
        self.all_res.append(r)
        return r

    def psum(self, name, shape, dtype):
        cm = self.nc.psum_tensor(name, list(shape), dtype)
        t = cm.__enter__()
        self._ctx.append(cm)
        r = Res(self, name, t)
        self.all_res.append(r)
        return r

    def dram(self, name, ap):
        r = Res(self, name, ap)
        self.all_res.append(r)
        return r

    def close(self):
        for cm in reversed(self._ctx):
            cm.__exit__(None, None, None)
        self._ctx = []

    def _wait(self, e, dep, raw):
        if dep is None:
            return
        if dep[0] == "eng":
            _, fname, seq = dep
            if fname == e.name:
                if not raw or e.name in ("pe", "sp"):
                    return
            if e.seen.get(fname, 0) >= seq:
                return
            e.h.wait_ge(self.engs[fname].sem, seq)
            e.seen[fname] = seq
        else:
            _, res, cnt = dep
            if e.seen_d.get(id(res), 0) >= cnt:
                return
            e.h.wait_ge(res.sem(), 16 * cnt)
            e.seen_d[id(res)] = cnt

    def _deps(self, e, reads, writes):
        for r in reads:
            self._wait(e, r.writer, True)
        for w in writes:
            self._wait(e, w.writer, False)
            for d in w.readers:
                self._wait(e, d, False)

    def op(self, ename, fn, reads=(), writes=(), signal=True):
        e = self.engs[ename]
        self._deps(e, reads, writes)
        ins = fn(e.h)
        seq = e.count + 1
        if signal:
            ins.then_inc(e.sem, 1)
            e.count = seq
        dep = ("eng", ename, seq)
        for r in reads:
            r.readers.append(dep)
        for w in writes:
            w.writer = dep
            w.readers = []
        return ins

    def dma(self, out_ap, in_ap, reads=(), writes=(), semres=None, q="sp", **kw):
        e = self.engs[q]
        self._deps(e, reads, writes)
        sr = semres if semres is not None else (list(writes) + list(reads))[0]
        ins = e.h.dma_start(out=out_ap, in_=in_ap, **kw)
        ins.then_inc(sr.sem(), 16)
        sr.dcount += 1
        dep = ("dma", sr, sr.dcount)
        for r in reads:
            r.readers.append(dep)
        for w in writes:
            w.writer = dep
            w.readers = []
        return ins

    def finish(self, ename="sp"):
        e = self.engs[ename]
        for r in self.all_res:
            if r.dcount:
                self._wait(e, ("dma", r, r.dcount), True)
        for n, f in self.engs.items():
            if n != ename and f.count:
                self._wait(e, ("eng", n, f.count), True)

    def barrier(self):
        for n in self.engs:
            self.finish(n)


def run(nc, in_maps, n=8, trace=False):
    return run_bass_kernel_spmd(nc, in_maps, core_ids=list(range(n)), trace=trace)


def build_p0():
    nc = bass.Bass("TRN2", target_bir_lowering=False)
    NCOL = 3072
    cT = nc.dram_tensor("cT", [128, 64], F32, kind="ExternalInput").ap()
    wa = nc.dram_tensor("wa", [2, 4096, NCOL], F32, kind="ExternalInput").ap()
    ba = nc.dram_tensor("ba", [1, 2 * NCOL], F32, kind="ExternalInput").ap()
    mod = nc.dram_tensor("mod", [2, 2 * NCOL], F32, kind="ExternalOutput").ap()
    k = K(nc)
    ct = k.sbuf("ct", [128, 64], F32)
    sc = k.sbuf("sc", [128, 64], F32)
    ones = k.sbuf("ones", [1, 2], F32)
    bt = k.sbuf("bt", [1, 2 * NCOL], F32)
    ob = k.sbuf("ob", [2, 2 * NCOL], F32)
    wt = [k.sbuf("wt%d" % i, [128, 32, 512], F32) for i in range(2)]
    ps = [k.psum("ps%d" % i, [2, 512], F32) for i in range(2)]
    k.dma(ct[:], cT, writes=[ct])
    k.dma(bt[:], ba, writes=[bt])
    k.op("act", lambda e: e.activation(out=sc[:], in_=ct[:], func=ACT.Silu), reads=[ct], writes=[sc])
    k.op("dve", lambda e: e.memset(ones[:], 1.0), writes=[ones])
    it = 0
    for l in range(2):
        for nb in range(NCOL // 512):
            w = wt[it % 2]; p = ps[it % 2]
            src = wa[l, :, nb * 512:(nb + 1) * 512].rearrange("(kc p) n -> p kc n", p=128)
            k.dma(w[:, 0:16, :], src[:, 0:16, :], writes=[w])
            k.dma(w[:, 16:32, :], src[:, 16:32, :], writes=[w])
            for kc in range(32):
                k.op("pe", lambda e: e.matmul(p[:], lhsT=sc[:, 2 * kc:2 * kc + 2], rhs=w[:, kc, :],
                                              start=(kc == 0), stop=False),
                     reads=[sc, w], writes=[p], signal=False)
            col = l * NCOL + nb * 512
            k.op("pe", lambda e: e.matmul(p[:], lhsT=ones[:], rhs=bt[:, col:col + 512], start=False, stop=True),
                 reads=[ones, bt], writes=[p])
            k.op("dve", lambda e: e.tensor_copy(out=ob[:, col:col + 512], in_=p[:]), reads=[p], writes=[ob])
            it += 1
    k.dma(mod, ob[:], reads=[ob])
    k.finish("sp")
    k.close()
    return nc


EPS = 1e-6

def nta_stage1(k, xt, tmp):
    ssq, rs, xs = tmp["ssq"], tmp["rs"], tmp["xs"]
    k.op("act", lambda e: e.activation(out=xs[:].bitcast(BF16)[:, 0:4096], in_=xt[:], func=ACT.Square, accum_out=ssq[:]),
         reads=[xt], writes=[xs, ssq])
    k.op("dve", lambda e: e.tensor_scalar(out=rs[:], in0=ssq[:], scalar1=1.0 / 4096, scalar2=EPS,
                                          op0=ALU.mult, op1=ALU.add), reads=[ssq], writes=[rs])
    k.op("act", lambda e: e.activation(out=rs[:], in_=rs[:], func=ACT.Sqrt), reads=[rs], writes=[rs])
    k.op("dve", lambda e: e.reciprocal(out=rs[:], in_=rs[:]), reads=[rs], writes=[rs])
    k.op("dve", lambda e: e.tensor_scalar(out=xs[:], in0=xt[:], scalar1=rs[:, 0:1], scalar2=None, op0=ALU.mult),
         reads=[xt, rs], writes=[xs])


def nta_stage2(k, ident, Gc, Sc, dst, dst_cols, pT, tmp, dst32=None):
    xs = tmp["xs"]
    for g in range(8):
        p = pT[g % 2]
        for j in range(4):
            kc = 4 * g + j
            k.op("pe", lambda e: e.transpose(out=p[:, j, :], in_=xs[:, kc * 128:(kc + 1) * 128], identity=ident[:]),
                 reads=[xs, ident], writes=[p], signal=(j == 3))
        for j in range(4):
            kc = 4 * g + j
            eng = "dve" if (j % 2 == 0) else "pool"
            if dst is None:
                pass
            elif eng == "pool":
                k.op("act", lambda e: e.activation(out=dst[:, kc, dst_cols], in_=p[:, j, :], func=ACT.Identity,
                                                   scale=Gc[:, kc:kc + 1], bias=Sc[:, kc:kc + 1]),
                     reads=[p, Gc, Sc], writes=[dst])
            else:
                k.op("dve", lambda e: e.tensor_scalar(out=dst[:, kc, dst_cols], in0=p[:, j, :],
                                                      scalar1=Gc[:, kc:kc + 1], scalar2=Sc[:, kc:kc + 1],
                                                      op0=ALU.mult, op1=ALU.add),
                     reads=[p, Gc, Sc], writes=[dst])
            if dst32 is not None:
                k.op("act", lambda e: e.activation(out=dst32[:, kc, :], in_=p[:, j, :], func=ACT.Identity,
                                                   scale=Gc[:, kc:kc + 1], bias=Sc[:, kc:kc + 1]),
                     reads=[p, Gc, Sc], writes=[dst32])


def norm_transpose_affine(k, xt, ident, Gc, Sc, dst, dst_cols, pT, tmp, dst32=None):
    nta_stage1(k, xt, tmp)
    nta_stage2(k, ident, Gc, Sc, dst, dst_cols, pT, tmp, dst32=dst32)


def load_w_stage(k, wsrc, stage, Wb, it, s):
    src = wsrc.rearrange("(kc p) n -> p kc n", p=128)
    st = stage[(it * 8 + s) % len(stage)]
    k.dma(st[:], src[:, 4 * s:4 * s + 4, :], writes=[st])
    eng = ("dve", "pool", "act", "pool")[s % 4]
    if eng == "act":
        k.op("act", lambda e: e.activation(out=Wb[:, 4 * s:4 * s + 4, :], in_=st[:], func=ACT.Copy),
             reads=[st], writes=[Wb])
    else:
        k.op(eng, lambda e: e.tensor_copy(out=Wb[:, 4 * s:4 * s + 4, :], in_=st[:]), reads=[st], writes=[Wb])


def load_w_block(k, wsrc, stage, Wb, it):
    for s in range(8):
        load_w_stage(k, wsrc, stage, Wb, it, s)


def build_pa(NT=2048, HALF=1024, NCB=20):
    nc = bass.Bass("TRN2", target_bir_lowering=False)
    NTT = NT // 128
    x = nc.dram_tensor("x", [NT, 4096], F32, kind="ExternalInput").ap()
    cols = nc.dram_tensor("cols", [128, 96], F32, kind="ExternalInput").ap()
    w = nc.dram_tensor("w", [4096, NCB * 512], F32, kind="ExternalInput").ap()
    qkg = nc.dram_tensor("qkg", [128, 256], F32, kind="ExternalInput").ap()
    pos = nc.dram_tensor("pos", [128, NTT], I32, kind="ExternalInput").ap()
    invf = nc.dram_tensor("invf", [128, 16], F32, kind="ExternalInput").ap()
    identd = nc.dram_tensor("ident", [128, 128], F32, kind="ExternalInput").ap()
    u = nc.dram_tensor("u", [NT, NCB * 512], BF16, kind="ExternalOutput").ap()
    wbf = nc.dram_tensor("wbf", [NCB, 128, 32 * 512], BF16, kind="Internal").ap()
    k = K(nc)
    wbfR = [k.dram("wbfR%d" % i, wbf[i]) for i in range(NCB)]
    colt = k.sbuf("colt", [128, 96], F32)
    Gc = k.sbuf("Gc", [128, 32], F32)
    qk = k.sbuf("qk", [128, 256], F32)
    post = k.sbuf("post", [128, NTT], I32)
    posf = k.sbuf("posf", [128, NTT], F32)
    invt = k.sbuf("invt", [128, 16], F32)
    ident = k.sbuf("ident_s", [128, 128], F32)
    cosA = k.sbuf("cosA", [128, NTT, 16], F32)
    sinA = k.sbuf("sinA", [128, NTT, 16], F32)
    ang = k.sbuf("ang", [128, 16], F32)
    hT = [k.sbuf("hT%d" % i, [128, 32, 128], BF16) for i in range(HALF // 128)]
    Wb = [k.sbuf("Wb%d" % i, [128, 32, 512], BF16) for i in range(2)]
    stage = [k.sbuf("stg%d" % i, [128, 4, 512], F32) for i in range(2)]
    xt = k.sbuf("xt", [128, 4096], F32)
    tmp = {"ssq": k.sbuf("ssq", [128, 1], F32), "rs": k.sbuf("rs", [128, 1], F32),
           "xs": k.sbuf("xs", [128, 4096], F32), "junk": None}
    pT = [k.psum("pT%d" % i, [128, 4, 128], F32) for i in range(2)]
    ps = [k.psum("ps%d" % i, [128, 512], F32) for i in range(3)]
    sq = k.sbuf("sq", [128, 512], BF16)
    s4 = k.sbuf("s4", [128, 4], F32)
    t1 = k.sbuf("t1", [128, 4, 128], F32)
    t2 = k.sbuf("t2", [128, 4, 128], F32)
    ra = k.sbuf("ra", [128, 4, 16], F32)
    rb = k.sbuf("rb", [128, 4, 16], F32)
    rc = k.sbuf("rc", [128, 4, 16], F32)
    rd = k.sbuf("rd", [128, 4, 16], F32)
    ob = [k.sbuf("ob%d" % i, [128, 512], BF16) for i in range(3)]
    udram = k.dram("udram", u)

    k.dma(colt[:], cols, writes=[colt])
    k.dma(qk[:], qkg, writes=[qk])
    k.dma(post[:], pos, writes=[post])
    k.dma(invt[:], invf, writes=[invt])
    k.dma(ident[:], identd, writes=[ident])
    k.op("dve", lambda e: e.tensor_scalar(out=Gc[:], in0=colt[:, 32:64], scalar1=1.0, scalar2=None, op0=ALU.add),
         reads=[colt], writes=[Gc])
    k.op("dve", lambda e: e.tensor_tensor(out=Gc[:], in0=Gc[:], in1=colt[:, 0:32], op=ALU.mult),
         reads=[Gc, colt], writes=[Gc])
    Sc = colt
    class _S:
        def __getitem__(self, idx):
            return colt.t[idx[0], slice(64 + idx[1].start, 64 + idx[1].stop)]
    k.op("dve", lambda e: e.tensor_scalar(out=qk[:, 0:128], in0=qk[:, 0:128], scalar1=128.0 ** -0.5, scalar2=None,
                                          op0=ALU.mult), reads=[qk], writes=[qk])
    k.op("dve", lambda e: e.tensor_copy(out=posf[:], in_=post[:]), reads=[post], writes=[posf])
    angA = k.sbuf("angA", [128, NTT, 16], F32)
    aA = k.sbuf("aA", [128, NTT, 16], F32)
    kI = k.sbuf("kI", [128, NTT, 16], I32)
    kF = k.sbuf("kF", [128, NTT, 16], F32)
    C1 = 6.28125
    C2 = 2.0 * math.pi - C1
    k.op("dve", lambda e: e.tensor_tensor(out=angA[:], in0=posf[:].unsqueeze(2).broadcast_to([128, NTT, 16]),
                                          in1=invt[:].unsqueeze(1).broadcast_to([128, NTT, 16]), op=ALU.mult),
         reads=[posf, invt], writes=[angA])
    for (dstT, off) in ((cosA, 0.5 * math.pi), (sinA, 0.0)):
        k.op("dve", lambda e: e.tensor_scalar(out=aA[:], in0=angA[:], scalar1=off, scalar2=None, op0=ALU.add),
             reads=[angA], writes=[aA])
        k.op("dve", lambda e: e.tensor_scalar(out=kI[:], in0=aA[:], scalar1=1.0 / (2.0 * math.pi), scalar2=None, op0=ALU.mult),
             reads=[aA], writes=[kI])
        k.op("dve", lambda e: e.tensor_copy(out=kF[:], in_=kI[:]), reads=[kI], writes=[kF])
        k.op("dve", lambda e: e.scalar_tensor_tensor(out=aA[:], in0=kF[:], scalar=-C1, in1=aA[:], op0=ALU.mult, op1=ALU.add),
             reads=[kF, aA], writes=[aA])
        k.op("dve", lambda e: e.scalar_tensor_tensor(out=aA[:], in0=kF[:], scalar=-C2, in1=aA[:], op0=ALU.mult, op1=ALU.add),
             reads=[kF, aA], writes=[aA])
        k.op("dve", lambda e: e.tensor_scalar(out=aA[:], in0=aA[:], scalar1=-math.pi, scalar2=math.pi, op0=ALU.max, op1=ALU.min),
             reads=[aA], writes=[aA])
        k.op("act", lambda e: e.activation(out=dstT[:], in_=aA[:], func=ACT.Sin), reads=[aA], writes=[dstT])
    ShiftView = _S()
    class _SRes:
        pass
    it = 0
    pi = 0
    oi = 0
    for half in range(NT // HALF):
        for tt in range(HALF // 128):
            gt = half * (HALF // 128) + tt
            k.dma(xt[:], x[gt * 128:(gt + 1) * 128, :], writes=[xt])
            norm_transpose_affine(k, xt, ident, Gc, _ColView(colt, 64), hT[tt], slice(0, 128), pT, tmp)
        for cb in range(NCB):
            W = Wb[it % 2]
            if it == 0:
                load_w_block(k, w[:, cb * 512:(cb + 1) * 512], stage, W, it)
                k.dma(wbf[cb], W[:].rearrange("p a b -> p (a b)"), reads=[W], writes=[wbfR[cb]], q="act")
            it += 1
            nxt = it if it < (NT // HALF) * NCB else None
            nparts = HALF // 128
            kind = "plain"
            if NCB == 20:
                if 2 <= cb < 8: kind = "q"
                elif 8 <= cb < 14: kind = "k"
            else:
                kind = ("plain", "q", "k", "plain")[cb % 4]
            for tt in range(HALF // 128):
                gt = half * (HALF // 128) + tt
                p = ps[pi % 3]; pi += 1
                for kc in range(32):
                    k.op("pe", lambda e: e.matmul(p[:], lhsT=hT[tt][:, kc, :], rhs=W[:, kc, :],
                                                  start=(kc == 0), stop=(kc == 31)),
                         reads=[hT[tt], W], writes=[p], signal=(kc == 31))
                if nxt is not None:
                    ncb = nxt % NCB
                    if nxt < NCB:
                        for s_ in range(8):
                            if s_ * nparts // 8 == tt:
                                load_w_stage(k, w[:, ncb * 512:(ncb + 1) * 512], stage, Wb[nxt % 2], nxt, s_)
                                if s_ == 7:
                                    k.dma(wbf[ncb], Wb[nxt % 2][:].rearrange("p a b -> p (a b)"), reads=[Wb[nxt % 2]],
                                          writes=[wbfR[ncb]], q="act")
                    elif tt == 0:
                        k.dma(Wb[nxt % 2][:].rearrange("p a b -> p (a b)"), wbf[ncb], reads=[wbfR[ncb]], writes=[Wb[nxt % 2]])
                o = ob[oi % 3]; oi += 1
                if kind == "plain":
                    k.op("act", lambda e: e.activation(out=o[:], in_=p[:], func=ACT.Copy), reads=[p], writes=[o])
                else:
                    g0 = 0 if kind == "q" else 128
                    pv = p[:].rearrange("p (h d) -> p h d", h=4)
                    k.op("act", lambda e: e.activation(out=sq[:], in_=p[:], func=ACT.Square), reads=[p], writes=[sq])
                    k.op("dve", lambda e: e.tensor_reduce(out=s4[:], in_=sq[:].rearrange("p (h d) -> p h d", h=4),
                                                          axis=AX.X, op=ALU.add), reads=[sq], writes=[s4])
                    k.op("dve", lambda e: e.tensor_scalar(out=s4[:], in0=s4[:], scalar1=1.0 / 128, scalar2=EPS,
                                                          op0=ALU.mult, op1=ALU.add), reads=[s4], writes=[s4])
                    k.op("act", lambda e: e.activation(out=s4[:], in_=s4[:], func=ACT.Sqrt), reads=[s4], writes=[s4])
                    k.op("dve", lambda e: e.reciprocal(out=s4[:], in_=s4[:]), reads=[s4], writes=[s4])
                    k.op("dve", lambda e: e.tensor_tensor(out=t1[:], in0=pv, in1=s4[:].unsqueeze(2).broadcast_to([128, 4, 128]),
                                                          op=ALU.mult), reads=[p, s4], writes=[t1])
                    k.op("pool", lambda e: e.tensor_tensor(out=t2[:], in0=t1[:],
                                                           in1=qk[:, g0:g0 + 128].unsqueeze(1).broadcast_to([128, 4, 128]),
                                                           op=ALU.mult), reads=[t1, qk], writes=[t2])
                    cb_ = cosA[:, gt, :].unsqueeze(1).broadcast_to([128, 4, 16])
                    sb_ = sinA[:, gt, :].unsqueeze(1).broadcast_to([128, 4, 16])
                    A = t2[:, :, 0:16]; B = t2[:, :, 16:32]
                    k.op("dve", lambda e: e.tensor_tensor(out=ra[:], in0=A, in1=cb_, op=ALU.mult), reads=[t2, cosA], writes=[ra])
                    k.op("pool", lambda e: e.tensor_tensor(out=rb[:], in0=B, in1=sb_, op=ALU.mult), reads=[t2, sinA], writes=[rb])
                    k.op("dve", lambda e: e.tensor_tensor(out=rc[:], in0=B, in1=cb_, op=ALU.mult), reads=[t2, cosA], writes=[rc])
                    k.op("pool", lambda e: e.tensor_tensor(out=rd[:], in0=A, in1=sb_, op=ALU.mult), reads=[t2, sinA], writes=[rd])
                    k.op("dve", lambda e: e.tensor_tensor(out=t2[:, :, 0:16], in0=ra[:], in1=rb[:], op=ALU.subtract),
                         reads=[ra, rb], writes=[t2])
                    k.op("dve", lambda e: e.tensor_tensor(out=t2[:, :, 16:32], in0=rc[:], in1=rd[:], op=ALU.add),
                         reads=[rc, rd], writes=[t2])
                    k.op("act", lambda e: e.activation(out=o[:], in_=t2[:].rearrange("p h d -> p (h d)"), func=ACT.Copy),
                         reads=[t2], writes=[o])
                k.dma(u[gt * 128:(gt + 1) * 128, cb * 512:(cb + 1) * 512], o[:], reads=[o], writes=[udram], q="act")
    k.finish("sp")
    k.close()
    return nc


class _ColView:
    def __init__(self, res, off):
        self.res = res; self.off = off
        self.__dict__["_r"] = res
    def __getitem__(self, idx):
        a, b = idx
        return self.res.t[a, slice(self.off + b.start, self.off + b.stop)]
    def __getattr__(self, n):
        return getattr(self.__dict__["_r"], n)
    def __setattr__(self, n, v):
        if n in ("res", "off"):
            self.__dict__[n] = v
        else:
            setattr(self.__dict__["_r"], n, v)


def host_inputs_pa(xc, gain1, scale1, shift1, w_in, q_gain, k_gain, positions_c):
    def col(v): return np.ascontiguousarray(v.reshape(32, 128).T)
    cols = np.concatenate([col(gain1), col(scale1), col(shift1)], axis=1).astype(np.float32)
    qkg = np.concatenate([np.tile(q_gain[None, :], (128, 1)), np.tile(k_gain[None, :], (128, 1))], axis=1).astype(np.float32)
    NT = xc.shape[0]
    pos = np.ascontiguousarray(positions_c.reshape(NT // 128, 128).T).astype(np.int32)
    invf = (np.float32(500000.0) ** (-np.arange(16, dtype=np.float32) * np.float32(2.0 / 32))).astype(np.float32)
    return {"x": xc, "cols": cols, "w": w_in, "qkg": qkg, "pos": pos,
            "invf": np.tile(invf[None, :], (128, 1)), "ident": np.eye(128, dtype=np.float32)}


PATS = (1, 4, 16)

def build_pb(S=8192, NU=6, NF=2):
    nc = bass.Bass("TRN2", target_bir_lowering=False)
    SP = S + 2112
    qTd = nc.dram_tensor("qT", [NU, 128, S], BF16, kind="ExternalInput").ap()
    kTd = nc.dram_tensor("kT", [NU, 128, S + 2048], BF16, kind="ExternalInput").ap()
    vpd = nc.dram_tensor("vp", [NU, SP, 129], BF16, kind="ExternalInput").ap()
    gad = nc.dram_tensor("ga", [NU, 128, 128], F32, kind="ExternalInput").ap()
    maskd = nc.dram_tensor("mask", [128, 256], BF16, kind="ExternalInput").ap()
    yad = nc.dram_tensor("ya", [NU, S, 128], BF16, kind="ExternalOutput").ap()
    NB = S // 128
    N1 = S // 128
    ufd = nc.dram_tensor("uf", [NF, N1, 128 * 128], BF16, kind="ExternalInput").ap()
    d64d = nc.dram_tensor("d64", [N1, 2 * N1], BF16, kind="ExternalInput").ap()
    gabd = nc.dram_tensor("gab", [2, 128, N1, 256], BF16, kind="ExternalInput").ap()
    csd = nc.dram_tensor("cs", [128, 256], BF16, kind="ExternalInput").ap()
    gfd = nc.dram_tensor("gf", [NF, 128, 128], F32, kind="ExternalInput").ap()
    yfd = nc.dram_tensor("yf", [NF, S, 128], BF16, kind="ExternalOutput").ap()
    accd = [nc.dram_tensor("acc%d" % i, [3, S, 129], F32, kind="Internal").ap() for i in range(2)]
    k = K(nc)
    accR = [k.dram("accR%d" % i, accd[i]) for i in range(2)]
    yaR = k.dram("yaR", yad); yfR = k.dram("yfR", yfd)
    mask = k.sbuf("mask_s", [128, 256], BF16)
    k.dma(mask[:], maskd, writes=[mask])
    qT = k.sbuf("qTs", [128, S], BF16)
    kT = k.sbuf("kTs", [128, S + 2048], BF16)
    NVT = max((S // (128 * d) + 1) * d for d in PATS)
    Vb = [k.sbuf("Vb%d" % i, [128, NVT, 129], BF16) for i in range(2)]
    gaL = [k.sbuf("ga_s%d" % i, [128, 128], F32) for i in range(2)]
    st = [k.psum("st%d" % i, [128, 512], F32) for i in range(2)]
    ops = [k.psum("o%d" % i, [128, 2, 129], F32) for i in range(2)]
    pt = [k.sbuf("pt%d" % i, [128, 512], BF16) for i in range(3)]
    GB_ = 8
    stg = [k.sbuf("stg%d" % i, [128, GB_, 129], F32) for i in range(2)]
    a3 = [k.sbuf("a3_%d" % i, [128, 8, 129], F32) for i in range(3)]
    sq8 = k.sbuf("sq8", [128, 8, 128], BF16)
    s8 = k.sbuf("s8", [128, 8], F32)
    d8 = k.sbuf("d8", [128, 8], F32)
    y1 = k.sbuf("y1", [128, 8, 128], F32)
    yo = [k.sbuf("yo%d" % i, [128, 8, 128], BF16) for i in range(2)]
    vi = 0; bi = 0; gi = 0; yi = 0
    def second_pass(u):
        nonlocal yi
        acc = accd[u % 2]; aR = accR[u % 2]; ga = gaL[u % 2]
        for qd in range(S // 1024):
            for pi_ in range(3):
                src = acc[pi_, qd * 1024:(qd + 1) * 1024, :].rearrange("(p t) c -> p t c", p=128)
                k.dma(a3[pi_][:], src, reads=[aR], writes=[a3[pi_]])
            k.op("dve", lambda e: e.tensor_tensor(out=a3[0][:], in0=a3[0][:], in1=a3[1][:], op=ALU.add),
                 reads=[a3[0], a3[1]], writes=[a3[0]])
            k.op("dve", lambda e: e.tensor_tensor(out=a3[0][:], in0=a3[0][:], in1=a3[2][:], op=ALU.add),
                 reads=[a3[0], a3[2]], writes=[a3[0]])
            num = a3[0][:, :, 0:128]
            den = a3[0][:, :, 128]
            k.op("act", lambda e: e.activation(out=sq8[:], in_=num, func=ACT.Square), reads=[a3[0]], writes=[sq8])
            k.op("dve", lambda e: e.tensor_reduce(out=s8[:], in_=sq8[:], axis=AX.X, op=ALU.add), reads=[sq8], writes=[s8])
            k.op("dve", lambda e: e.tensor_tensor(out=d8[:], in0=den, in1=den, op=ALU.mult), reads=[a3[0]], writes=[d8])
            k.op("dve", lambda e: e.tensor_scalar(out=d8[:], in0=d8[:], scalar1=EPS, scalar2=None, op0=ALU.mult), reads=[d8], writes=[d8])
            k.op("dve", lambda e: e.scalar_tensor_tensor(out=s8[:], in0=s8[:], scalar=1.0 / 128, in1=d8[:], op0=ALU.mult, op1=ALU.add),
                 reads=[s8, d8], writes=[s8])
            k.op("act", lambda e: e.activation(out=s8[:], in_=s8[:], func=ACT.Sqrt), reads=[s8], writes=[s8])
            k.op("dve", lambda e: e.reciprocal(out=s8[:], in_=s8[:]), reads=[s8], writes=[s8])
            k.op("dve", lambda e: e.tensor_tensor(out=y1[:], in0=num, in1=s8[:].unsqueeze(2).broadcast_to([128, 8, 128]), op=ALU.mult),
                 reads=[a3[0], s8], writes=[y1])
            yo_ = yo[yi % 2]; yi += 1
            k.op("pool", lambda e: e.tensor_tensor(out=yo_[:], in0=y1[:], in1=ga[:].unsqueeze(1).broadcast_to([128, 8, 128]), op=ALU.mult),
                 reads=[y1, ga], writes=[yo_])
            k.dma(yad[u, qd * 1024:(qd + 1) * 1024, :].rearrange("(p t) c -> p t c", p=128), yo_[:], reads=[yo_], writes=[yaR])

    for u in range(NU):
        acc = accd[u % 2]; aR = accR[u % 2]
        k.dma(qT[:], qTd[u], writes=[qT])
        k.dma(kT[:], kTd[u], writes=[kT])
        ga = gaL[u % 2]
        k.dma(ga[:], gad[u], writes=[ga])
        def load_v(uu, d, V):
            nblk_ = S // (128 * d)
            for r in range(d):
                base = 1024 - 64 * d + r
                L = (nblk_ + 1) * 128 * d
                src = vpd[uu, base:base + L, :].rearrange("(j p d) c -> p j d c", p=128, d=d)[:, :, 0, :]
                for j0 in range(0, nblk_ + 1, 16):
                    j1 = min(nblk_ + 1, j0 + 16)
                    k.dma(V[:, r * (nblk_ + 1) + j0:r * (nblk_ + 1) + j1, :], src[:, j0:j1, :], writes=[V])

        if u == 0:
            load_v(0, PATS[0], Vb[vi % 2])
        for pi_, d in enumerate(PATS):
            nblk = S // (128 * d)
            V = Vb[vi % 2]; vi += 1
            if pi_ + 1 < len(PATS):
                load_v(u, PATS[pi_ + 1], Vb[vi % 2])
            elif u + 1 < NU:
                load_v(u + 1, PATS[0], Vb[vi % 2])
            groups = [(r, n) for r in range(d) for n in range(0, nblk, 2)]

            def emit_qk(gidx_):
                r, n = groups[gidx_]
                s_ = st[gidx_ % 2]
                for bb in range(2):
                    qs = (128 * (n + bb)) * d + r
                    qcols = qT[:, qs:qs + 127 * d + 1:d]
                    for half in range(2):
                        ks = 1024 + (128 * (n + bb + half) - 64) * d + r
                        c0 = bb * 256 + half * 128
                        k.op("pe", lambda e: e.matmul(s_[:, c0:c0 + 128], lhsT=kT[:, ks:ks + 127 * d + 1:d],
                                                      rhs=qcols, start=True, stop=True),
                             reads=[kT, qT], writes=[s_], signal=(bb == 1 and half == 1))

            emit_qk(0)
            sg = None
            if pi_ == 1 and u > 0:
                second_pass(u - 1)
            for gix_, (r, n) in enumerate(groups):
                n0 = (n // GB_) * GB_
                if n == n0:
                    sg = stg[gi % 2]; gi += 1
                s_ = st[gix_ % 2]; o_ = ops[gix_ % 2]; p_ = pt[gix_ % 3]
                k.op("act", lambda e: e.activation(out=p_[:], in_=s_[:], func=ACT.Exp), reads=[s_], writes=[p_])
                if gix_ + 1 < len(groups):
                    emit_qk(gix_ + 1)
                k.op("dve" if bi % 2 == 0 else "pool",
                     lambda e: e.tensor_tensor(out=p_[:].rearrange("p (b c) -> p b c", b=2), in0=p_[:].rearrange("p (b c) -> p b c", b=2),
                                               in1=mask[:].unsqueeze(1).broadcast_to([128, 2, 256]), op=ALU.mult),
                     reads=[p_, mask], writes=[p_])
                for bb in range(2):
                    for half in range(2):
                        c0 = bb * 256 + half * 128
                        k.op("pe", lambda e: e.matmul(o_[:, bb, :], lhsT=p_[:, c0:c0 + 128],
                                                      rhs=V[:, r * (nblk + 1) + n + bb + half, :], start=(half == 0), stop=(half == 1)),
                             reads=[p_, V], writes=[o_], signal=(bb == 1 and half == 1))
                if bi % 2 == 0:
                    k.op("dve", lambda e: e.tensor_copy(out=sg[:, n - n0:n - n0 + 2, :], in_=o_[:]), reads=[o_], writes=[sg])
                else:
                    k.op("act", lambda e: e.activation(out=sg[:, n - n0:n - n0 + 2, :], in_=o_[:], func=ACT.Copy), reads=[o_], writes=[sg])
                bi += 1
                n1 = min(nblk, n0 + GB_)
                if n + 2 >= n1:
                    seg = acc[pi_, 128 * n0 * d:128 * n1 * d, :].rearrange("(n p d) c -> p n d c", p=128, d=d)[:, :, r, :]
                    k.dma(seg, sg[:, 0:n1 - n0, :], reads=[sg], writes=[aR], q="act")
    second_pass(NU - 1)
    if NF:
        UX = k.sbuf("UX", [128, 16384], BF16)
        Z = k.sbuf("Z", [128, 128, 2 * N1], BF16)
        D64 = k.sbuf("D64", [N1, 2 * N1], BF16)
        CS = k.sbuf("CS", [128, 256], BF16)
        gf = k.sbuf("gf_s", [128, 128], F32)
        GAB = [k.sbuf("GAB%d" % i, [128, 2, 4, 256], BF16) for i in range(2)]
        pz = [k.psum("pz%d" % i, [128, 512], F32) for i in range(2)]
        ys = k.sbuf("ys", [128, N1, 128], BF16)
        sq4 = k.sbuf("sq4", [128, 4, 128], BF16)
        s4 = k.sbuf("s4f", [128, 4], F32)
        y4 = k.sbuf("y4", [128, 4, 128], F32)
        k.dma(D64[:], d64d, writes=[D64])
        k.dma(CS[:], csd, writes=[CS])
        zi = 0; gbi = 0
        for f in range(NF):
            k.dma(UX[0:N1, 0:16384], ufd[f], writes=[UX])
            k.dma(gf[:], gfd[f], writes=[gf])
            W2 = 2 * N1
            per = 512 // W2
            for c0 in range(0, 128, per):
                p_ = pz[zi % 2]; zi += 1
                for j in range(per):
                    c = c0 + j
                    k.op("pe", lambda e: e.matmul(p_[:, j * W2:(j + 1) * W2], lhsT=UX[0:N1, c:16384:128], rhs=D64[:], start=True, stop=True),
                         reads=[UX, D64], writes=[p_], signal=(j == per - 1))
                k.op("act" if zi % 2 else "dve",
                     (lambda e: e.activation(out=Z[:, c0:c0 + per, :].rearrange("p c w -> p (c w)"), in_=p_[:, 0:per * W2], func=ACT.Copy)) if zi % 2 else
                     (lambda e: e.tensor_copy(out=Z[:, c0:c0 + per, :].rearrange("p c w -> p (c w)"), in_=p_[:, 0:per * W2])),
                     reads=[p_], writes=[Z])
            XT = UX[:, 0:N1 * 256].rearrange("p (k w) -> p k w", w=256)
            for kg in range(0, N1, 4):
                G = GAB[gbi % 2]; gbi += 1
                kn = min(4, N1 - kg)
                k.dma(G[:, 0, 0:kn, :], gabd[0, :, kg:kg + kn, :], writes=[G])
                k.dma(G[:, 1, 0:kn, :], gabd[1, :, kg:kg + kn, :], writes=[G])
                for k1 in range(kg, kg + kn):
                    p_ = pz[zi % 2]; zi += 1
                    k.op("pe", lambda e: e.matmul(p_[:, 0:256], lhsT=Z[:, :, k1], rhs=G[:, 0, k1 - kg, :], start=True, stop=False),
                         reads=[Z, G], writes=[p_], signal=False)
                    k.op("pe", lambda e: e.matmul(p_[:, 0:256], lhsT=Z[:, :, N1 + k1], rhs=G[:, 1, k1 - kg, :], start=False, stop=True),
                         reads=[Z, G], writes=[p_])
                    if zi % 2:
                        k.op("act", lambda e: e.activation(out=XT[:, k1, :], in_=p_[:, 0:256], func=ACT.Copy), reads=[p_], writes=[UX])
                    else:
                        k.op("dve", lambda e: e.tensor_copy(out=XT[:, k1, :], in_=p_[:, 0:256]), reads=[p_], writes=[UX])
            for k0 in range(0, N1, 4):
                p_ = pz[zi % 2]; zi += 1
                for j in range(4):
                    k1 = k0 + j
                    k.op("pe", lambda e: e.matmul(p_[:, j * 128:(j + 1) * 128], lhsT=XT[:, k1, 0:128], rhs=CS[:, 0:128], start=True, stop=False),
                         reads=[UX, CS], writes=[p_], signal=False)
                    k.op("pe", lambda e: e.matmul(p_[:, j * 128:(j + 1) * 128], lhsT=XT[:, k1, 128:256], rhs=CS[:, 128:256], start=False, stop=True),
                         reads=[UX, CS], writes=[p_], signal=(j == 3))
                pv = p_[:].rearrange("p (j c) -> p j c", j=4)
                k.op("act", lambda e: e.activation(out=sq4[:], in_=pv, func=ACT.Square), reads=[p_], writes=[sq4])
                k.op("dve", lambda e: e.tensor_reduce(out=s4[:], in_=sq4[:], axis=AX.X, op=ALU.add), reads=[sq4], writes=[s4])
                k.op("dve", lambda e: e.tensor_scalar(out=s4[:], in0=s4[:], scalar1=1.0 / 128, scalar2=EPS, op0=ALU.mult, op1=ALU.add),
                     reads=[s4], writes=[s4])
                k.op("act", lambda e: e.activation(out=s4[:], in_=s4[:], func=ACT.Sqrt), reads=[s4], writes=[s4])
                k.op("dve", lambda e: e.reciprocal(out=s4[:], in_=s4[:]), reads=[s4], writes=[s4])
                k.op("dve", lambda e: e.tensor_tensor(out=y4[:], in0=pv, in1=s4[:].unsqueeze(2).broadcast_to([128, 4, 128]), op=ALU.mult),
                     reads=[p_, s4], writes=[y4])
                k.op("pool", lambda e: e.tensor_tensor(out=ys[:, k0:k0 + 4, :], in0=y4[:], in1=gf[:].unsqueeze(1).broadcast_to([128, 4, 128]), op=ALU.mult),
                     reads=[y4, gf], writes=[ys])
            k.dma(yfd[f].rearrange("(k2 k1) c -> k2 k1 c", k1=N1), ys[:], reads=[ys], writes=[yfR])
    k.finish("sp")
    k.close()
    return nc


def fourier_tables(S):
    N1 = S // 128
    n1 = np.arange(N1)
    th = 2 * np.pi * np.outer(n1, n1) / N1
    d64 = np.concatenate([np.cos(th), -np.sin(th)], axis=1)
    n2 = np.arange(128)[:, None, None]; k1 = np.arange(N1)[None, :, None]; k2 = np.arange(128)[None, None, :]
    th = 2 * np.pi * ((n2 * (k1 + N1 * k2)) % S) / S
    Gr, Gi = np.cos(th), -np.sin(th)
    ga = np.concatenate([Gr, Gi], axis=2); gb = np.concatenate([-Gi, Gr], axis=2)
    c = np.arange(128)
    ph = 2 * np.pi * np.outer(c, c) / 128
    cs = np.concatenate([np.cos(ph), np.sin(ph)], axis=1)
    return d64, np.stack([ga, gb]), cs


def build_pc(NT=2048, HALF=512, NCB=8):
    nc = bass.Bass("TRN2", target_bir_lowering=False)
    NTT = NT // 128
    yTd = nc.dram_tensor("yT", [4096, NT], BF16, kind="ExternalInput").ap()
    x = nc.dram_tensor("x", [NT, 4096], F32, kind="ExternalInput").ap()
    w = nc.dram_tensor("w", [4096, NCB * 512], F32, kind="ExternalInput").ap()
    g1d = nc.dram_tensor("g1", [128, 4096], F32, kind="ExternalInput").ap()
    cols = nc.dram_tensor("cols", [128, 96], F32, kind="ExternalInput").ap()
    wrd = nc.dram_tensor("wr", [128, 32 * 16], F32, kind="ExternalInput").ap()
    identd = nc.dram_tensor("ident", [128, 128], F32, kind="ExternalInput").ap()
    x1 = nc.dram_tensor("x1", [NT, 4096], F32, kind="ExternalOutput").ap()
    affd = nc.dram_tensor("aff", [NT, 16], F32, kind="ExternalOutput").ap()
    wbf = nc.dram_tensor("wbf", [NCB, 128, 32 * 512], BF16, kind="Internal").ap()
    k = K(nc)
    x1R = k.dram("x1R", x1); affR = k.dram("affR", affd)
    wbfR = [k.dram("wbfR%d" % i, wbf[i]) for i in range(NCB)]
    colt = k.sbuf("colt", [128, 96], F32)
    Gc = k.sbuf("Gc", [128, 32], F32)
    g1 = k.sbuf("g1s", [128, 4096], F32)
    wr = k.sbuf("wrs", [128, 32, 16], F32)
    ident = k.sbuf("ident_s", [128, 128], F32)
    yT = [k.sbuf("yTs%d" % i, [128, 32, 128], BF16) for i in range(HALF // 128)]
    Wb = [k.sbuf("Wb%d" % i, [128, 32, 512], BF16) for i in range(2)]
    stage = [k.sbuf("stg%d" % i, [128, 4, 512], F32) for i in range(2)]
    xt = k.sbuf("xt", [128, 4096], F32)
    tmp = {"ssq": k.sbuf("ssq", [128, 1], F32), "rs": k.sbuf("rs", [128, 1], F32),
           "xs": k.sbuf("xs", [128, 4096], F32), "junk": None}
    h32 = k.sbuf("h32", [128, 32, 128], F32)
    xc = [k.sbuf("xc%d" % i, [128, 512], F32) for i in range(3)]
    tt_ = [k.sbuf("tt%d" % i, [128, 512], F32) for i in range(3)]
    pT = [k.psum("pT%d" % i, [128, 4, 128], F32) for i in range(2)]
    ps = [k.psum("ps%d" % i, [128, 512], F32) for i in range(3)]
    pr = k.psum("pr", [128, 16], F32)
    mx = k.sbuf("mx", [128, 1], F32); sm = k.sbuf("sm", [128, 1], F32)
    ex = k.sbuf("ex", [128, 16], F32); af = k.sbuf("af", [128, 16], F32)
    k.dma(colt[:], cols, writes=[colt]); k.dma(g1[:], g1d, writes=[g1])
    k.dma(wr[:].rearrange("p a b -> p (a b)"), wrd, writes=[wr]); k.dma(ident[:], identd, writes=[ident])
    k.op("dve", lambda e: e.tensor_scalar(out=Gc[:], in0=colt[:, 32:64], scalar1=1.0, scalar2=None, op0=ALU.add), reads=[colt], writes=[Gc])
    k.op("dve", lambda e: e.tensor_tensor(out=Gc[:], in0=Gc[:], in1=colt[:, 0:32], op=ALU.mult), reads=[Gc, colt], writes=[Gc])
    it = 0; pi = 0; oi = 0
    for half in range(NT // HALF):
        for tt in range(HALF // 128):
            k.dma(yT[tt][:], yTd[:, half * HALF + tt * 128:half * HALF + (tt + 1) * 128].rearrange("(kc p) t -> p kc t", p=128), writes=[yT[tt]])
        for cb in range(NCB):
            W = Wb[it % 2]
            if it == 0:
                load_w_block(k, w[:, cb * 512:(cb + 1) * 512], stage, W, it)
                k.dma(wbf[cb], W[:].rearrange("p a b -> p (a b)"), reads=[W], writes=[wbfR[cb]], q="act")
            it += 1
            nxt = it if it < (NT // HALF) * NCB else None
            nparts = HALF // 128
            for tt in range(HALF // 128):
                gt = half * (HALF // 128) + tt
                p = ps[pi % 3]; pi += 1
                xcb = xc[oi % 3]; tb = tt_[oi % 3]; oi += 1
                k.dma(xcb[:], x[gt * 128:(gt + 1) * 128, cb * 512:(cb + 1) * 512], writes=[xcb])
                for kc in range(32):
                    k.op("pe", lambda e: e.matmul(p[:], lhsT=yT[tt][:, kc, :], rhs=W[:, kc, :],
                                                  start=(kc == 0), stop=(kc == 31)), reads=[yT[tt], W], writes=[p], signal=(kc == 31))
                if nxt is not None:
                    ncb = nxt % NCB
                    if nxt < NCB:
                        for s_ in range(8):
                            if s_ * nparts // 8 == tt:
                                load_w_stage(k, w[:, ncb * 512:(ncb + 1) * 512], stage, Wb[nxt % 2], nxt, s_)
                                if s_ == 7:
                                    k.dma(wbf[ncb], Wb[nxt % 2][:].rearrange("p a b -> p (a b)"), reads=[Wb[nxt % 2]],
                                          writes=[wbfR[ncb]], q="act")
                    elif tt == 0:
                        k.dma(Wb[nxt % 2][:].rearrange("p a b -> p (a b)"), wbf[ncb], reads=[wbfR[ncb]], writes=[Wb[nxt % 2]])
                k.op("dve", lambda e: e.tensor_tensor(out=tb[:], in0=p[:], in1=g1[:, cb * 512:(cb + 1) * 512], op=ALU.mult),
                     reads=[p, g1], writes=[tb])
                k.op("pool", lambda e: e.tensor_tensor(out=tb[:], in0=tb[:], in1=xcb[:], op=ALU.add), reads=[tb, xcb], writes=[tb])
                k.dma(x1[gt * 128:(gt + 1) * 128, cb * 512:(cb + 1) * 512], tb[:], reads=[tb], writes=[x1R], q="act")
    Sv = _ColView(colt, 64)
    for gt in range(NTT):
        k.dma(xt[:], x1[gt * 128:(gt + 1) * 128, :], reads=[x1R], writes=[xt])
        norm_transpose_affine(k, xt, ident, Gc, Sv, None, None, pT, tmp, dst32=h32)
        for kc in range(32):
            k.op("pe", lambda e: e.matmul(pr[:], lhsT=h32[:, kc, :], rhs=wr[:, kc, :], start=(kc == 0), stop=(kc == 31)),
                 reads=[h32, wr], writes=[pr], signal=(kc == 31))
        k.op("dve", lambda e: e.tensor_reduce(out=mx[:], in_=pr[:], axis=AX.X, op=ALU.max), reads=[pr], writes=[mx])
        k.op("dve", lambda e: e.tensor_scalar(out=mx[:], in0=mx[:], scalar1=-1.0, scalar2=None, op0=ALU.mult), reads=[mx], writes=[mx])
        k.op("act", lambda e: e.activation(out=ex[:], in_=pr[:], func=ACT.Exp, bias=mx[:, 0:1], accum_out=sm[:]),
             reads=[pr, mx], writes=[ex, sm])
        k.op("dve", lambda e: e.reciprocal(out=sm[:], in_=sm[:]), reads=[sm], writes=[sm])
        k.op("dve", lambda e: e.tensor_scalar(out=af[:], in0=ex[:], scalar1=sm[:, 0:1], scalar2=None, op0=ALU.mult),
             reads=[ex, sm], writes=[af])
        k.dma(affd[gt * 128:(gt + 1) * 128, :], af[:], reads=[af], writes=[affR])
    k.finish("sp"); k.close()
    return nc


CAP = 1024
ZROW = 16 * 1024

def build_pd1(NITER=34):
    nc = bass.Bass("TRN2", target_bir_lowering=False)
    affd = nc.dram_tensor("affu", [128, 4 * 64], F32, kind="ExternalInput").ap()
    eoffd = nc.dram_tensor("eoff", [128, 4], F32, kind="ExternalInput").ap()
    onesd = nc.dram_tensor("ones", [128, 128], F32, kind="ExternalInput").ap()
    lowd = nc.dram_tensor("lstrict", [128, 128], F32, kind="ExternalInput").ap()
    identd = nc.dram_tensor("ident", [128, 128], F32, kind="ExternalInput").ap()
    gidxd = nc.dram_tensor("gidx", [128, 4 * 64], I32, kind="ExternalOutput").ap()
    maskd = nc.dram_tensor("msk", [128, 4 * 64], F32, kind="ExternalOutput").ap()
    k = K(nc)
    aff = k.sbuf("aff", [128, 4, 64], F32); eoff = k.sbuf("eoff_s", [128, 4], F32)
    ones = k.sbuf("ones_s", [128, 128], F32); low = k.sbuf("low_s", [128, 128], F32); ident = k.sbuf("ident_s", [128, 128], F32)
    lo = k.sbuf("lo", [128, 4], F32); hi = k.sbuf("hi", [128, 4], F32); mid = k.sbuf("mid", [128, 4], F32)
    cmp_ = k.sbuf("cmp", [128, 4, 64], F32); cnt = k.sbuf("cnt", [128, 4], F32); ge = k.sbuf("ge", [128, 4], F32)
    d1 = k.sbuf("d1", [128, 4], F32)
    tot = k.psum("tot", [128, 4], F32)
    k.dma(aff[:].rearrange("p a b -> p (a b)"), affd, writes=[aff]); k.dma(eoff[:], eoffd, writes=[eoff])
    k.dma(ones[:], onesd, writes=[ones]); k.dma(low[:], lowd, writes=[low]); k.dma(ident[:], identd, writes=[ident])
    k.op("dve", lambda e: e.memset(lo[:], 0.0), writes=[lo])
    k.op("dve", lambda e: e.memset(hi[:], 1.0), writes=[hi])
    for it in range(NITER):
        k.op("dve", lambda e: e.tensor_tensor(out=mid[:], in0=lo[:], in1=hi[:], op=ALU.add), reads=[lo, hi], writes=[mid])
        k.op("dve", lambda e: e.tensor_scalar(out=mid[:], in0=mid[:], scalar1=0.5, scalar2=None, op0=ALU.mult), reads=[mid], writes=[mid])
        k.op("dve", lambda e: e.tensor_tensor(out=cmp_[:], in0=aff[:], in1=mid[:].unsqueeze(2).broadcast_to([128, 4, 64]), op=ALU.is_ge),
             reads=[aff, mid], writes=[cmp_])
        k.op("dve", lambda e: e.tensor_reduce(out=cnt[:], in_=cmp_[:], axis=AX.X, op=ALU.add), reads=[cmp_], writes=[cnt])
        k.op("pe", lambda e: e.matmul(tot[:], lhsT=ones[:], rhs=cnt[:], start=True, stop=True), reads=[ones, cnt], writes=[tot])
        k.op("dve", lambda e: e.tensor_scalar(out=ge[:], in0=tot[:], scalar1=float(CAP), scalar2=None, op0=ALU.is_ge), reads=[tot], writes=[ge])
        k.op("dve", lambda e: e.tensor_tensor(out=d1[:], in0=mid[:], in1=lo[:], op=ALU.subtract), reads=[mid, lo], writes=[d1])
        k.op("dve", lambda e: e.tensor_tensor(out=d1[:], in0=d1[:], in1=ge[:], op=ALU.mult), reads=[d1, ge], writes=[d1])
        k.op("dve", lambda e: e.tensor_tensor(out=lo[:], in0=lo[:], in1=d1[:], op=ALU.add), reads=[lo, d1], writes=[lo])
        k.op("dve", lambda e: e.tensor_tensor(out=d1[:], in0=hi[:], in1=mid[:], op=ALU.subtract), reads=[hi, mid], writes=[d1])
        k.op("dve", lambda e: e.tensor_tensor(out=d1[:], in0=d1[:], in1=ge[:], op=ALU.mult), reads=[d1, ge], writes=[d1])
        k.op("dve", lambda e: e.tensor_tensor(out=hi[:], in0=mid[:], in1=d1[:], op=ALU.add), reads=[mid, d1], writes=[hi])
    msk = k.sbuf("msk_s", [128, 4, 64], F32)
    k.op("dve", lambda e: e.tensor_tensor(out=msk[:], in0=aff[:], in1=lo[:].unsqueeze(2).broadcast_to([128, 4, 64]), op=ALU.is_ge),
         reads=[aff, lo], writes=[msk])
    k.op("dve", lambda e: e.tensor_reduce(out=cnt[:], in_=msk[:], axis=AX.X, op=ALU.add), reads=[msk], writes=[cnt])
    offp = k.psum("offp", [128, 4], F32)
    k.op("pe", lambda e: e.matmul(offp[:], lhsT=low[:], rhs=cnt[:], start=True, stop=True), reads=[low, cnt], writes=[offp])
    offs = k.sbuf("offs", [128, 4], F32)
    k.op("dve", lambda e: e.tensor_copy(out=offs[:], in_=offp[:]), reads=[offp], writes=[offs])
    mT = k.sbuf("mT", [64, 128], F32)
    tp = k.psum("tp", [64, 128], F32)
    wp = k.psum("wp", [128, 64], F32)
    pos = k.sbuf("pos", [128, 4, 64], F32)
    sel = k.sbuf("sel", [128, 4, 64], F32)
    gi = k.sbuf("gi", [128, 4, 64], I32)
    for u in range(4):
        k.op("pe", lambda e: e.transpose(out=tp[:], in_=msk[:, u, :], identity=ident[:]), reads=[msk, ident], writes=[tp])
        k.op("dve", lambda e: e.tensor_copy(out=mT[:], in_=tp[:]), reads=[tp], writes=[mT])
        k.op("pe", lambda e: e.matmul(wp[:], lhsT=mT[:], rhs=low[0:64, 0:64], start=True, stop=True), reads=[mT, low], writes=[wp])
        k.op("dve", lambda e: e.tensor_scalar(out=pos[:, u, :], in0=wp[:], scalar1=offs[:, u:u + 1], scalar2=None, op0=ALU.add),
             reads=[wp, offs], writes=[pos])
    k.op("dve", lambda e: e.tensor_scalar(out=sel[:], in0=pos[:], scalar1=float(CAP), scalar2=None, op0=ALU.is_lt), reads=[pos], writes=[sel])
    k.op("dve", lambda e: e.tensor_tensor(out=sel[:], in0=sel[:], in1=msk[:], op=ALU.mult), reads=[sel, msk], writes=[sel])
    k.op("dve", lambda e: e.tensor_tensor(out=pos[:], in0=pos[:], in1=eoff[:].unsqueeze(2).broadcast_to([128, 4, 64]), op=ALU.add),
         reads=[pos, eoff], writes=[pos])
    k.op("dve", lambda e: e.tensor_tensor(out=pos[:], in0=pos[:], in1=sel[:], op=ALU.mult), reads=[pos, sel], writes=[pos])
    k.op("dve", lambda e: e.tensor_scalar(out=pos[:], in0=pos[:], scalar1=float(ZROW), scalar2=None, op0=ALU.add), reads=[pos], writes=[pos])
    k.op("dve", lambda e: e.tensor_copy(out=gi[:], in_=pos[:]), reads=[pos], writes=[gi])
    gR = k.dram("gR", gidxd); mR = k.dram("mR", maskd)
    k.dma(gidxd, gi[:].rearrange("p a b -> p (a b)"), reads=[gi], writes=[gR])
    k.dma(maskd, sel[:].rearrange("p a b -> p (a b)"), reads=[sel], writes=[mR])
    k.finish("sp"); k.close()
    return nc

def d1_consts():
    ii = np.arange(128)
    return {"ones": np.ones((128, 128), np.float32), "lstrict": (ii[:, None] < ii[None, :]).astype(np.float32),
            "ident": np.eye(128, dtype=np.float32)}


def build_pd2(NUNIT=4, NSL=1024):
    nc = bass.Bass("TRN2", target_bir_lowering=False)
    NST = NSL // 128
    xed = nc.dram_tensor("xe", [NUNIT, NSL, 4096], F32, kind="ExternalInput").ap()
    afd = nc.dram_tensor("affs", [NUNIT, 128, NST], F32, kind="ExternalInput").ap()
    cold = nc.dram_tensor("cols", [NUNIT, 128, 96], F32, kind="ExternalInput").ap()
    NE = (NUNIT + 1) // 2
    wgd = nc.dram_tensor("wg", [NE, 4096, 1024], F32, kind="ExternalInput").ap()
    wud = nc.dram_tensor("wu", [NE, 4096, 1024], F32, kind="ExternalInput").ap()
    wdd = nc.dram_tensor("wd", [NE, 1024, 4096], F32, kind="ExternalInput").ap()
    identd = nc.dram_tensor("ident", [128, 128], F32, kind="ExternalInput").ap()
    yed = nc.dram_tensor("ye", [NUNIT, NSL, 4096], F32, kind="ExternalOutput").ap()
    k = K(nc)
    yR = k.dram("yR", yed)
    ident = k.sbuf("ident_s", [128, 128], F32)
    k.dma(ident[:], identd, writes=[ident])
    coltL = [k.sbuf("colt%d" % i, [128, 96], F32) for i in range(2)]; GcL = [k.sbuf("Gc%d" % i, [128, 32], F32) for i in range(2)]
    afsL = [k.sbuf("afs%d" % i, [128, NST], F32) for i in range(2)]
    xt = k.sbuf("xt", [128, 4096], F32)
    tmp = {"ssq": k.sbuf("ssq", [128, 1], F32), "rs": k.sbuf("rs", [128, 1], F32), "xs": k.sbuf("xs", [128, 4096], F32), "junk": None}
    pT = [k.psum("pT%d" % i, [128, 4, 128], F32) for i in range(2)]
    stg = [k.sbuf("stg%d" % i, [128, 4096], F32) for i in range(2)]
    wb = [k.sbuf("wb%d" % i, [128, 4096], BF16) for i in range(4)]
    h1T = k.sbuf("h1T", [128, 8, NSL], BF16)
    sg = [k.sbuf("sg%d" % i, [128, 512], F32) for i in range(2)]
    pg = [k.psum("pg%d" % i, [128, 512], F32) for i in range(2)]
    pu = [k.psum("pu%d" % i, [128, 512], F32) for i in range(2)]
    py = [k.psum("py%d" % i, [128, 512], F32) for i in range(2)]
    ot = [k.sbuf("ot%d" % i, [128, 512], F32) for i in range(3)]
    si = 0; wi = 0; gi = 0; yi = 0; oi = 0
    NSH = max(1, NSL // 512); SHW = min(512, NSL)
    TPH = SHW // 128
    xeTL = [k.sbuf("xeT%d" % i, [128, 32, SHW], BF16) for i in range(NSH)]

    def prep_unit(u):
        colt = coltL[u % 2]; Gc = GcL[u % 2]
        k.dma(colt[:], cold[u], writes=[colt]); k.dma(afsL[u % 2][:], afd[u], writes=[afsL[u % 2]])
        k.op("dve", lambda e: e.tensor_scalar(out=Gc[:], in0=colt[:, 32:64], scalar1=1.0, scalar2=None, op0=ALU.add), reads=[colt], writes=[Gc])
        k.op("dve", lambda e: e.tensor_tensor(out=Gc[:], in0=Gc[:], in1=colt[:, 0:32], op=ALU.mult), reads=[Gc, colt], writes=[Gc])

    def norm_tile(u, st):
        k.dma(xt[:], xed[u, st * 128:(st + 1) * 128, :], writes=[xt])
        c0 = (st % TPH) * 128
        norm_transpose_affine(k, xt, ident, GcL[u % 2], _ColView(coltL[u % 2], 64), xeTL[st // TPH], slice(c0, c0 + 128), pT, tmp)

    prep_unit(0)
    for st in range(NST):
        norm_tile(0, st)
    for u in range(NUNIT):
        e_ = u // 2
        afs = afsL[u % 2]
        def load_gu(fc):
            nonlocal si, wi
            wbs = []
            for wsrc in (wgd, wud):
                s_ = stg[si % 2]; si += 1
                b_ = wb[wi % 4]; wi += 1
                sv = s_[:].rearrange("p (kc n) -> p kc n", n=128)
                src = wsrc[e_, :, fc * 128:(fc + 1) * 128].rearrange("(kc p) n -> p kc n", p=128)
                k.dma(sv[:, 0:16, :], src[:, 0:16, :], writes=[s_])
                k.dma(sv[:, 16:32, :], src[:, 16:32, :], writes=[s_])
                k.op("pool", lambda e: e.tensor_copy(out=b_[:, 0:1536], in_=s_[:, 0:1536]), reads=[s_], writes=[b_])
                k.op("dve", lambda e: e.tensor_copy(out=b_[:, 1536:4096], in_=s_[:, 1536:4096]), reads=[s_], writes=[b_])
                wbs.append(b_)
            return wbs

        def load_d(db):
            nonlocal si, wi
            s_ = stg[si % 2]; si += 1
            b_ = wb[wi % 4]; wi += 1
            sv = s_[:].rearrange("p (fc n) -> p fc n", n=512)
            src = wdd[e_, :, db * 512:(db + 1) * 512].rearrange("(fc p) n -> p fc n", p=128)
            k.dma(sv[:, 0:4, :], src[:, 0:4, :], writes=[s_])
            k.dma(sv[:, 4:8, :], src[:, 4:8, :], writes=[s_])
            k.op("pool", lambda e: e.tensor_copy(out=b_[:, 0:1536], in_=s_[:, 0:1536]), reads=[s_], writes=[b_])
            k.op("dve", lambda e: e.tensor_copy(out=b_[:, 1536:4096], in_=s_[:, 1536:4096]), reads=[s_], writes=[b_])
            return b_

        pend = load_gu(0)
        for fc in range(8):
            wbs = pend
            if fc + 1 < 8:
                pend = load_gu(fc + 1)
            else:
                pend_d = load_d(0)
            for sh in range(NSH):
                g_ = pg[gi % 2]; u_ = pu[gi % 2]; s2 = sg[gi % 2]; gi += 1
                for (pp, b_) in ((g_, wbs[0]), (u_, wbs[1])):
                    bv = b_[:].rearrange("p (kc n) -> p kc n", n=128)
                    for kc in range(32):
                        k.op("pe", lambda e: e.matmul(pp[:, 0:SHW], lhsT=bv[:, kc, :], rhs=xeTL[sh][:, kc, :],
                                                      start=(kc == 0), stop=(kc == 31)), reads=[b_, xeTL[sh]], writes=[pp], signal=(kc == 31))
                k.op("act", lambda e: e.activation(out=s2[:, 0:SHW], in_=g_[:, 0:SHW], func=ACT.Silu), reads=[g_], writes=[s2])
                k.op("dve", lambda e: e.tensor_tensor(out=h1T[:, fc, sh * SHW:(sh + 1) * SHW], in0=s2[:, 0:SHW], in1=u_[:, 0:SHW], op=ALU.mult),
                     reads=[s2, u_], writes=[h1T])
        for db in range(8):
            b_ = pend_d
            if db + 1 < 8:
                pend_d = load_d(db + 1)
            bv = b_[:].rearrange("p (fc n) -> p fc n", n=512)
            for st in range(NST):
                y_ = py[yi % 2]; yi += 1
                for fc in range(8):
                    k.op("pe", lambda e: e.matmul(y_[:], lhsT=h1T[:, fc, st * 128:(st + 1) * 128], rhs=bv[:, fc, :],
                                                  start=(fc == 0), stop=(fc == 7)), reads=[h1T, b_], writes=[y_], signal=(fc == 7))
                o_ = ot[oi % 3]; oi += 1
                k.op("act", lambda e: e.activation(out=o_[:], in_=y_[:], func=ACT.Copy, scale=afs[:, st:st + 1]), reads=[y_, afs], writes=[o_])
                k.dma(yed[u, st * 128:(st + 1) * 128, db * 512:(db + 1) * 512], o_[:], reads=[o_], writes=[yR], q="act")
            if u + 1 < NUNIT:
                if db == 0:
                    prep_unit(u + 1)
                for st2 in range(db * NST // 8, (db + 1) * NST // 8):
                    norm_tile(u + 1, st2)
    k.finish("sp"); k.close()
    return nc


ZROW = 16 * 1024

def build_pe(NTOK=4096, CW=2048):
    nc = bass.Bass("TRN2", target_bir_lowering=False)
    NTT = NTOK // 128
    x1d = nc.dram_tensor("x1c", [NTOK, CW], F32, kind="ExternalInput").ap()
    yed = nc.dram_tensor("yec", [ZROW + 1, CW], F32, kind="ExternalInput").ap()
    gid = nc.dram_tensor("gidx", [128, NTT * 16], I32, kind="ExternalInput").ap()
    g2d = nc.dram_tensor("g2", [128, CW], F32, kind="ExternalInput").ap()
    x2d = nc.dram_tensor("x2c", [NTOK, CW], F32, kind="ExternalOutput").ap()
    k = K(nc)
    xR = k.dram("xR", x2d)
    gix = k.sbuf("gix", [128, NTT * 16], I32)
    g2 = k.sbuf("g2s", [128, CW], F32)
    k.dma(gix[:], gid, writes=[gix]); k.dma(g2[:], g2d, writes=[g2])
    NG = 6
    breg = nc.gpsimd.to_reg(ZROW - 1)
    zt = k.sbuf("zt", [128, CW], F32)
    k.op("dve", lambda e: e.memset(zt[:], 0.0), writes=[zt])
    G = [k.sbuf("G%d" % i, [128, CW], F32) for i in range(NG)]
    xt = [k.sbuf("xt%d" % i, [128, CW], F32) for i in range(2)]
    acc = [k.sbuf("acc%d" % i, [128, CW], F32) for i in range(2)]
    gi = 0
    for tt in range(NTT):
        x_ = xt[tt % 2]; a_ = acc[tt % 2]
        k.dma(x_[:], x1d[tt * 128:(tt + 1) * 128, :], writes=[x_])
        for e_ in range(16):
            g_ = G[gi % NG]; gi += 1
            col = tt * 16 + e_
            en = k.engs["pool"]
            k._deps(en, [gix], [g_])
            k.op("act", lambda e: e.activation(out=g_[:], in_=zt[:], func=ACT.Copy), reads=[zt], writes=[g_])
            k._deps(en, [gix], [g_])
            ins = nc.gpsimd.indirect_dma_start(out=g_[:], out_offset=None, in_=yed,
                                               in_offset=bass.IndirectOffsetOnAxis(ap=gix[:, col:col + 1].bitcast(U32), axis=0),
                                               bounds_check=breg, oob_is_err=False)
            ins.then_inc(g_.sem(), 16)
            g_.dcount += 1
            dep = ("dma", g_, g_.dcount)
            gix.readers.append(dep); g_.writer = dep; g_.readers = []
            if e_ == 0:
                k.op("dve", lambda e: e.tensor_copy(out=a_[:], in_=g_[:]), reads=[g_], writes=[a_])
            else:
                k.op("dve", lambda e: e.tensor_tensor(out=a_[:], in0=a_[:], in1=g_[:], op=ALU.add), reads=[a_, g_], writes=[a_])
        k.op("dve", lambda e: e.tensor_tensor(out=a_[:], in0=a_[:], in1=g2[:], op=ALU.mult), reads=[a_, g2], writes=[a_])
        k.op("pool", lambda e: e.tensor_tensor(out=a_[:], in0=a_[:], in1=x_[:], op=ALU.add), reads=[a_, x_], writes=[a_])
        k.dma(x2d[tt * 128:(tt + 1) * 128, :], a_[:], reads=[a_], writes=[xR])
    k.finish("sp"); k.close()
    return nc


BF = ml_dtypes.bfloat16
_PROGS = {}


def _prog(name, fn):
    if name not in _PROGS:
        _PROGS[name] = fn()
    return _PROGS[name]


def _col(v):
    return np.ascontiguousarray(np.asarray(v, np.float32).reshape(32, 128).T)


def _rep(v):
    v = np.asarray(v, np.float32)
    return np.ascontiguousarray(np.broadcast_to(v[None, :], (128, v.shape[0])))


def kernel(x, c, positions, norm1_gain, norm2_gain, w_ada, b_ada, w_in, q_gain, k_gain,
           out_gain_fourier, out_gain_attn, w_out, w_router, w_gate, w_up, w_down):
    f32 = np.float32
    x = np.asarray(x, f32); c = np.asarray(c, f32); positions = np.asarray(positions, np.int32)
    norm1_gain = np.asarray(norm1_gain, f32); norm2_gain = np.asarray(norm2_gain, f32)
    w_ada = np.asarray(w_ada, f32); b_ada = np.asarray(b_ada, f32); w_in = np.asarray(w_in, f32)
    q_gain = np.asarray(q_gain, f32); k_gain = np.asarray(k_gain, f32)
    out_gain_fourier = np.asarray(out_gain_fourier, f32); out_gain_attn = np.asarray(out_gain_attn, f32)
    w_out = np.asarray(w_out, f32); w_router = np.asarray(w_router, f32)
    w_gate = np.asarray(w_gate, f32); w_up = np.asarray(w_up, f32); w_down = np.asarray(w_down, f32)
    B, S, D = x.shape
    NC = 8
    ident = np.eye(128, dtype=f32)

    cT = np.ascontiguousarray(c.T.reshape(32, 128, 2).transpose(1, 0, 2).reshape(128, 64))
    ims = []
    for cc in range(NC):
        sl = slice(cc * 3072, (cc + 1) * 3072)
        ims.append({"cT": cT, "wa": np.ascontiguousarray(w_ada[:, :, sl]),
                    "ba": np.ascontiguousarray(b_ada[:, sl]).reshape(1, -1)})
    res = run(_prog("p0", build_p0), ims)
    mod = np.zeros((2, 2, 6 * D), f32)
    for cc in range(NC):
        o = res.results[cc]["mod"].reshape(2, 2, 3072)
        mod[:, :, cc * 3072:(cc + 1) * 3072] = o.transpose(1, 0, 2)
    del ims, res

    ii = np.arange(128)
    maskc = np.concatenate([(ii[:, None] >= ii[None, :]), (ii[:, None] <= ii[None, :])], axis=1).astype(BF)
    d64, gab, cs = fourier_tables(S)
    d64 = d64.astype(BF); gab = gab.astype(BF); cs = cs.astype(BF)
    d1c = d1_consts()
    TPC = (B * S) // NC
    CPB = NC // B

    for l in range(2):
        shift1, scale1, gate1, shift2, scale2, gate2 = [mod[l][:, i * D:(i + 1) * D] for i in range(6)]
        ims = []
        for cc in range(NC):
            b = cc // CPB; t0 = (cc % CPB) * TPC
            ims.append(host_inputs_pa(np.ascontiguousarray(x[b, t0:t0 + TPC]), norm1_gain[l], scale1[b], shift1[b], w_in[l],
                                      q_gain[l], k_gain[l], positions[b, t0:t0 + TPC]))
        res = run(_prog("pa", build_pa), ims)
        U = np.stack([res.results[cc]["u"] for cc in range(NC)], 0).reshape(B, S, 10240)
        del ims, res
        ims = []
        for cc in range(NC):
            qT = np.zeros((6, 128, S), BF); kT = np.zeros((6, 128, S + 2048), BF); vp = np.zeros((6, S + 2112, 129), BF)
            ga = np.zeros((6, 128, 128), f32)
            for b in range(B):
                for hh in range(3):
                    h = 3 * cc + hh; u = b * 3 + hh
                    qT[u] = U[b, :, 1024 + h * 128:1024 + (h + 1) * 128].T
                    kT[u, :, 1024:1024 + S] = U[b, :, 4096 + h * 128:4096 + (h + 1) * 128].T
                    vp[u, 1024:1024 + S, :128] = U[b, :, 7168 + h * 128:7168 + (h + 1) * 128]
                    vp[u, 1024:1024 + S, 128] = 1.0
                    ga[u] = _rep(out_gain_attn[l][h * 128:(h + 1) * 128])
            uf = np.stack([np.ascontiguousarray(U[b, :, cc * 128:(cc + 1) * 128]).reshape(S // 128, 128 * 128) for b in range(B)], 0)
            gf = np.stack([_rep(out_gain_fourier[l][cc * 128:(cc + 1) * 128])] * B, 0)
            ims.append({"qT": qT, "kT": kT, "vp": vp, "ga": ga, "mask": maskc, "uf": uf, "d64": d64, "gab": gab, "cs": cs, "gf": gf})
        res = run(_prog("pb", build_pb), ims)
        y = np.zeros((B, S, D), BF)
        for cc in range(NC):
            ya = res.results[cc]["ya"]; yf = res.results[cc]["yf"]
            for b in range(B):
                y[b, :, cc * 128:(cc + 1) * 128] = yf[b]
                for hh in range(3):
                    h = 3 * cc + hh
                    y[b, :, 1024 + h * 128:1024 + (h + 1) * 128] = ya[b * 3 + hh]
        del ims, res, U
        wr = np.ascontiguousarray(w_router[l].reshape(32, 128, 16).transpose(1, 0, 2).reshape(128, 512))
        ims = []
        for cc in range(NC):
            b = cc // CPB; t0 = (cc % CPB) * TPC
            ims.append({"yT": np.ascontiguousarray(y[b, t0:t0 + TPC].T), "x": np.ascontiguousarray(x[b, t0:t0 + TPC]), "w": w_out[l],
                        "g1": _rep(gate1[b]), "cols": np.concatenate([_col(norm2_gain[l]), _col(scale2[b]), _col(shift2[b])], 1),
                        "wr": wr, "ident": ident})
        res = run(_prog("pc", build_pc), ims)
        x1 = np.stack([res.results[cc]["x1"] for cc in range(NC)], 0).reshape(B, S, D)
        aff = np.stack([res.results[cc]["aff"] for cc in range(NC)], 0).reshape(B, S, 16)
        del ims, res, y
        ims = []
        for cc in range(NC):
            affu = np.zeros((128, 4, 64), f32); eo = np.zeros(4, f32)
            for u in range(4):
                e = 2 * cc + u // 2; b = u % 2
                affu[:, u, :] = aff[b, :, e].reshape(128, 64)
                eo[u] = e * 1024 - ZROW
            ims.append({"affu": affu.reshape(128, 256), "eoff": _rep(eo), **d1c})
        res = run(_prog("pd1", build_pd1), ims)
        gfull = np.full((B, S, 16), ZROW, np.int32)
        idxs = {}
        for cc in range(NC):
            g = res.results[cc]["gidx"].reshape(128, 4, 64); m = res.results[cc]["msk"].reshape(128, 4, 64)
            for u in range(4):
                e = 2 * cc + u // 2; b = u % 2
                gfull[b, :, e] = g[:, u, :].reshape(S)
                sel = np.flatnonzero(m[:, u, :].reshape(S) > 0.5)[:1024]
                if sel.shape[0] < 1024:
                    sel = np.concatenate([sel, np.zeros(1024 - sel.shape[0], sel.dtype)])
                idxs[(b, e)] = sel
        del ims, res
        ims = []
        for cc in range(NC):
            xe = np.zeros((4, 1024, D), f32); affs = np.zeros((4, 128, 8), f32); cols = np.zeros((4, 128, 96), f32)
            for u in range(4):
                e = 2 * cc + u // 2; b = u % 2
                sel = idxs[(b, e)]
                xe[u] = x1[b, sel]
                affs[u] = aff[b, sel, e].reshape(8, 128).T
                cols[u] = np.concatenate([_col(norm2_gain[l]), _col(scale2[b]), _col(shift2[b])], 1)
            ims.append({"xe": xe, "affs": affs, "cols": cols, "wg": np.ascontiguousarray(w_gate[l, 2 * cc:2 * cc + 2]),
                        "wu": np.ascontiguousarray(w_up[l, 2 * cc:2 * cc + 2]), "wd": np.ascontiguousarray(w_down[l, 2 * cc:2 * cc + 2]),
                        "ident": ident})
        res = run(_prog("pd2", build_pd2), ims)
        yeall = np.zeros((B, ZROW + 1, D), f32)
        for cc in range(NC):
            ye = res.results[cc]["ye"]
            for u in range(4):
                e = 2 * cc + u // 2; b = u % 2
                yeall[b, e * 1024:(e + 1) * 1024] = ye[u]
        del ims, res
        ims = []
        for cc in range(NC):
            b = cc // CPB; th = (cc % CPB) // 2; ch = cc % 2
            ts_ = slice(th * 4096, (th + 1) * 4096); cs_ = slice(ch * 2048, (ch + 1) * 2048)
            gi = np.ascontiguousarray(gfull[b][ts_].reshape(4096 // 128, 128, 16).transpose(1, 0, 2).reshape(128, -1))
            ims.append({"x1c": np.ascontiguousarray(x1[b][ts_, cs_]), "yec": np.ascontiguousarray(yeall[b][:, cs_]), "gidx": gi,
                        "g2": _rep(gate2[b][cs_])})
        res = run(_prog("pe", build_pe), ims)
        xn = np.zeros((B, S, D), f32)
        for cc in range(NC):
            b = cc // CPB; th = (cc % CPB) // 2; ch = cc % 2
            xn[b][th * 4096:(th + 1) * 4096, ch * 2048:(ch + 1) * 2048] = res.results[cc]["x2c"]
        del ims, res, x1, yeall
        x = xn
    return x
```
